# Optimizing a Trainium2 kernel written in Bass

```python
import math
import jax, jax.numpy as jnp
from jax import lax
import numpy as np

D_MODEL = 1024
BATCH = 8
SEQ = 2048
DEPTH = 4

MIX_WIDTH = D_MODEL
N_DA_HEADS = 4
DA_HEAD_DIM = 64
DA_V_DIM = 2 * DA_HEAD_DIM
DA_WIDTH = N_DA_HEADS * DA_V_DIM
N_GDN_HEADS = 4
GDN_HEAD_DIM = 128
GDN_WIDTH = N_GDN_HEADS * GDN_HEAD_DIM
CONV_WIDTH = 4
GDN_CHUNK = 64
Q_BLOCK = 128
ROPE_THETA = 10000.0
DA_Q_COLS = N_DA_HEADS * 2 * DA_HEAD_DIM
DA_K_COLS = N_DA_HEADS * 2 * DA_HEAD_DIM
DA_V_COLS = DA_WIDTH
GDN_QKV_COLS = 3 * GDN_WIDTH
GDN_Z_COLS = GDN_WIDTH
GDN_A_COLS = N_GDN_HEADS
GDN_B_COLS = N_GDN_HEADS
D_IN_PROJ = DA_Q_COLS + DA_K_COLS + DA_V_COLS + GDN_QKV_COLS + GDN_Z_COLS + GDN_A_COLS + GDN_B_COLS
D_FF_DENSE = 2816
N_EXPERTS = 8
TOP_K = 2
D_FF_EXPERT = 3584
EXPERT_BLOCK = 256
N_DENSE = (DEPTH + 1) // 2
N_MOE = DEPTH // 2
DEEPNORM_ALPHA = (2.0 * DEPTH) ** 0.25
DEEPNORM_BETA = (8.0 * DEPTH) ** -0.25
LN_EPS = 1e-5
RMS_EPS = 1e-6

kernel_name = "hybrid_diffattn_gdn_deepnorm_moe"


def layer_norm(x, g, b):
    xf = x.astype(jnp.float32)
    mu = xf.mean(-1, keepdims=True)
    var = jnp.square(xf - mu).mean(-1, keepdims=True)
    return ((xf - mu) * lax.rsqrt(var + LN_EPS) * g.astype(jnp.float32) + b.astype(jnp.float32)).astype(x.dtype)


def rms_norm(x, w):
    xf = x.astype(jnp.float32)
    return (xf * lax.rsqrt(jnp.mean(xf * xf, -1, keepdims=True) + RMS_EPS) * w.astype(jnp.float32)).astype(x.dtype)


def l2_normalize(x):
    xf = x.astype(jnp.float32)
    return xf * lax.rsqrt(jnp.sum(xf * xf, -1, keepdims=True) + RMS_EPS)


def apply_rope(x, cos, sin):
    half = x.shape[-1] // 2
    x1, x2 = x[..., :half], x[..., half:]
    rot = jnp.concatenate([-x2, x1], axis=-1)
    return x * cos + rot * sin


def diff_attention(q, k, v, lam, lam_init, subln_w, cos, sin):
    B, S, H = q.shape[0], q.shape[1], q.shape[2]
    nb = S // Q_BLOCK
    q = apply_rope(q, cos, sin) * (DA_HEAD_DIM ** -0.5)
    k = apply_rope(k, cos, sin)
    qb = jnp.moveaxis(q.reshape(B, nb, Q_BLOCK, H, 2, DA_HEAD_DIM), 1, 0)
    k_pos = jnp.arange(S)

    def block(args):
        q_blk, bi = args
        s = jnp.einsum('bqhcd,bkhcd->bhcqk', q_blk, k).astype(jnp.float32)
        q_pos = bi * Q_BLOCK + jnp.arange(Q_BLOCK)
        s = jnp.where(q_pos[:, None] >= k_pos[None, :], s, -jnp.inf)
        p = jax.nn.softmax(s, axis=-1)
        p_diff = (p[:, :, 0] - lam * p[:, :, 1]).astype(v.dtype)
        return jnp.einsum('bhqk,bkhe->bqhe', p_diff, v)

    o = lax.map(block, (qb, jnp.arange(nb)))
    o = jnp.moveaxis(o, 0, 1).reshape(B, S, H, DA_V_DIM)
    o = rms_norm(o, subln_w) * (1.0 - lam_init)
    return o.reshape(B, S, H * DA_V_DIM)


def causal_short_conv(x, w):
    C = x.shape[-1]
    return lax.conv_general_dilated(
        x, w.astype(x.dtype)[:, None, :], window_strides=(1,), padding=[(CONV_WIDTH - 1, 0)],
        dimension_numbers=('NWC', 'WIO', 'NWC'), feature_group_count=C)


def gated_delta_rule_chunked(q, k, v, g, beta):
    B, S, H, Dk = q.shape
    Dv = v.shape[-1]
    C = GDN_CHUNK
    n = S // C

    def chunks(t):
        return jnp.moveaxis(t.reshape(B, n, C, H, *t.shape[3:]), 3, 1)

    q, k, v, g, beta = chunks(q), chunks(k), chunks(v), chunks(g), chunks(beta)
    G = jnp.cumsum(g, axis=-1)
    idx = jnp.arange(C)
    tril = idx[:, None] >= idx[None, :]
    strict = idx[:, None] > idx[None, :]
    decay = jnp.exp(jnp.where(tril, G[..., :, None] - G[..., None, :], -jnp.inf))
    k_beta = k * beta[..., None]
    L = jnp.where(strict, jnp.einsum('bhncd,bhnkd->bhnck', k_beta, k) * decay, 0.0)
    tri = L + jnp.eye(C, dtype=L.dtype)
    rhs = jnp.concatenate([v * beta[..., None], k_beta * jnp.exp(G)[..., None]], axis=-1)
    sol = lax.linalg.triangular_solve(tri, rhs, left_side=True, lower=True, unit_diagonal=True)
    u, w = sol[..., :Dv], sol[..., Dv:]
    qk = jnp.where(tril, jnp.einsum('bhncd,bhnkd->bhnck', q, k) * decay, 0.0)
    q_dec = q * jnp.exp(G)[..., None]
    k_tail = k * jnp.exp(G[..., -1:] - G)[..., None]
    chunk_decay = jnp.exp(G[..., -1])

    def step(state, xs):
        u_c, w_c, qk_c, qd_c, kt_c, cd_c = xs
        v_new = u_c - jnp.einsum('bhcd,bhde->bhce', w_c, state)
        out = jnp.einsum('bhcd,bhde->bhce', qd_c, state) + jnp.einsum('bhck,bhke->bhce', qk_c, v_new)
        state = state * cd_c[..., None, None] + jnp.einsum('bhcd,bhce->bhde', kt_c, v_new)
        return state, out

    xs = tuple(jnp.moveaxis(t, 2, 0) for t in (u, w, qk, q_dec, k_tail, chunk_decay))
    _, o = lax.scan(step, jnp.zeros((B, H, Dk, Dv), jnp.float32), xs)
    return jnp.transpose(o, (1, 0, 3, 2, 4)).reshape(B, S, H, Dv)


def hybrid_mixer(x, w_in, conv_w, a_log, dt_bias, gdn_norm_w, lam_q1, lam_k1, lam_q2, lam_k2,
                 subln_w, w_out, cos, sin, lam_init):
    B, S, _ = x.shape
    proj = jnp.einsum('bsd,de->bse', x, w_in)
    splits = np.cumsum([DA_Q_COLS, DA_K_COLS, DA_V_COLS, GDN_QKV_COLS, GDN_Z_COLS, GDN_A_COLS]).tolist()
    da_q, da_k, da_v, g_qkv, g_z, g_a, g_b = jnp.split(proj, splits, axis=-1)

    lam = (jnp.exp(jnp.sum(lam_q1.astype(jnp.float32) * lam_k1.astype(jnp.float32)))
           - jnp.exp(jnp.sum(lam_q2.astype(jnp.float32) * lam_k2.astype(jnp.float32))) + lam_init)
    da_out = diff_attention(
        da_q.reshape(B, S, N_DA_HEADS, 2, DA_HEAD_DIM), da_k.reshape(B, S, N_DA_HEADS, 2, DA_HEAD_DIM),
        da_v.reshape(B, S, N_DA_HEADS, DA_V_DIM), lam, lam_init, subln_w, cos, sin)

    qkv = jax.nn.silu(causal_short_conv(g_qkv, conv_w))
    gq, gk, gv = jnp.split(qkv, 3, axis=-1)
    gq = l2_normalize(gq.reshape(B, S, N_GDN_HEADS, GDN_HEAD_DIM)) * (GDN_HEAD_DIM ** -0.5)
    gk = l2_normalize(gk.reshape(B, S, N_GDN_HEADS, GDN_HEAD_DIM))
    gv = gv.reshape(B, S, N_GDN_HEADS, GDN_HEAD_DIM).astype(jnp.float32)
    beta = jax.nn.sigmoid(g_b.astype(jnp.float32))
    g = -jnp.exp(a_log.astype(jnp.float32)) * jax.nn.softplus(g_a.astype(jnp.float32) + dt_bias.astype(jnp.float32))
    o = gated_delta_rule_chunked(gq, gk, gv, g, beta).astype(x.dtype)
    o = rms_norm(o, gdn_norm_w) * jax.nn.silu(g_z.reshape(B, S, N_GDN_HEADS, GDN_HEAD_DIM))
    gdn_out = o.reshape(B, S, GDN_WIDTH)

    merged = jnp.concatenate([da_out, gdn_out], axis=-1)
    return jnp.einsum('bse,ed->bsd', merged, w_out)


def swiglu(x, w_gate, w_up, w_down):
    return (jax.nn.silu(x @ w_gate) * (x @ w_up)) @ w_down


def moe_ffn(x, router_w, w_gate, w_up, w_down):
    B, S, D = x.shape
    T = B * S
    xf = x.reshape(T, D)
    logits = jnp.einsum('td,de->te', xf, router_w).astype(jnp.float32)
    top_val, top_idx = lax.top_k(logits, TOP_K)
    gates = jax.nn.softmax(top_val, axis=-1)
    A = T * TOP_K
    e = top_idx.reshape(A)
    tok = jnp.repeat(jnp.arange(T, dtype=jnp.int32), TOP_K)
    wts = gates.reshape(A)
    order = jnp.argsort(e, stable=True)
    e_s, tok_s, w_s = e[order], tok[order], wts[order]
    counts = jnp.bincount(e, length=N_EXPERTS)
    starts = jnp.cumsum(counts) - counts
    padded = ((counts + EXPERT_BLOCK - 1) // EXPERT_BLOCK) * EXPERT_BLOCK
    pend = jnp.cumsum(padded)
    pstarts = pend - padded
    dest = pstarts[e_s] + (jnp.arange(A) - starts[e_s])
    P = A + N_EXPERTS * EXPERT_BLOCK
    n_blk = P // EXPERT_BLOCK
    tok_buf = jnp.zeros((P,), jnp.int32).at[dest].set(tok_s)
    w_buf = jnp.zeros((P,), jnp.float32).at[dest].set(w_s)
    blk_e = jnp.minimum(jnp.searchsorted(pend, jnp.arange(n_blk) * EXPERT_BLOCK, side='right'), N_EXPERTS - 1)

    def expert_block(args):
        tok_b, e_b = args
        xb = xf[tok_b]
        return swiglu(xb, w_gate[e_b], w_up[e_b], w_down[e_b])

    y = lax.map(expert_block, (tok_buf.reshape(n_blk, EXPERT_BLOCK), blk_e)).reshape(P, D)
    y = y * w_buf[:, None].astype(y.dtype)
    out = jax.ops.segment_sum(y, tok_buf, num_segments=T)
    return out.reshape(B, S, D)


def setup_inputs(seed: int = 0) -> dict:
    key = jax.random.key(seed)
    ks = jax.random.split(key, 24)
    f32 = jnp.float32
    nrm = lambda k, shape, s: jax.random.normal(k, shape, f32) * s
    x = jax.random.normal(ks[0], (BATCH, SEQ, D_MODEL), f32)
    positions = jnp.broadcast_to(jnp.arange(SEQ, dtype=jnp.int32), (BATCH, SEQ))
    w_in = nrm(ks[1], (DEPTH, D_MODEL, D_IN_PROJ), D_MODEL ** -0.5)
    conv_w = nrm(ks[2], (DEPTH, CONV_WIDTH, GDN_QKV_COLS), 0.5)
    a_log = jnp.log(jax.random.uniform(ks[3], (DEPTH, N_GDN_HEADS), f32, 1.0, 16.0))
    dt = jnp.exp(jax.random.uniform(ks[4], (DEPTH, N_GDN_HEADS), f32, math.log(1e-3), math.log(1e-1)))
    dt_bias = dt + jnp.log(-jnp.expm1(-dt))
    gdn_norm_w = 1.0 + nrm(ks[5], (DEPTH, GDN_HEAD_DIM), 0.02)
    lam_q1 = nrm(ks[6], (DEPTH, DA_HEAD_DIM), 0.1)
    lam_k1 = nrm(ks[7], (DEPTH, DA_HEAD_DIM), 0.1)
    lam_q2 = nrm(ks[8], (DEPTH, DA_HEAD_DIM), 0.1)
    lam_k2 = nrm(ks[9], (DEPTH, DA_HEAD_DIM), 0.1)
    subln_w = 1.0 + nrm(ks[10], (DEPTH, DA_V_DIM), 0.02)
    w_out = nrm(ks[11], (DEPTH, MIX_WIDTH, D_MODEL), MIX_WIDTH ** -0.5 * DEEPNORM_BETA)
    ln1_g = 1.0 + nrm(ks[12], (DEPTH, D_MODEL), 0.02)
    ln1_b = nrm(ks[13], (DEPTH, D_MODEL), 0.02)
    ln2_g = 1.0 + nrm(ks[14], (DEPTH, D_MODEL), 0.02)
    ln2_b = nrm(ks[15], (DEPTH, D_MODEL), 0.02)
    ffn_w_gate = nrm(ks[16], (N_DENSE, D_MODEL, D_FF_DENSE), D_MODEL ** -0.5)
    ffn_w_up = nrm(ks[17], (N_DENSE, D_MODEL, D_FF_DENSE), D_MODEL ** -0.5)
    ffn_w_down = nrm(ks[18], (N_DENSE, D_FF_DENSE, D_MODEL), D_FF_DENSE ** -0.5 * DEEPNORM_BETA)
    router_w = nrm(ks[19], (N_MOE, D_MODEL, N_EXPERTS), D_MODEL ** -0.5)
    moe_w_gate = nrm(ks[20], (N_MOE, N_EXPERTS, D_MODEL, D_FF_EXPERT), D_MODEL ** -0.5)
    moe_w_up = nrm(ks[21], (N_MOE, N_EXPERTS, D_MODEL, D_FF_EXPERT), D_MODEL ** -0.5)
    moe_w_down = nrm(ks[22], (N_MOE, N_EXPERTS, D_FF_EXPERT, D_MODEL), D_FF_EXPERT ** -0.5 * DEEPNORM_BETA)
    return {"x": x, "positions": positions, "w_in": w_in, "conv_w": conv_w, "a_log": a_log,
            "dt_bias": dt_bias, "gdn_norm_w": gdn_norm_w, "lam_q1": lam_q1, "lam_k1": lam_k1,
            "lam_q2": lam_q2, "lam_k2": lam_k2, "subln_w": subln_w, "w_out": w_out,
            "ln1_g": ln1_g, "ln1_b": ln1_b, "ln2_g": ln2_g, "ln2_b": ln2_b,
            "ffn_w_gate": ffn_w_gate, "ffn_w_up": ffn_w_up, "ffn_w_down": ffn_w_down,
            "router_w": router_w, "moe_w_gate": moe_w_gate, "moe_w_up": moe_w_up, "moe_w_down": moe_w_down}


def reference(x, positions, w_in, conv_w, a_log, dt_bias, gdn_norm_w, lam_q1, lam_k1, lam_q2, lam_k2,
              subln_w, w_out, ln1_g, ln1_b, ln2_g, ln2_b, ffn_w_gate, ffn_w_up, ffn_w_down,
              router_w, moe_w_gate, moe_w_up, moe_w_down):
    inv_freq = ROPE_THETA ** (-jnp.arange(0, DA_HEAD_DIM, 2, dtype=jnp.float32) / DA_HEAD_DIM)
    ang = positions.astype(jnp.float32)[..., None] * inv_freq
    ang = jnp.concatenate([ang, ang], axis=-1)
    cos = jnp.cos(ang)[:, :, None, None, :].astype(x.dtype)
    sin = jnp.sin(ang)[:, :, None, None, :].astype(x.dtype)
    for l in range(DEPTH):
        lam_init = 0.8 - 0.6 * math.exp(-0.3 * l)
        h = hybrid_mixer(x, w_in[l], conv_w[l], a_log[l], dt_bias[l], gdn_norm_w[l], lam_q1[l], lam_k1[l],
                         lam_q2[l], lam_k2[l], subln_w[l], w_out[l], cos, sin, lam_init)
        x = layer_norm(DEEPNORM_ALPHA * x + h, ln1_g[l], ln1_b[l])
        if l % 2 == 0:
            f = swiglu(x, ffn_w_gate[l // 2], ffn_w_up[l // 2], ffn_w_down[l // 2])
        else:
            f = moe_ffn(x, router_w[l // 2], moe_w_gate[l // 2], moe_w_up[l // 2], moe_w_down[l // 2])
        x = layer_norm(DEEPNORM_ALPHA * x + f, ln2_g[l], ln2_b[l])
    return x
```

```python
import numpy as np
from contextlib import ExitStack
import concourse.bass as bass
import concourse.mybir as mybir
from concourse.bass_utils import run_bass_kernel_spmd

F32 = mybir.dt.float32
F32R = mybir.dt.float32r
BF16 = mybir.dt.bfloat16
I32 = mybir.dt.int32
AF = mybir.ActivationFunctionType
ALU = mybir.AluOpType
AX = mybir.AxisListType

EPOCH = 16384
USE_F32R = True


class Res:
    __slots__ = ("w", "rs", "name")

    def __init__(self, name=""):
        self.w = None
        self.rs = []
        self.name = name


class Prog:
    ENGS = ("pe", "act", "dve", "pool", "sp")

    def __init__(self, nc, stack, n_dma_sems=32):
        self.nc = nc
        self.stack = stack
        self.streams = {e: [] for e in self.ENGS}
        self.cnt = {e: 0 for e in self.ENGS}
        self.esems = {e: [] for e in self.ENGS}
        self.seen = {e: {} for e in self.ENGS}
        self.dsems = [stack.enter_context(nc.semaphore(f"dq{i}")) for i in range(n_dma_sems)]
        self.duse = [0] * n_dma_sems
        half = n_dma_sems // 2
        self.dpool = {"sp": list(range(0, half)), "pool": list(range(half, n_dma_sems))}
        self.dnext = {"sp": 0, "pool": 0}
        self.out_events = []

    def _esem(self, eng, epoch):
        lst = self.esems[eng]
        while len(lst) <= epoch:
            lst.append(self.stack.enter_context(self.nc.semaphore(f"c_{eng}_{len(lst)}")))
        return lst[epoch]

    def _collect(self, eng, reads, writes, is_dma):
        need = {}

        def add(ev, hazard):
            if ev is None:
                return
            if ev[0] == "c":
                if ev[1] == eng and not is_dma and hazard != "raw" and eng == "pe":
                    return
                key = ("c", ev[1])
            else:
                key = ("d", ev[1])
            if need.get(key, 0) < ev[2]:
                need[key] = ev[2]

        for r in reads:
            add(r.w, "raw")
        for w in writes:
            add(w.w, "waw")
            for ev in w.rs:
                add(ev, "war")
        waits = []
        seen = self.seen[eng]
        for key, val in need.items():
            if seen.get(key, 0) >= val:
                continue
            seen[key] = val
            if key[0] == "c":
                ep, v = divmod(val - 1, EPOCH)
                waits.append((self._esem(key[1], ep), v + 1))
            else:
                waits.append((self.dsems[key[1]], val))
        return waits

    def _commit(self, ev, reads, writes):
        for r in reads:
            r.rs.append(ev)
        for w in writes:
            w.w = ev
            w.rs = []

    def op(self, eng, fn, reads=(), writes=()):
        waits = self._collect(eng, reads, writes, False)
        self.cnt[eng] += 1
        g = self.cnt[eng]
        ep, v = divmod(g - 1, EPOCH)
        self.streams[eng].append((fn, waits, (self._esem(eng, ep), 1)))
        ev = ("c", eng, g)
        self._commit(ev, reads, writes)
        return ev

    def dma(self, out, in_, reads=(), writes=(), eng="sp", is_output=False, **kw):
        lst = self.dpool[eng]
        j = lst[self.dnext[eng] % len(lst)]
        self.dnext[eng] += 1
        waits = self._collect(eng, reads, writes, True)
        prev = self.duse[j] * 16
        seen = self.seen[eng]
        if prev and seen.get(("d", j), 0) < prev:
            seen[("d", j)] = prev
            waits.append((self.dsems[j], prev))
        self.duse[j] += 1
        val = self.duse[j] * 16
        fn = lambda e, out=out, in_=in_, kw=kw: e.dma_start(out=out, in_=in_, **kw)
        self.streams[eng].append((fn, waits, (self.dsems[j], 16)))
        ev = ("d", j, val)
        self._commit(ev, reads, writes)
        if is_output:
            self.out_events.append(ev)
        return ev

    def finish(self):
        need = {}
        for ev in self.out_events:
            need[ev[1]] = max(need.get(ev[1], 0), ev[2])
        fin = [(self.dsems[j], v) for j, v in need.items()]
        nc = self.nc
        streams = self.streams
        with nc.Block() as block:
            def emit(e, lst, final=()):
                for fn, waits, inc in lst:
                    for s, v in waits:
                        e.wait_ge(s, v)
                    fn(e).then_inc(inc[0], inc[1])
                for s, v in final:
                    e.wait_ge(s, v)

            @block.tensor
            def _(e):
                emit(e, streams["pe"])

            @block.scalar
            def _(e):
                emit(e, streams["act"])

            @block.vector
            def _(e):
                emit(e, streams["dve"])

            @block.gpsimd
            def _(e):
                emit(e, streams["pool"])

            @block.sync
            def _(e):
                emit(e, streams["sp"], fin)


SEQ = 2048
DM = 1024
NT = 16
NKC = 8
DEPTH = 4
D_IN = 3592
FF_DENSE = 2816
FF_MOE = 3584
NEXP = 8
ALPHA = (2.0 * DEPTH) ** 0.25
LN_EPS = 1e-5
RMS_EPS = 1e-6
TWO_PI = 6.283185307179586
ARENA_W = 22528
MT_OFF = 0


def _dsize(dt):
    return 4 if dt in (F32, I32) else 2


class Arena:
    def __init__(self, t):
        self.t = t
        self.live = []
        self.frozen = []

    @staticmethod
    def _compress(res_list):
        d = {}
        for r in res_list:
            for ev in ([r.w] if r.w else []) + list(r.rs):
                key = (ev[0], ev[1])
                if key not in d or d[key][2] < ev[2]:
                    d[key] = ev
        return d

    def carve(self, lo, shape, dt, nres=1):
        n = 1
        for s in shape:
            n *= s
        nbytes = n * _dsize(dt)
        hi = lo + nbytes
        assert lo % 4 == 0 and hi <= ARENA_W * 4, (lo, hi)
        evs = {}
        keep = []
        for (l2, h2, rl) in self.live:
            if l2 < hi and lo < h2:
                d = self._compress(rl)
                self.frozen.append((l2, h2, d))
            else:
                keep.append((l2, h2, rl))
        newf = []
        for (l2, h2, d) in self.frozen:
            if l2 < hi and lo < h2:
                for key, ev in d.items():
                    if key not in evs or evs[key][2] < ev[2]:
                        evs[key] = ev
            if not (lo <= l2 and h2 <= hi):
                newf.append((l2, h2, d))
        self.frozen = newf
        res = [Res() for _ in range(nres)]
        for r in res:
            r.rs = list(evs.values())
        self.live = keep + [(lo, hi, res)]
        ap = self.t[:, lo // 4:(hi + 3) // 4]
        if dt != F32:
            ap = ap.bitcast(dt)
        if len(shape) == 2:
            ap = ap.rearrange("p (a b) -> p a b", a=shape[0])
        elif len(shape) == 3:
            ap = ap.rearrange("p (a b c) -> p a b c", a=shape[0], b=shape[1])
        return ap, res


class Kern:
    def __init__(self, nc, P, st, n_layers=DEPTH, upto=None):
        self.nc, self.P, self.st = nc, P, st
        self.n_layers = n_layers
        self.upto = upto
        dr = lambda name, shape, dt=F32, kind="ExternalInput": nc.dram_tensor(name, shape, dt, kind=kind).ap()
        self.d = {}
        self.d["x"] = dr("x", [SEQ, DM])
        self.d["pos"] = dr("pos", [1, SEQ], I32)
        self.d["cf"] = dr("cf", [128, 648])
        self.d["w_in"] = dr("w_in", [DEPTH, DM, D_IN])
        self.d["conv_w"] = dr("conv_w", [DEPTH, 4, 1536])
        self.d["a_log"] = dr("a_log", [DEPTH, 4])
        self.d["dt_bias"] = dr("dt_bias", [DEPTH, 4])
        self.d["gdn_norm_w"] = dr("gdn_norm_w", [DEPTH, 128])
        for k in ("lam_q1", "lam_k1", "lam_q2", "lam_k2"):
            self.d[k] = dr(k, [DEPTH, 64])
        self.d["subln_w"] = dr("subln_w", [DEPTH, 128])
        self.d["w_out"] = dr("w_out", [DEPTH, DM, DM])
        for k in ("ln1_g", "ln1_b", "ln2_g", "ln2_b"):
            self.d[k] = dr(k, [DEPTH, DM])
        self.d["ffn_w_gate"] = dr("ffn_w_gate", [2, DM, FF_DENSE])
        self.d["ffn_w_up"] = dr("ffn_w_up", [2, DM, FF_DENSE])
        self.d["ffn_w_down"] = dr("ffn_w_down", [2, FF_DENSE, DM])
        self.d["router_w"] = dr("router_w", [2, DM, NEXP])
        self.d["moe_w_gate"] = dr("moe_w_gate", [2, NEXP, DM, FF_MOE])
        self.d["moe_w_up"] = dr("moe_w_up", [2, NEXP, DM, FF_MOE])
        self.d["moe_w_down"] = dr("moe_w_down", [2, NEXP, FF_MOE, DM])
        self.d["y"] = dr("y", [SEQ, DM], F32, "ExternalOutput")
        if upto is not None:
            self.d["dbg"] = dr("dbg", [SEQ, DM], F32, "ExternalOutput")

        sb = lambda name, shape, dt: st.enter_context(nc.sbuf_tensor(name, shape, dt))
        self.X = sb("X", [128, NT, DM], F32)
        self.RX = [Res(f"X{t}") for t in range(NT)]
        self.XT = sb("XT", [128, NKC, SEQ], BF16)
        self.RXT = [Res(f"XT{t}") for t in range(NT)]
        self.CF = sb("CF", [128, 648], F32)
        self.RCF = Res("CF")
        self.CB = sb("CB", [128, 5, 128], BF16)
        self.RCB = Res("CB")
        self.cosT = sb("cosT", [128, SEQ], BF16)
        self.sinT = sb("sinT", [128, SEQ], BF16)
        self.RTAB = Res("tab")
        self.SM = sb("SM", [128, 1024], F32)
        self.Rsm = Res("sm")
        self.AR = Arena(sb("AR", [128, ARENA_W], F32))
        self.RR = sb("RR", [128, 16, 128], F32)
        self.RRr = [Res(f"rr{i}") for i in range(16)]
        self.PSB = [st.enter_context(nc.psum_tensor(f"ps{b}", [128, 512], F32)) for b in range(8)]
        self.RPS = [Res(f"ps{b}") for b in range(8)]
        self.pools = {}
        self.identf = self.CF[:, 0:128]
        self.Uf = self.CF[:, 128:256]
        self.Usf = self.CF[:, 256:384]
        self.onesf = self.CF[:, 384:512]
        self.invf = self.CF[:, 640:641]
        self.identb = self.CB[:, 0, :]
        self.Ub = self.CB[:, 1, :]
        self.onesb = self.CB[:, 2, :]
        self.Rmb = self.CB[:, 3, :]

    def bank(self, pool, banks=None):
        if pool not in self.pools:
            self.pools[pool] = [banks, 0]
        lst, c = self.pools[pool]
        self.pools[pool][1] += 1
        b = lst[c % len(lst)]
        return b

    def psf(self, b):
        return self.PSB[b][:]

    def psb16(self, b):
        return self.PSB[b][:].bitcast(BF16)

    def mm(self, out, lhsT, rhs, start, stop, reads, writes):
        self.P.op("pe", lambda e: e.matmul(out, lhsT=lhsT, rhs=rhs, start=start, stop=stop), reads, writes)

    def mmr(self, out, lhsT, rhs, start, stop, reads, writes):
        if not USE_F32R:
            return self.mm(out, lhsT, rhs, start, stop, reads, writes)
        a, b = lhsT.bitcast(F32R), rhs.bitcast(F32R)
        self.P.op("pe", lambda e: e.matmul(out, lhsT=a, rhs=b, start=start, stop=stop), reads, writes)

    def tr(self, out, in_, ident, reads, writes):
        self.P.op("pe", lambda e: e.transpose(out, in_, ident), reads, writes)

    def act(self, out, in_, func, reads, writes, **kw):
        self.P.op("act", lambda e: e.activation(out=out, in_=in_, func=func, **kw), reads, writes)

    def tt(self, eng, out, in0, in1, op, reads, writes):
        self.P.op(eng, lambda e: e.tensor_tensor(out=out, in0=in0, in1=in1, op=op), reads, writes)

    def ts(self, eng, out, in0, s1, s2, op0, op1, reads, writes, **kw):
        if op1 is None:
            self.P.op(eng, lambda e: e.tensor_scalar(out=out, in0=in0, scalar1=s1, scalar2=None, op0=op0, **kw), reads, writes)
        else:
            self.P.op(eng, lambda e: e.tensor_scalar(out=out, in0=in0, scalar1=s1, scalar2=s2, op0=op0, op1=op1, **kw), reads, writes)

    def stt(self, eng, out, in0, scalar, in1, op0, op1, reads, writes, **kw):
        self.P.op(eng, lambda e: e.scalar_tensor_tensor(out=out, in0=in0, scalar=scalar, in1=in1, op0=op0, op1=op1, **kw), reads, writes)

    def mmr2(self, out, lhsT, rhs, start, stop, reads, writes):
        return self.mmr(out, lhsT, rhs, start, stop, reads, writes)

    def cp(self, eng, out, in_, reads, writes):
        if eng == "act":
            self.P.op("act", lambda e: e.activation(out=out, in_=in_, func=AF.Copy), reads, writes)
        else:
            self.P.op(eng, lambda e: e.tensor_copy(out=out, in_=in_), reads, writes)

    def memset(self, eng, ap, val, writes):
        self.P.op(eng, lambda e: e.memset(ap, val), (), writes)

    def prologue(self):
        P, d = self.P, self.d
        P.dma(self.CF[:], d["cf"], writes=[self.RCF])
        for i, lo in enumerate([0, 128, 384, 512]):
            P.dma(self.CB[:, i, :], d["cf"][:, lo:lo + 128], writes=[self.RCB], eng="pool")
        xr = d["x"].rearrange("(t p) d -> p t d", p=128)
        for t in range(NT):
            P.dma(self.X[:, t, :], xr[:, t, :], writes=[self.RX[t]])
        import os
        skip = os.environ.get("PRO_SKIP", "")
        if "rope" in skip:
            if "xt" not in skip:
                self.make_xt(range(NT))
            return
        A = self.AR
        base = 32768
        posi, (Rp,) = A.carve(base, [SEQ], I32)
        r, (Rr,) = A.carve(base + 8192, [SEQ], F32)
        r2, (Rr2,) = A.carve(base + 16384, [SEQ], F32)
        ki, (Rki,) = A.carve(base + 24576, [SEQ], I32)
        kf, (Rkf,) = A.carve(base + 32768, [SEQ], F32)
        mk, (Rmk,) = A.carve(base + 40960, [SEQ], F32)
        P.dma(posi, d["pos"].broadcast_to([128, SEQ]), writes=[Rp])
        if "s1" in skip:
            self.make_xt(range(NT)); return
        self.cp("dve", r, posi, [Rp], [Rr])
        self.ts("dve", r, r, self.invf, None, ALU.mult, None, [Rr, self.RCF], [Rr])
        if "s2" in skip:
            self.make_xt(range(NT)); return
        for tab, shift in ((self.sinT, 0.0), (self.cosT, 0.25)):
            self.ts("dve", r2, r, shift, None, ALU.add, None, [Rr], [Rr2])
            self.cp("dve", ki, r2, [Rr2], [Rki])
            self.cp("dve", kf, ki, [Rki], [Rkf])
            self.tt("dve", r2, r2, kf, ALU.subtract, [Rr2, Rkf], [Rr2])
            self.ts("dve", mk, r2, 0.5, None, ALU.is_gt, None, [Rr2], [Rmk])
            self.tt("dve", r2, r2, mk, ALU.subtract, [Rr2, Rmk], [Rr2])
            self.ts("dve", mk, r2, -0.5, None, ALU.is_lt, None, [Rr2], [Rmk])
            self.tt("dve", r2, r2, mk, ALU.add, [Rr2, Rmk], [Rr2])
            if "s3" in skip:
                if "dumpr2" in skip:
                    k0 = 0 if shift == 0.0 else 2
                    self.cp("dve", self.X[:, k0, :], r2[:, 0:1024], [Rr2], [self.RX[k0]])
                    self.cp("dve", self.X[:, k0 + 1, :], r2[:, 1024:2048], [Rr2], [self.RX[k0 + 1]])
                continue
            self.act(tab[:], r2, AF.Sin, [Rr2], [self.RTAB], scale=TWO_PI * (1.0 - 1e-6))
        self.make_xt(range(NT))

    def make_xt(self, tiles):
        A = self.AR
        xb2, Rxb = A.carve(32768 + 49152, [2, DM], BF16, nres=2)
        for t in tiles:
            s = t % 2
            self.cp("act", xb2[:, s, :], self.X[:, t, :], [self.RX[t]], [Rxb[s]])
            b = self.bank("tp", [6, 7])
            pv = self.psb16(b)
            for kc in range(NKC):
                self.tr(pv[:, kc * 128:(kc + 1) * 128], xb2[:, s, kc * 128:(kc + 1) * 128], self.identb,
                        [Rxb[s], self.RCB], [self.RPS[b]])
            self.cp("dve", self.XT[:, :, t * 128:(t + 1) * 128], pv.rearrange("p (c n) -> p c n", c=NKC),
                    [], [self.RPS[b], self.RXT[t]])

    def dump_featmajor_bf16(self, ap3, res_list):
        C = ap3.shape[1]
        dst = self.d["dbg"].rearrange("(a b) d -> a (b d)", b=2)
        dst = dst.rearrange("(c p) t -> p c t", p=128)
        self.P.dma(dst[:, 0:C, :], ap3, reads=res_list, eng="pool", is_output=True)

    def dump_X(self, name="dbg"):
        yr = self.d[name].rearrange("(t p) d -> p t d", p=128)
        for t in range(NT):
            self.P.dma(yr[:, t, :], self.X[:, t, :], reads=[self.RX[t]], is_output=True)

    def layer_params(self, l):
        P, d, SM = self.P, self.d, self.SM
        Rsm = self.Rsm
        lam_init = 0.8 - 0.6 * float(np.exp(-0.3 * l))
        self.lam_init = lam_init
        for i, k in enumerate(("lam_q1", "lam_k1", "lam_q2", "lam_k2")):
            P.dma(SM[:, i * 64:(i + 1) * 64], d[k][l:l + 1, :].broadcast_to([128, 64]), writes=[Rsm])
        P.dma(SM[:, 256:384], d["subln_w"][l:l + 1, :].broadcast_to([128, 128]), writes=[Rsm])
        P.dma(SM[:, 384:512], d["gdn_norm_w"][l:l + 1, :].broadcast_to([128, 128]), writes=[Rsm])
        P.dma(SM[:, 512:516], d["dt_bias"][l:l + 1, :].broadcast_to([128, 4]), writes=[Rsm])
        P.dma(SM[:, 516:520], d["a_log"][l:l + 1, :].broadcast_to([128, 4]), writes=[Rsm])
        self.stt("dve", SM[:, 960:1024], SM[:, 0:64], 1.0, SM[:, 64:128], ALU.mult, ALU.mult, [Rsm], [Rsm], accum_out=SM[:, 520:521])
        self.stt("dve", SM[:, 960:1024], SM[:, 128:192], 1.0, SM[:, 192:256], ALU.mult, ALU.mult, [Rsm], [Rsm], accum_out=SM[:, 521:522])
        self.act(SM[:, 522:524], SM[:, 520:522], AF.Exp, [Rsm], [Rsm])
        self.tt("dve", SM[:, 520:521], SM[:, 522:523], SM[:, 523:524], ALU.subtract, [Rsm], [Rsm])
        self.ts("dve", SM[:, 524:525], SM[:, 520:521], -1.0, -lam_init, ALU.mult, ALU.add, [Rsm], [Rsm])
        self.neglam = SM[:, 524:525]
        self.ts("dve", SM[:, 256:384], SM[:, 256:384], 1.0 - lam_init, None, ALU.mult, None, [Rsm], [Rsm])
        self.WSUB = SM[:, 256:384]
        self.GNW = SM[:, 384:512]
        self.act(SM[:, 516:520], SM[:, 516:520], AF.Exp, [Rsm], [Rsm])
        self.ts("dve", SM[:, 516:520], SM[:, 516:520], -1.0, None, ALU.mult, None, [Rsm], [Rsm])

    def layer_norm_all(self, gname, bname, l, lnoff, need_xt=True):
        P, d, A = self.P, self.d, self.AR
        LNG, (Rg,) = A.carve(lnoff, [DM], F32)
        LNB, (Rb,) = A.carve(lnoff + 4096, [DM], F32)
        junk, (Rj,) = A.carve(lnoff + 8192, [DM], F32)
        ST, (Rs,) = A.carve(lnoff + 12288, [128], F32)
        junk2, (Rj2,) = A.carve(lnoff + 12800, [DM], F32)
        P.dma(LNG, d[gname][l:l + 1, :].broadcast_to([128, DM]), writes=[Rg])
        P.dma(LNB, d[bname][l:l + 1, :].broadcast_to([128, DM]), writes=[Rb])
        X, RX = self.X, self.RX
        for t in range(NT):
            self.act(junk, X[:, t, :], AF.Identity, [RX[t]], [Rj, Rs], accum_out=ST[:, t:t + 1])
            self.stt("dve", junk2, X[:, t, :], 1.0, X[:, t, :], ALU.mult, ALU.mult, [RX[t]], [Rj2, Rs], accum_out=ST[:, 16 + t:17 + t])
        self.ts("dve", ST[:, 0:32], ST[:, 0:32], 1.0 / DM, None, ALU.mult, None, [Rs], [Rs])
        self.tt("dve", ST[:, 32:48], ST[:, 0:16], ST[:, 0:16], ALU.mult, [Rs], [Rs])
        self.tt("dve", ST[:, 48:64], ST[:, 16:32], ST[:, 32:48], ALU.subtract, [Rs], [Rs])
        self.act(ST[:, 64:80], ST[:, 48:64], AF.Ln, [Rs], [Rs], bias=LN_EPS)
        self.act(ST[:, 80:96], ST[:, 64:80], AF.Exp, [Rs], [Rs], scale=-0.5)
        self.stt("dve", ST[:, 96:112], ST[:, 0:16], -1.0, ST[:, 80:96], ALU.mult, ALU.mult, [Rs], [Rs])
        for t in range(NT):
            self.act(X[:, t, :], X[:, t, :], AF.Identity, [RX[t], Rs], [RX[t]], scale=ST[:, 80 + t:81 + t], bias=ST[:, 96 + t:97 + t])
            self.tt("dve", X[:, t, :], X[:, t, :], LNG, ALU.mult, [RX[t], Rg], [RX[t]])
            self.tt("pool", X[:, t, :], X[:, t, :], LNB, ALU.add, [RX[t], Rb], [RX[t]])
        if need_xt:
            self.make_xt(range(NT))

    def da_head(self, l, h):
        P, d, A = self.P, self.d, self.AR
        XT, RXT = self.XT, self.RXT
        base = 32768
        W2, RW2 = A.carve(base, [2, NKC, 384], BF16, nres=2) if h == 0 else (self._daW, self._daRW)
        self._daW, self._daRW = W2, RW2
        W, RW = W2[:, h % 2], RW2[h % 2]
        o = base + 12288
        qk, Rqk = A.carve(o, [2, SEQ], BF16, nres=2); o += 8192
        V, (RV,) = A.carve(o, [NT, 132], BF16); o += 4224
        rawb, Rraw = A.carve(o, [2, 512], BF16, nres=2); o += 2048
        t1, Rt1 = A.carve(o, [2, 512], F32, nres=2); o += 4096
        t2, Rt2 = A.carve(o, [2, 512], F32, nres=2); o += 4096
        sq, Rsq = A.carve(o, [2, 512], BF16, nres=2); o += 2048
        ET, RET = A.carve(o, [4, 256], BF16, nres=4); o += 2048
        ep_t, Rept = A.carve(o, [2, 128], F32, nres=2); o += 1024
        ep_o, Repo = A.carve(o, [2, 128], F32, nres=2); o += 1024
        ep_j, (Repj,) = A.carve(o, [128], F32); o += 512
        ep_n, Repn = A.carve(o, [2, 128], BF16, nres=2); o += 512
        st, (Rst, RnegM, Rst_a, Rst_b) = A.carve(o, [64], F32, nres=4); o += 256
        Rst2 = [Rst_a, Rst_b]
        U2, (RU2,) = A.carve(o, [2, 128], BF16); o += 512
        kz, (Rkz,) = A.carve(o, [2, SEQ], BF16); o += 8192
        self.memset("pool", kz[64:128, 0, :], 0.0, [Rkz])
        self.memset("pool", kz[0:64, 1, :], 0.0, [Rkz])
        win = d["w_in"][l].rearrange("(c p) n -> p c n", p=128)
        for i, c0 in enumerate((h * 128, 512 + h * 128, 1024 + h * 128)):
            P.dma(W[:, :, i * 128:(i + 1) * 128], win[:, :, c0:c0 + 128], writes=[RW], eng="pool")
        self.cp("pool", U2[:, 0, :], self.Ub, [self.RCB], [RU2])
        self.cp("pool", U2[:, 1, :], self.Ub, [self.RCB], [RU2])
        cnt = 0
        for which in range(2):
            for tg in range(4):
                s = cnt % 2
                cnt += 1
                cols = slice(tg * 512, (tg + 1) * 512)
                b = self.bank("proj", [0, 1])
                ps = self.psf(b)
                for kc in range(NKC):
                    self.mm(ps, W[:, kc, which * 128:(which + 1) * 128], XT[:, kc, cols], kc == 0, kc == NKC - 1,
                            [RW] + RXT[4 * tg:4 * tg + 4], [self.RPS[b]])
                self.cp("act", rawb[:, s, :], ps, [], [self.RPS[b], Rraw[s]])
                self.tt("dve", t1[:, s, :], ps, self.cosT[:, cols], ALU.mult, [self.RTAB], [self.RPS[b], Rt1[s]])
                b2 = self.bank("rot", [2, 3])
                ps2 = self.psf(b2)
                self.mm(ps2, self.Rmb, rawb[:, s, :], True, True, [self.RCB, Rraw[s]], [self.RPS[b2]])
                self.tt("dve", t2[:, s, :], ps2, self.sinT[:, cols], ALU.mult, [self.RTAB], [self.RPS[b2], Rt2[s]])
                self.tt("dve", qk[:, which, cols], t1[:, s, :], t2[:, s, :], ALU.add, [Rt1[s], Rt2[s]], [Rqk[which]])
                self.act(sq[:, s, :], qk[:, which, cols], AF.Square, [Rqk[which]], [Rsq[s]])
                if which == 1:
                    self.cp("act", kz[0:64, 0, cols], qk[0:64, 1, cols], [Rqk[1]], [Rkz])
                    self.cp("pool", kz[64:128, 1, cols], qk[64:128, 1, cols], [Rqk[1]], [Rkz])
                b3 = self.bank("nrm", [4, 5])
                ps3 = self.psf(b3)
                self.mm(ps3, self.onesb, sq[:, s, :], True, True, [self.RCB, Rsq[s]], [self.RPS[b3]])
                self.P.op("dve", lambda e, o_=st[:, which * 4 + tg:which * 4 + tg + 1], i_=ps3: e.reduce_max(out=o_, in_=i_, axis=AX.X),
                          [], [self.RPS[b3], Rst])
        self.P.op("dve", lambda e: e.reduce_max(out=st[:, 8:9], in_=st[:, 0:4], axis=AX.X), [Rst], [Rst])
        self.P.op("dve", lambda e: e.reduce_max(out=st[:, 9:10], in_=st[:, 4:8], axis=AX.X), [Rst], [Rst])
        self.tt("dve", st[:, 10:11], st[:, 8:9], st[:, 9:10], ALU.mult, [Rst], [Rst])
        self.act(st[:, 11:12], st[:, 10:11], AF.Ln, [Rst], [Rst], bias=1e-30)
        self.act(st[:, 12:13], st[:, 11:12], AF.Exp, [Rst], [Rst], scale=0.5)
        self.ts("dve", st[:, 13:14], st[:, 12:13], -1.05 / 8.0, None, ALU.mult, None, [Rst], [RnegM])
        negM = st[:, 13:14]
        self.memset("pool", V[:, :, 128:129], 1.0, [RV])
        VT, RVT = A.carve(o, [4, 512], BF16, nres=4); o += 4096
        for tg in range(4):
            b = self.bank("proj", [0, 1])
            ps = self.psf(b)
            for kc in range(NKC):
                self.mm(ps, W[:, kc, 256:384], XT[:, kc, tg * 512:(tg + 1) * 512], kc == 0, kc == NKC - 1,
                        [RW] + RXT[4 * tg:4 * tg + 4], [self.RPS[b]])
            self.cp("act", VT[:, tg, :], ps, [], [self.RPS[b], RVT[tg]])
            b2 = self.bank("rot", [2, 3])
            pv = self.psb16(b2)
            for ti in range(4):
                self.tr(pv[:, ti * 128:(ti + 1) * 128], VT[:, tg, ti * 128:(ti + 1) * 128], self.identb, [RVT[tg], self.RCB], [self.RPS[b2]])
            self.cp("dve", V[:, 4 * tg:4 * tg + 4, 0:128], pv[:, 0:512].rearrange("p (a n) -> p a n", a=4), [], [self.RPS[b2], RV])
        qT, kT = qk[:, 0, :], qk[:, 1, :]
        pairs = [(i, j) for i in range(NT) for j in range(i + 1)]
        info = {}

        def emit_st(n):
            i, j = pairs[n]
            b = self.bank("st", [0, 1, 6])
            ps = self.psf(b)
            for c in range(2):
                self.mm(ps[:, c * 128:(c + 1) * 128], kz[:, c, j * 128:(j + 1) * 128],
                        qT[:, i * 128:(i + 1) * 128], True, True, [Rqk[0], Rkz], [self.RPS[b]])
            e = n % 4
            self.act(ET[:, e, :], ps[:, 0:256], AF.Exp, [RnegM], [self.RPS[b], RET[e]], scale=0.125, bias=negM)
            if i == j:
                self.tt("pool", ET[:, e, :], ET[:, e, :], U2.rearrange("p a n -> p (a n)"), ALU.mult, [RET[e], RU2], [RET[e]])

        def emit_pv(n):
            i, j = pairs[n]
            e = n % 4
            for c in range(2):
                b = [2, 3, 4, 5][2 * c + (i % 2)]
                self.mm(self.psf(b)[:, 0:129], ET[:, e, c * 128:(c + 1) * 128], V[:, j, 0:129], j == 0, j == i,
                        [RET[e], RV], [self.RPS[b]])

        def epilogue(i):
            s = i % 2
            b0, b1 = [2, 3][s], [4, 5][s]
            O0, O1 = self.psf(b0), self.psf(b1)
            Rst = Rst2[s]
            c = 16 + 4 * s
            self.P.op("dve", lambda e: e.reciprocal(out=st[:, c:c + 1], in_=O0[:, 128:129]), [], [self.RPS[b0], Rst])
            self.P.op("dve", lambda e: e.reciprocal(out=st[:, c + 1:c + 2], in_=O1[:, 128:129]), [], [self.RPS[b1], Rst])
            self.tt("dve", st[:, c + 2:c + 3], st[:, c + 1:c + 2], self.neglam, ALU.mult, [Rst, self.Rsm], [Rst])
            self.ts("dve", ep_t[:, s, :], O1[:, 0:128], st[:, c + 2:c + 3], None, ALU.mult, None, [Rst], [self.RPS[b1], Rept[s]])
            self.stt("dve", ep_o[:, s, :], O0[:, 0:128], st[:, c:c + 1], ep_t[:, s, :], ALU.mult, ALU.add, [Rst, Rept[s]], [self.RPS[b0], Repo[s]])
            self.stt("dve", ep_j, ep_o[:, s, :], 1.0, ep_o[:, s, :], ALU.mult, ALU.mult, [Repo[s]], [Repj, Rst], accum_out=st[:, c + 3:c + 4])
            self.act(st[:, 24 + s:25 + s], st[:, c + 3:c + 4], AF.Ln, [Rst], [Rst], scale=1.0 / 128.0, bias=RMS_EPS)
            self.act(st[:, 26 + s:27 + s], st[:, 24 + s:25 + s], AF.Exp, [Rst], [Rst], scale=-0.5)
            self.stt("dve", ep_n[:, s, :], ep_o[:, s, :], st[:, 26 + s:27 + s], self.WSUB, ALU.mult, ALU.mult, [Repo[s], Rst, self.Rsm], [Repn[s]])
            bt = 7
            pv = self.psb16(bt)
            self.tr(pv[:, 0:128], ep_n[:, s, :], self.identb, [Repn[s], self.RCB], [self.RPS[bt]])
            self.cp("dve", self.MT[:, h, i * 128:(i + 1) * 128], pv[:, 0:128], [], [self.RPS[bt], self.RMT[h][i]])

        emit_st(0)
        emit_st(1)
        pending = []
        for n in range(len(pairs)):
            if n + 2 < len(pairs):
                emit_st(n + 2)
            i, j = pairs[n]
            while pending and pending[0][1] <= i - 2:
                epilogue(pending.pop(0)[1])
            emit_pv(n)
            if i == j:
                pending.append((n + 3, i))
            while pending and pending[0][0] <= n:
                epilogue(pending.pop(0)[1])
        while pending:
            epilogue(pending.pop(0)[1])

    def gdn_prep(self, l):
        P, d, A, SM = self.P, self.d, self.AR, self.SM
        XT, RXT = self.XT, self.RXT
        Rsm = self.Rsm
        base = 32768
        WAB, (Rwab,) = A.carve(base, [NKC, 8], BF16)
        GS, (Rgs,) = A.carve(base + 1024, [6, 64], F32)
        CWR, (Rcwr,) = A.carve(base + 4096, [1536], F32)
        CW, (Rcw,) = A.carve(base + 4096 + 6144, [12, 4], F32)
        self.GS, self.Rgs, self.CW, self.Rcw = GS, Rgs, CW, Rcw
        win = d["w_in"][l].rearrange("(c p) n -> p c n", p=128)
        P.dma(WAB, win[:, :, 3584:3592], writes=[Rwab], eng="pool")
        P.dma(CWR[0:4, :], d["conv_w"][l], writes=[Rcwr])
        b = self.bank("misc", [6, 7])
        ps = self.psf(b)
        for t in range(NT):
            for kc in range(NKC):
                self.mm(ps[:, t * 8:(t + 1) * 8], XT[:, kc, t * 128:(t + 1) * 128], WAB[:, kc, :], kc == 0, kc == NKC - 1,
                        [Rwab, RXT[t]], [self.RPS[b]])
        AB = SM[:, 640:768]
        self.cp("dve", AB, ps[:, 0:128], [], [self.RPS[b], Rsm])
        AB3 = AB.rearrange("p (t e) -> p t e", e=8)
        g3 = SM[:, 768:832].rearrange("p (t h) -> p t h", h=4)
        be3 = SM[:, 832:896].rearrange("p (t h) -> p t h", h=4)
        nb3 = SM[:, 896:960].rearrange("p (t h) -> p t h", h=4)
        tmp = SM[:, 960:976]
        for h in range(4):
            self.ts("dve", tmp, AB3[:, :, h], SM[:, 512 + h:513 + h], None, ALU.add, None, [Rsm], [Rsm])
            self.act(tmp, tmp, AF.Exp, [Rsm], [Rsm])
            self.act(tmp, tmp, AF.Ln, [Rsm], [Rsm], bias=1.0)
            self.ts("dve", g3[:, :, h], tmp, SM[:, 516 + h:517 + h], None, ALU.mult, None, [Rsm], [Rsm])
        self.act(SM[:, 832:896].rearrange("p (t h) -> p t h", h=4), AB3[:, :, 4:8], AF.Sigmoid, [Rsm], [Rsm])
        self.ts("dve", SM[:, 896:960], SM[:, 832:896], -1.0, None, ALU.mult, None, [Rsm], [Rsm])
        self.g3, self.be3, self.nb3 = g3, be3, nb3
        b = self.bank("misc", [6, 7])
        ps = self.psf(b)
        self.mm(ps[:, 0:64], self.Uf, SM[:, 768:832], True, True, [self.RCF, Rsm], [self.RPS[b]])
        self.mm(ps[:, 64:128], self.onesf, SM[:, 768:832], True, True, [self.RCF, Rsm], [self.RPS[b]])
        self.cp("dve", GS[:, 0, :], ps[:, 0:64], [], [self.RPS[b], Rgs])
        self.act(GS[:, 1, :], ps[:, 0:64], AF.Exp, [], [self.RPS[b], Rgs])
        self.act(GS[:, 3, :], ps[:, 64:128], AF.Exp, [], [self.RPS[b], Rgs])
        self.tt("dve", GS[:, 4, :], ps[:, 64:128], GS[:, 0, :], ALU.subtract, [Rgs], [self.RPS[b], Rgs])
        self.act(GS[:, 2, :], GS[:, 4, :], AF.Exp, [Rgs], [Rgs])
        b = self.bank("misc", [6, 7])
        ps = self.psf(b)
        for c in range(12):
            self.mm(ps[:, c * 4:(c + 1) * 4], CWR[0:4, c * 128:(c + 1) * 128], self.identf[0:4, 0:4], True, True,
                    [Rcwr, self.RCF], [self.RPS[b]])
        self.cp("dve", CW, ps[:, 0:48].rearrange("p (c j) -> p c j", j=4), [], [self.RPS[b], Rcw])

    def gdn_head(self, l, h):
        P, d, A, SM = self.P, self.d, self.AR, self.SM
        XT, RXT = self.XT, self.RXT
        Rsm, GS, Rgs = self.Rsm, self.GS, self.Rgs
        o = 43264
        WG, (RWG,) = A.carve(o, [NKC, 512], BF16); o += 8192
        qkv, Rqkv = A.carve(o, [3, SEQ], BF16, nres=3); o += 12288
        SZ, (RSZ,) = A.carve(o, [NT, 128], BF16); o += 4096
        area = o
        Raw, (RRaw,) = A.carve(area, [2052], F32)
        acc, (Racc,) = A.carve(area + 8208, [SEQ], F32)
        sqb, Rsqb = A.carve(area + 16400, [2, 512], BF16, nres=2)
        rn, (Rrn,) = A.carve(area + 18448, [512], F32)
        win = d["w_in"][l].rearrange("(c p) n -> p c n", p=128)
        for i, c0 in enumerate((1536, 2048, 2560, 3072)):
            P.dma(WG[:, :, i * 128:(i + 1) * 128], win[:, :, c0 + h * 128:c0 + (h + 1) * 128], writes=[RWG], eng="pool")
        self.memset("pool", Raw[:, 0:3], 0.0, [RRaw])
        cnt = 0
        for which in range(3):
            for tg in range(4):
                b = self.bank("g", list(range(8)))
                ps = self.psf(b)
                for kc in range(NKC):
                    self.mm(ps, WG[:, kc, which * 128:(which + 1) * 128], XT[:, kc, tg * 512:(tg + 1) * 512], kc == 0, kc == NKC - 1,
                            [RWG] + RXT[4 * tg:4 * tg + 4], [self.RPS[b]])
                self.cp("act", Raw[:, 3 + tg * 512:3 + (tg + 1) * 512], ps, [], [self.RPS[b], RRaw])
            cw = self.CW[:, which * 4 + h, :]
            self.ts("dve", acc, Raw[:, 0:SEQ], cw[:, 0:1], None, ALU.mult, None, [RRaw, self.Rcw], [Racc])
            for j in range(1, 4):
                self.stt("dve", acc, Raw[:, j:j + SEQ], cw[:, j:j + 1], acc, ALU.mult, ALU.add, [RRaw, self.Rcw, Racc], [Racc])
            if which == 2:
                self.act(qkv[:, 2, :], acc, AF.Silu, [Racc], [Rqkv[2]])
            else:
                self.act(acc, acc, AF.Silu, [Racc], [Racc])
                for tg in range(4):
                    s = cnt % 2
                    cnt += 1
                    cols = slice(tg * 512, (tg + 1) * 512)
                    self.tt("pool", sqb[:, s, :], acc[:, cols], acc[:, cols], ALU.mult, [Racc], [Rsqb[s]])
                    b = self.bank("g", list(range(8)))
                    ps = self.psf(b)
                    self.mm(ps, self.onesb, sqb[:, s, :], True, True, [self.RCB, Rsqb[s]], [self.RPS[b]])
                    self.act(rn, ps, AF.Ln, [], [self.RPS[b], Rrn], bias=RMS_EPS)
                    self.act(rn, rn, AF.Exp, [Rrn], [Rrn], scale=-0.5, bias=(-0.5 * float(np.log(128.0)) if which == 0 else 0.0))
                    self.tt("dve", qkv[:, which, cols], acc[:, cols], rn, ALU.mult, [Racc, Rrn], [Rqkv[which]])
        SZT = SZ.rearrange("p a n -> p (a n)")
        for tg in range(4):
            b = self.bank("g", list(range(8)))
            ps = self.psf(b)
            for kc in range(NKC):
                self.mm(ps, WG[:, kc, 384:512], XT[:, kc, tg * 512:(tg + 1) * 512], kc == 0, kc == NKC - 1,
                        [RWG] + RXT[4 * tg:4 * tg + 4], [self.RPS[b]])
            self.act(SZT[:, tg * 512:(tg + 1) * 512], ps, AF.Silu, [], [self.RPS[b], RSZ])
        mats, Rm0 = A.carve(area, [16, 128], F32, nres=16)
        bm, Rb = A.carve(area + 16 * 512, [20, 128], BF16, nres=20)
        def _loc(i):
            if i < 26:
                bp, k = divmod(i, 13)
                if k < 5:
                    return ("a", bp * 5 + k)
                return ("r", bp * 8 + (k - 5))
            return ("a", 10 + (i - 26))

        class _RmProxy:
            def __getitem__(_s, i):
                kind, j = _loc(i)
                return Rm0[j] if kind == "a" else self.RRr[j]
        Rm = _RmProxy()

        def M(i):
            kind, j = _loc(i)
            return mats[:, j, :] if kind == "a" else self.RR[:, j, :]

        def MR(i):
            kind, j = _loc(i)
            assert kind == "r"
            return self.RR[:, j, :].bitcast(F32R) if USE_F32R else self.RR[:, j, :]
        B = lambda i: bm[:, i, :]
        qT, kT, vT = qkv[:, 0, :], qkv[:, 1, :], qkv[:, 2, :]
        S_i = [17, 18]
        VN, OG = 16, 19
        JK, ON = 30, 31
        og = B(OG)
        self.memset("pool", B(S_i[0]), 0.0, [Rb[S_i[0]]])
        Uf, Usf, identf, onesf = self.Uf, self.Usf, self.identf, self.onesf
        RCF = self.RCF
        g3, be3, nb3 = self.g3, self.be3, self.nb3
        gs = lambda k, n: GS[:, k, n * 4 + h:n * 4 + h + 1]
        busy = set()
        rr = [0]

        def newbank():
            for _ in range(8):
                b = rr[0] % 8
                rr[0] += 1
                if b not in busy:
                    busy.add(b)
                    return b
            raise RuntimeError("no free PSUM bank")

        def rel(b):
            busy.discard(b)

        def pre_steps(n, bpos, par):
            T = lambda k: bpos * 13 + k
            Pb = lambda k: par * 8 + bpos * 4 + k
            Pu = 26 + par * 2 + bpos
            cs = slice(n * 128, (n + 1) * 128)
            beta = be3[:, n, h:h + 1]
            st = {}
            steps = []

            def s0():
                self.act(M(T(0)), onesf, AF.Copy, [RCF, Rsm], [Rm[T(0)]], scale=g3[:, n, h:h + 1])
                b = newbank(); st["G"] = b
                self.mm(self.psf(b)[:, 0:128], M(T(0)), Uf, True, True, [Rm[T(0)], RCF], [self.RPS[b]])
                b = newbank(); st["A"] = b
                self.mm(self.psf(b)[:, 0:128], kT[:, cs], kT[:, cs], True, True, [Rqkv[1]], [self.RPS[b]])
                self.mm(self.psf(b)[:, 128:256], kT[:, cs], qT[:, cs], True, True, [Rqkv[0], Rqkv[1]], [self.RPS[b]])
                b = newbank(); st["KV"] = b
                pv = self.psb16(b)
                self.tr(pv[:, 0:128], kT[:, cs], self.identb, [Rqkv[1], self.RCB], [self.RPS[b]])
                self.tr(pv[:, 128:256], vT[:, cs], self.identb, [Rqkv[2], self.RCB], [self.RPS[b]])
            steps.append(s0)

            def s1():
                b = st["G"]
                self.ts("dve", M(T(1)), self.psf(b)[:, 0:128], gs(0, n), 0.0, ALU.subtract, ALU.min, [Rgs], [self.RPS[b], Rm[T(1)]])
                self.act(M(T(4)), self.psf(b)[:, 0:128], AF.Exp, [], [self.RPS[b], Rm[T(4)]])
                self.act(M(T(1)), M(T(1)), AF.Exp, [Rm[T(1)]], [Rm[T(1)]])
                rel(b)
                b = st["KV"]
                pv = self.psb16(b)
                self.act(B(Pb(2)), pv[:, 0:128], AF.Copy, [Rgs], [self.RPS[b], Rb[Pb(2)]], scale=gs(2, n))
            steps.append(s1)

            def s2():
                self.tt("pool", M(T(2)), M(T(1)), Uf, ALU.mult, [Rm[T(1)], RCF], [Rm[T(2)]])
                self.tt("pool", M(T(3)), M(T(1)), Usf, ALU.mult, [Rm[T(1)], RCF], [Rm[T(3)]])
                self.tt("pool", B(Pb(0)), qT[:, cs], M(T(4)), ALU.mult, [Rqkv[0], Rm[T(4)]], [Rb[Pb(0)]])
            steps.append(s2)

            def s3():
                b = st["A"]
                self.stt("dve", MR(T(5)), self.psf(b)[:, 0:128], beta, M(T(3)), ALU.mult, ALU.mult, [Rsm, Rm[T(3)]], [self.RPS[b], Rm[T(5)]])
                self.tt("dve", B(Pb(1)), self.psf(b)[:, 128:256], M(T(2)), ALU.mult, [Rm[T(2)]], [self.RPS[b], Rb[Pb(1)]])
                rel(b)
            steps.append(s3)

            def s4():
                b = newbank(); st["X"] = b
                self.tr(self.psf(b)[:, 0:128], M(T(5)), identf, [Rm[T(5)], RCF], [self.RPS[b]])
                self.tt("pool", MR(T(9)), identf, M(T(5)), ALU.subtract, [RCF, Rm[T(5)]], [Rm[T(9)]])
            steps.append(s4)

            def s5():
                b = st["X"]
                self.cp("act", MR(T(6)), self.psf(b)[:, 0:128], [], [self.RPS[b], Rm[T(6)]])
                rel(b)
                b = st["KV"]
                pv = self.psb16(b)
                self.ts("dve", MR(T(11)), pv[:, 0:128], gs(1, n), None, ALU.mult, None, [Rgs], [self.RPS[b], Rm[T(11)]])
                self.cp("act", MR(T(12)), pv[:, 128:256], [], [self.RPS[b], Rm[T(12)]])
                rel(b)
            steps.append(s5)
            zs = {0: T(5)}
            zts = {0: T(6)}
            ns = {0: T(9)}
            for j in range(1, 7):
                zs[j] = T(7) if j % 2 == 1 else T(5)
                zts[j] = T(8) if j % 2 == 1 else T(6)
                ns[j] = T(10) if j % 2 == 1 else T(9)
            KE, VC = T(11), T(12)

            def lvl_a(j):
                def f():
                    b = newbank(); st["Z%d" % j] = b
                    self.mm(self.psf(b)[:, 0:128], MR(zs[j - 1]), MR(zts[j - 1]), True, True, [Rm[zs[j - 1]], Rm[zts[j - 1]]], [self.RPS[b]])
                    if j < 6:
                        self.mm(self.psf(b)[:, 128:256], MR(zts[j - 1]), MR(zs[j - 1]), True, True, [Rm[zs[j - 1]], Rm[zts[j - 1]]], [self.RPS[b]])
                return f

            def lvl_b(j):
                def f():
                    b = st["Z%d" % j]
                    self.cp("act", MR(zts[j]), self.psf(b)[:, 0:128], [], [self.RPS[b], Rm[zts[j]]])
                    if j < 6:
                        self.cp("dve", MR(zs[j]), self.psf(b)[:, 128:256], [], [self.RPS[b], Rm[zs[j]]])
                    rel(b)
                return f

            def lvl_c(j):
                def f():
                    b = newbank(); st["N%d" % j] = b
                    self.mm(self.psf(b)[:, 0:128], MR(zts[j]), MR(ns[j - 1]), True, True, [Rm[zts[j]], Rm[ns[j - 1]]], [self.RPS[b]])
                return f

            def lvl_d(j):
                def f():
                    b = st["N%d" % j]
                    self.tt("dve", MR(ns[j]), self.psf(b)[:, 0:128], M(ns[j - 1]), ALU.add, [Rm[ns[j - 1]]], [self.RPS[b], Rm[ns[j]]])
                    rel(b)
                return f

            def both(f1, f2):
                def f():
                    f1()
                    if f2 is not None:
                        f2()
                return f
            steps += [lvl_a(1), lvl_b(1)]
            for j in range(1, 7):
                steps += [both(lvl_c(j), lvl_a(j + 1) if j < 6 else None), both(lvl_d(j), lvl_b(j + 1) if j < 6 else None)]
            NTi = ns[6]

            def s8():
                b = newbank(); st["WU"] = b
                self.mm(self.psf(b)[:, 0:128], MR(KE), MR(NTi), True, True, [Rm[KE], Rm[NTi]], [self.RPS[b]])
                self.mm(self.psf(b)[:, 128:256], MR(NTi), MR(VC), True, True, [Rm[VC], Rm[NTi]], [self.RPS[b]])
            steps.append(s8)

            def s9():
                b = st["WU"]
                self.cp("act", B(Pb(3)), self.psf(b)[:, 0:128], [], [self.RPS[b], Rb[Pb(3)]])
                self.ts("dve", M(Pu), self.psf(b)[:, 128:256], beta, None, ALU.mult, None, [Rsm], [self.RPS[b], Rm[Pu]])
                rel(b)
            steps.append(s9)
            return steps

        def scan_steps(n, bpos, par):
            Pb = lambda k: par * 8 + bpos * 4 + k
            Pu = 26 + par * 2 + bpos
            cs = slice(n * 128, (n + 1) * 128)
            Sc, Sn = S_i[n % 2], S_i[(n + 1) % 2]
            st = {}
            steps = []

            def a0():
                b = newbank(); st["1"] = b
                self.mm(self.psf(b)[:, 0:128], B(Pb(3)), B(Sc), True, True, [Rb[Pb(3)], Rb[Sc]], [self.RPS[b]])
            steps.append(a0)

            def a1():
                b = st["1"]
                self.stt("dve", B(VN), self.psf(b)[:, 0:128], nb3[:, n, h:h + 1], M(Pu), ALU.mult, ALU.add, [Rsm, Rm[Pu]], [self.RPS[b], Rb[VN]])
                rel(b)
            steps.append(a1)

            def a2():
                b = newbank(); st["O"] = b
                self.mm(self.psf(b)[:, 0:128], B(Pb(0)), B(Sc), True, False, [Rb[Pb(0)], Rb[Sc]], [self.RPS[b]])
                self.mm(self.psf(b)[:, 0:128], B(Pb(1)), B(VN), False, True, [Rb[Pb(1)], Rb[VN]], [self.RPS[b]])
                b = newbank(); st["S"] = b
                self.mm(self.psf(b)[:, 0:128], B(Pb(2)), B(VN), True, True, [Rb[Pb(2)], Rb[VN]], [self.RPS[b]])
            steps.append(a2)

            def a3():
                b = st["S"]
                self.stt("dve", B(Sn), B(Sc), gs(3, n), self.psf(b)[:, 0:128], ALU.mult, ALU.add, [Rb[Sc], Rgs], [self.RPS[b], Rb[Sn]])
                rel(b)
                b = st["O"]
                self.act(M(JK), self.psf(b)[:, 0:128], AF.Square, [], [self.RPS[b], Rm[JK], Rm[ON]], accum_out=M(ON)[:, 0:1])
            steps.append(a3)

            def a4():
                self.act(M(ON)[:, 1:2], M(ON)[:, 0:1], AF.Ln, [Rm[ON]], [Rm[ON]], scale=1.0 / 128.0, bias=RMS_EPS)
                self.act(M(ON)[:, 2:3], M(ON)[:, 1:2], AF.Exp, [Rm[ON]], [Rm[ON]], scale=-0.5)
            steps.append(a4)

            def a5():
                b = st["O"]
                self.stt("dve", og, self.psf(b)[:, 0:128], M(ON)[:, 2:3], self.GNW, ALU.mult, ALU.mult, [Rm[ON], Rsm], [self.RPS[b], Rb[OG]])
                rel(b)
                b = newbank(); st["T"] = b
                self.tr(self.psb16(b)[:, 0:128], og, self.identb, [Rb[OG], self.RCB], [self.RPS[b]])
            steps.append(a5)

            def a6():
                b = st["T"]
                self.tt("dve", self.MT[:, 4 + h, cs], self.psb16(b)[:, 0:128], SZT[:, cs], ALU.mult, [RSZ], [self.RPS[b], self.RMT[4 + h][n]])
                rel(b)
            steps.append(a6)
            return steps

        def merged(step_lists):
            idx = [0] * len(step_lists)
            alive = True
            while alive:
                alive = False
                for k, sl in enumerate(step_lists):
                    if idx[k] < len(sl):
                        sl[idx[k]]()
                        idx[k] += 1
                        alive = True

        nb = NT // 2
        pre = lambda k: [pre_steps(2 * k, 0, k % 2), pre_steps(2 * k + 1, 1, k % 2)]
        merged(pre(0))
        for k in range(nb):
            lists = []
            if k + 1 < nb:
                lists += pre(k + 1)
            sc = scan_steps(2 * k, 0, k % 2) + scan_steps(2 * k + 1, 1, k % 2)
            lists.append(sc)
            merged(lists)

    def mixer(self, l):
        A = self.AR
        self.MT, R = A.carve(0, [8, SEQ], BF16, nres=8 * NT)
        self.RMT = [[R[k * NT + t] for t in range(NT)] for k in range(8)]
        self.layer_params(l)
        for h in range(4):
            self.da_head(l, h)
        if self.upto == "da":
            return
        self.gdn_prep(l)
        for h in range(4):
            self.gdn_head(l, h)

    def out_proj_ln1(self, l):
        P, d, A = self.P, self.d, self.AR
        Wo, (RWo,) = A.carve(32768, [NKC, DM], BF16)
        P.dma(Wo, d["w_out"][l].rearrange("(c p) n -> p c n", p=128), writes=[RWo], eng="pool")
        for t in range(NT):
            for hf in range(2):
                b = self.bank("o", [0, 1, 2, 3])
                ps = self.psf(b)
                for kc in range(NKC):
                    self.mm(ps, self.MT[:, kc, t * 128:(t + 1) * 128], Wo[:, kc, hf * 512:(hf + 1) * 512], kc == 0, kc == NKC - 1,
                            [self.RMT[kc][t], RWo], [self.RPS[b]])
                xs = self.X[:, t, hf * 512:(hf + 1) * 512]
                self.stt("dve", xs, xs, ALPHA, ps, ALU.mult, ALU.add, [self.RX[t]], [self.RPS[b], self.RX[t]])
        self.layer_norm_all("ln1_g", "ln1_b", l, 49152)

    def ffn_ln2(self, l, last):
        P, d, A = self.P, self.d, self.AR
        X, RX, XT, RXT = self.X, self.RX, self.XT, self.RXT
        moe = (l % 2 == 1)
        li = l // 2
        o = 0
        WS = []
        for s in range(2):
            wg, (Rwg,) = A.carve(o, [NKC, 512], BF16); o += 8192
            wu, (Rwu,) = A.carve(o, [NKC, 512], BF16); o += 8192
            wd, (Rwd,) = A.carve(o, [4, DM], BF16); o += 8192
            WS.append((wg, Rwg, wu, Rwu, wd, Rwd))
        hT, RhT = A.carve(o, [2, 4, 512], BF16, nres=2); o += 8192
        sg, Rsg = A.carve(o, [2, 512], BF16, nres=2); o += 2048
        lnoff = o; o += 16896
        GT, (RGT,) = A.carve(o, [NT, 8], F32); o += 512
        if moe:
            RWt, (RRW,) = A.carve(o, [NKC, 8], F32); o += 256
            lg, (Rlg,) = A.carve(o, [64], F32); o += 256
            assert o <= 81920
            XTf, RXTf = A.carve(lnoff, [2, NKC, 128], F32, nres=2)
            P.dma(RWt, d["router_w"][li].rearrange("(c p) e -> p c e", p=128), writes=[RRW])
            for t in range(NT):
                s = t % 2
                for half in range(2):
                    b = self.bank("rt", [4, 5, 6, 7])
                    ps = self.psf(b)
                    for q in range(4):
                        kc = half * 4 + q
                        self.tr(ps[:, q * 128:(q + 1) * 128], X[:, t, kc * 128:(kc + 1) * 128], self.identf, [RX[t], self.RCF], [self.RPS[b]])
                    self.cp("act" if half else "dve", XTf[:, s, half * 4:half * 4 + 4, :], ps.rearrange("p (a n) -> p a n", a=4),
                            [], [self.RPS[b], RXTf[s]])
                b = self.bank("rt", [4, 5, 6, 7])
                ps = self.psf(b)
                for kc in range(NKC):
                    self.mm(ps[:, 0:8], XTf[:, s, kc, :], RWt[:, kc, :], kc == 0, kc == NKC - 1, [RXTf[s], RRW], [self.RPS[b]])
                self.cp("dve", lg[:, 0:8], ps[:, 0:8], [], [self.RPS[b], Rlg])
                self.P.op("dve", lambda e: e.max(out=lg[:, 8:16], in_=lg[:, 0:8]), [Rlg], [Rlg])
                self.tt("dve", lg[:, 16:17], lg[:, 9:10], lg[:, 8:9], ALU.subtract, [Rlg], [Rlg])
                self.act(lg[:, 17:18], lg[:, 16:17], AF.Exp, [Rlg], [Rlg])
                self.ts("dve", lg[:, 18:19], lg[:, 17:18], 1.0, None, ALU.add, None, [Rlg], [Rlg])
                self.P.op("dve", lambda e: e.reciprocal(out=lg[:, 19:20], in_=lg[:, 18:19]), [Rlg], [Rlg])
                self.tt("dve", lg[:, 20:21], lg[:, 17:18], lg[:, 19:20], ALU.mult, [Rlg], [Rlg])
                self.ts("dve", lg[:, 24:32], lg[:, 0:8], lg[:, 8:9], lg[:, 19:20], ALU.is_equal, ALU.mult, [Rlg], [Rlg])
                self.ts("dve", lg[:, 32:40], lg[:, 0:8], lg[:, 9:10], lg[:, 20:21], ALU.is_equal, ALU.mult, [Rlg], [Rlg])
                self.tt("dve", GT[:, t, :], lg[:, 24:32], lg[:, 32:40], ALU.add, [Rlg], [RGT])
        for t in range(NT):
            self.P.op("act", lambda e, t=t: e.mul(out=X[:, t, :], in_=X[:, t, :], mul=ALPHA), [RX[t]], [RX[t]])
        if moe:
            groups = [(e, c0, 4) for e in range(NEXP) for c0 in range(0, FF_MOE, 512)]
        else:
            groups = [(None, c0, min(4, (FF_DENSE - c0) // 128)) for c0 in range(0, FF_DENSE, 512)]

        def load(gi):
            e, c0, nch = groups[gi]
            wg, Rwg, wu, Rwu, wd, Rwd = WS[gi % 2]
            if moe:
                srcg, srcu, srcd = d["moe_w_gate"][li, e], d["moe_w_up"][li, e], d["moe_w_down"][li, e]
            else:
                srcg, srcu, srcd = d["ffn_w_gate"][li], d["ffn_w_up"][li], d["ffn_w_down"][li]
            w = nch * 128
            P.dma(wg[:, :, 0:w], srcg.rearrange("(c p) n -> p c n", p=128)[:, :, c0:c0 + w], writes=[Rwg], eng="pool")
            P.dma(wu[:, :, 0:w], srcu.rearrange("(c p) n -> p c n", p=128)[:, :, c0:c0 + w], writes=[Rwu], eng="pool")
            P.dma(wd[:, 0:nch, :], srcd[c0:c0 + w, :].rearrange("(c p) n -> p c n", p=128), writes=[Rwd], eng="pool")

        items = [(gi, tg) for gi in range(len(groups)) for tg in range(4)]

        def up(k):
            gi, tg = items[k]
            e, c0, nch = groups[gi]
            wg, Rwg, wu, Rwu, wd, Rwd = WS[gi % 2]
            hs = k % 2
            cols = slice(tg * 512, (tg + 1) * 512)
            for c in range(nch):
                bg = self.bank("fg", [0, 1])
                bu = self.bank("fu", [2, 3])
                for kc in range(NKC):
                    self.mm(self.psf(bg), wg[:, kc, c * 128:(c + 1) * 128], XT[:, kc, cols], kc == 0, kc == NKC - 1,
                            [Rwg] + RXT[4 * tg:4 * tg + 4], [self.RPS[bg]])
                for kc in range(NKC):
                    self.mm(self.psf(bu), wu[:, kc, c * 128:(c + 1) * 128], XT[:, kc, cols], kc == 0, kc == NKC - 1,
                            [Rwu] + RXT[4 * tg:4 * tg + 4], [self.RPS[bu]])
                s = self.bank("sg", [0, 1])
                self.act(sg[:, s, :], self.psf(bg), AF.Silu, [], [self.RPS[bg], Rsg[s]])
                self.tt("dve", hT[:, hs, c, :], self.psf(bu), sg[:, s, :], ALU.mult, [Rsg[s]], [self.RPS[bu], RhT[hs]])

        def down(k):
            gi, tg = items[k]
            e, c0, nch = groups[gi]
            wg, Rwg, wu, Rwu, wd, Rwd = WS[gi % 2]
            hs = k % 2
            for ti in range(4):
                t = 4 * tg + ti
                for hf in range(2):
                    b = self.bank("fd", [4, 5, 6, 7])
                    ps = self.psf(b)
                    for c in range(nch):
                        self.mm(ps, hT[:, hs, c, ti * 128:(ti + 1) * 128], wd[:, c, hf * 512:(hf + 1) * 512], c == 0, c == nch - 1,
                                [RhT[hs], Rwd], [self.RPS[b]])
                    xs = X[:, t, hf * 512:(hf + 1) * 512]
                    if moe:
                        self.stt("dve", xs, ps, GT[:, t, e:e + 1], xs, ALU.mult, ALU.add, [RGT, RX[t]], [self.RPS[b], RX[t]])
                    else:
                        self.tt("dve", xs, ps, xs, ALU.add, [RX[t]], [self.RPS[b], RX[t]])

        load(0)
        for k in range(len(items)):
            gi, tg = items[k]
            up(k)
            if k > 0:
                down(k - 1)
            if tg == 0 and gi + 1 < len(groups):
                load(gi + 1)
        down(len(items) - 1)
        self.layer_norm_all("ln2_g", "ln2_b", l, lnoff, need_xt=not last)

    def layer(self, l):
        if self.upto == "pro":
            return
        self.mixer(l)
        if self.upto in ("da", "mix"):
            return
        self.out_proj_ln1(l)
        if self.upto == "ln1":
            return
        self.ffn_ln2(l, last=(l == self.n_layers - 1))

    def output(self):
        self.dump_X("y")

    def debug_dump(self):
        if self.upto == "pro":
            import os
            if "xt" in os.environ.get("PRO_SKIP", ""):
                self.dump_X("dbg")
            else:
                self.dump_featmajor_bf16(self.XT[:], self.RXT)
        elif self.upto == "da":
            self.dump_featmajor_bf16(self.MT[:, 0:4, :], [r for rl in self.RMT[0:4] for r in rl])
        elif self.upto == "mix":
            self.dump_featmajor_bf16(self.MT, [r for rl in self.RMT for r in rl])
        else:
            self.dump_X("dbg")


_CONST_CACHE = {}


def _consts():
    if "cf" in _CONST_CACHE:
        return _CONST_CACHE["cf"]
    cf = np.zeros((128, 648), np.float32)
    idx = np.arange(128)
    cf[:, 0:128] = np.eye(128, dtype=np.float32)
    cf[:, 128:256] = (idx[:, None] <= idx[None, :]).astype(np.float32)
    cf[:, 256:384] = (idx[:, None] < idx[None, :]).astype(np.float32)
    cf[:, 384:512] = 1.0
    rm = np.zeros((128, 128), np.float32)
    for blk in (0, 64):
        for dd in range(32):
            rm[blk + dd + 32, blk + dd] = -1.0
            rm[blk + dd, blk + dd + 32] = 1.0
    cf[:, 512:640] = rm
    inv_freq = 10000.0 ** (-np.arange(0, 64, 2, dtype=np.float32) / 64.0)
    cf[:, 640] = (inv_freq[idx % 32].astype(np.float64) / TWO_PI).astype(np.float32)
    _CONST_CACHE["cf"] = cf
    return cf


def build_program(n_layers=DEPTH, upto=None):
    nc = bass.Bass("TRN2", target_bir_lowering=False)
    with ExitStack() as st:
        P = Prog(nc, st)
        K = Kern(nc, P, st, n_layers, upto)
        K.prologue()
        for l in range(n_layers):
            K.layer(l)
        if upto is None:
            K.output()
        else:
            K.debug_dump()
        P.finish()
    return nc


WEIGHT_KEYS = ("w_in", "conv_w", "a_log", "dt_bias", "gdn_norm_w", "lam_q1", "lam_k1", "lam_q2", "lam_k2",
               "subln_w", "w_out", "ln1_g", "ln1_b", "ln2_g", "ln2_b", "ffn_w_gate", "ffn_w_up", "ffn_w_down",
               "router_w", "moe_w_gate", "moe_w_up", "moe_w_down")


def make_in_maps(inputs, cores):
    cf = _consts()
    shared = {k: np.ascontiguousarray(np.asarray(inputs[k], dtype=np.float32)) for k in WEIGHT_KEYS}
    x = np.asarray(inputs["x"], dtype=np.float32)
    pos = np.asarray(inputs["positions"]).astype(np.int32)
    maps = []
    for b in cores:
        m = dict(shared)
        m["x"] = np.ascontiguousarray(x[b])
        m["pos"] = np.ascontiguousarray(pos[b:b + 1])
        m["cf"] = cf
        maps.append(m)
    return maps


def kernel(**inputs):
    nc = build_program()
    in_maps = make_in_maps(inputs, range(8))
    res = run_bass_kernel_spmd(nc, in_maps, core_ids=list(range(8)))
    out = np.stack([np.asarray(r["y"], dtype=np.float32) for r in res.results], axis=0)
    return out
```

```python
import numpy as np
from contextlib import ExitStack
import concourse.bass as bass
import concourse.mybir as mybir
from concourse.bass_utils import run_bass_kernel_spmd

F32 = mybir.dt.float32
F32R = mybir.dt.float32r
BF16 = mybir.dt.bfloat16
I32 = mybir.dt.int32
AF = mybir.ActivationFunctionType
ALU = mybir.AluOpType
AX = mybir.AxisListType

EPOCH = 16384
USE_F32R = True


class Res:
    __slots__ = ("w", "rs", "name")

    def __init__(self, name=""):
        self.w = None
        self.rs = []
        self.name = name


class Prog:
    ENGS = ("pe", "act", "dve", "pool", "sp")

    def __init__(self, nc, stack, n_dma_sems=32):
        self.nc = nc
        self.stack = stack
        self.streams = {e: [] for e in self.ENGS}
        self.cnt = {e: 0 for e in self.ENGS}
        self.esems = {e: [] for e in self.ENGS}
        self.seen = {e: {} for e in self.ENGS}
        self.dsems = [stack.enter_context(nc.semaphore(f"dq{i}")) for i in range(n_dma_sems)]
        self.duse = [0] * n_dma_sems
        half = n_dma_sems // 2
        self.dpool = {"sp": list(range(0, half)), "pool": list(range(half, n_dma_sems))}
        self.dnext = {"sp": 0, "pool": 0}
        self.out_events = []

    def _esem(self, eng, epoch):
        lst = self.esems[eng]
        while len(lst) <= epoch:
            lst.append(self.stack.enter_context(self.nc.semaphore(f"c_{eng}_{len(lst)}")))
        return lst[epoch]

    def _collect(self, eng, reads, writes, is_dma):
        need = {}

        def add(ev, hazard):
            if ev is None:
                return
            if ev[0] == "c":
                if ev[1] == eng and not is_dma and hazard != "raw" and eng == "pe":
                    return
                key = ("c", ev[1])
            else:
                key = ("d", ev[1])
            if need.get(key, 0) < ev[2]:
                need[key] = ev[2]

        for r in reads:
            add(r.w, "raw")
        for w in writes:
            add(w.w, "waw")
            for ev in w.rs:
                add(ev, "war")
        waits = []
        seen = self.seen[eng]
        for key, val in need.items():
            if seen.get(key, 0) >= val:
                continue
            seen[key] = val
            if key[0] == "c":
                ep, v = divmod(val - 1, EPOCH)
                waits.append((self._esem(key[1], ep), v + 1))
            else:
                waits.append((self.dsems[key[1]], val))
        return waits

    def _commit(self, ev, reads, writes):
        for r in reads:
            r.rs.append(ev)
        for w in writes:
            w.w = ev
            w.rs = []

    def op(self, eng, fn, reads=(), writes=()):
        waits = self._collect(eng, reads, writes, False)
        self.cnt[eng] += 1
        g = self.cnt[eng]
        ep, v = divmod(g - 1, EPOCH)
        self.streams[eng].append((fn, waits, (self._esem(eng, ep), 1)))
        ev = ("c", eng, g)
        self._commit(ev, reads, writes)
        return ev

    def dma(self, out, in_, reads=(), writes=(), eng="sp", is_output=False, **kw):
        lst = self.dpool[eng]
        j = lst[self.dnext[eng] % len(lst)]
        self.dnext[eng] += 1
        waits = self._collect(eng, reads, writes, True)
        prev = self.duse[j] * 16
        seen = self.seen[eng]
        if prev and seen.get(("d", j), 0) < prev:
            seen[("d", j)] = prev
            waits.append((self.dsems[j], prev))
        self.duse[j] += 1
        val = self.duse[j] * 16
        fn = lambda e, out=out, in_=in_, kw=kw: e.dma_start(out=out, in_=in_, **kw)
        self.streams[eng].append((fn, waits, (self.dsems[j], 16)))
        ev = ("d", j, val)
        self._commit(ev, reads, writes)
        if is_output:
            self.out_events.append(ev)
        return ev

    def finish(self):
        need = {}
        for ev in self.out_events:
            need[ev[1]] = max(need.get(ev[1], 0), ev[2])
        fin = [(self.dsems[j], v) for j, v in need.items()]
        nc = self.nc
        streams = self.streams
        with nc.Block() as block:
            def emit(e, lst, final=()):
                for fn, waits, inc in lst:
                    for s, v in waits:
                        e.wait_ge(s, v)
                    fn(e).then_inc(inc[0], inc[1])
                for s, v in final:
                    e.wait_ge(s, v)

            @block.tensor
            def _(e):
                emit(e, streams["pe"])

            @block.scalar
            def _(e):
                emit(e, streams["act"])

            @block.vector
            def _(e):
                emit(e, streams["dve"])

            @block.gpsimd
            def _(e):
                emit(e, streams["pool"])

            @block.sync
            def _(e):
                emit(e, streams["sp"], fin)


SEQ = 2048
DM = 1024
NT = 16
NKC = 8
DEPTH = 4
D_IN = 3592
FF_DENSE = 2816
FF_MOE = 3584
NEXP = 8
ALPHA = (2.0 * DEPTH) ** 0.25
LN_EPS = 1e-5
RMS_EPS = 1e-6
TWO_PI = 6.283185307179586
ARENA_W = 22528
MT_OFF = 0


def _dsize(dt):
    return 4 if dt in (F32, I32) else 2


class Arena:
    def __init__(self, t):
        self.t = t
        self.live = []
        self.frozen = []

    @staticmethod
    def _compress(res_list):
        d = {}
        for r in res_list:
            for ev in ([r.w] if r.w else []) + list(r.rs):
                key = (ev[0], ev[1])
                if key not in d or d[key][2] < ev[2]:
                    d[key] = ev
        return d

    def carve(self, lo, shape, dt, nres=1):
        n = 1
        for s in shape:
            n *= s
        nbytes = n * _dsize(dt)
        hi = lo + nbytes
        assert lo % 4 == 0 and hi <= ARENA_W * 4, (lo, hi)
        evs = {}
        keep = []
        for (l2, h2, rl) in self.live:
            if l2 < hi and lo < h2:
                d = self._compress(rl)
                self.frozen.append((l2, h2, d))
            else:
                keep.append((l2, h2, rl))
        newf = []
        for (l2, h2, d) in self.frozen:
            if l2 < hi and lo < h2:
                for key, ev in d.items():
                    if key not in evs or evs[key][2] < ev[2]:
                        evs[key] = ev
            if not (lo <= l2 and h2 <= hi):
                newf.append((l2, h2, d))
        self.frozen = newf
        res = [Res() for _ in range(nres)]
        for r in res:
            r.rs = list(evs.values())
        self.live = keep + [(lo, hi, res)]
        ap = self.t[:, lo // 4:(hi + 3) // 4]
        if dt != F32:
            ap = ap.bitcast(dt)
        if len(shape) == 2:
            ap = ap.rearrange("p (a b) -> p a b", a=shape[0])
        elif len(shape) == 3:
            ap = ap.rearrange("p (a b c) -> p a b c", a=shape[0], b=shape[1])
        return ap, res


class Kern:
    def __init__(self, nc, P, st, n_layers=DEPTH, upto=None):
        self.nc, self.P, self.st = nc, P, st
        self.n_layers = n_layers
        self.upto = upto
        dr = lambda name, shape, dt=F32, kind="ExternalInput": nc.dram_tensor(name, shape, dt, kind=kind).ap()
        self.d = {}
        self.d["x"] = dr("x", [SEQ, DM])
        self.d["pos"] = dr("pos", [1, SEQ], I32)
        self.d["cf"] = dr("cf", [128, 648])
        self.d["w_in"] = dr("w_in", [DEPTH, DM, D_IN])
        self.d["conv_w"] = dr("conv_w", [DEPTH, 4, 1536])
        self.d["a_log"] = dr("a_log", [DEPTH, 4])
        self.d["dt_bias"] = dr("dt_bias", [DEPTH, 4])
        self.d["gdn_norm_w"] = dr("gdn_norm_w", [DEPTH, 128])
        for k in ("lam_q1", "lam_k1", "lam_q2", "lam_k2"):
            self.d[k] = dr(k, [DEPTH, 64])
        self.d["subln_w"] = dr("subln_w", [DEPTH, 128])
        self.d["w_out"] = dr("w_out", [DEPTH, DM, DM])
        for k in ("ln1_g", "ln1_b", "ln2_g", "ln2_b"):
            self.d[k] = dr(k, [DEPTH, DM])
        self.d["ffn_w_gate"] = dr("ffn_w_gate", [2, DM, FF_DENSE])
        self.d["ffn_w_up"] = dr("ffn_w_up", [2, DM, FF_DENSE])
        self.d["ffn_w_down"] = dr("ffn_w_down", [2, FF_DENSE, DM])
        self.d["router_w"] = dr("router_w", [2, DM, NEXP])
        self.d["moe_w_gate"] = dr("moe_w_gate", [2, NEXP, DM, FF_MOE])
        self.d["moe_w_up"] = dr("moe_w_up", [2, NEXP, DM, FF_MOE])
        self.d["moe_w_down"] = dr("moe_w_down", [2, NEXP, FF_MOE, DM])
        self.d["y"] = dr("y", [SEQ, DM], F32, "ExternalOutput")
        if upto is not None:
            self.d["dbg"] = dr("dbg", [SEQ, DM], F32, "ExternalOutput")

        sb = lambda name, shape, dt: st.enter_context(nc.sbuf_tensor(name, shape, dt))
        self.X = sb("X", [128, NT, DM], F32)
        self.RX = [Res(f"X{t}") for t in range(NT)]
        self.XT = sb("XT", [128, NKC, SEQ], BF16)
        self.RXT = [Res(f"XT{t}") for t in range(NT)]
        self.CF = sb("CF", [128, 648], F32)
        self.RCF = Res("CF")
        self.CB = sb("CB", [128, 5, 128], BF16)
        self.RCB = Res("CB")
        self.cosT = sb("cosT", [128, SEQ], BF16)
        self.sinT = sb("sinT", [128, SEQ], BF16)
        self.RTAB = Res("tab")
        self.SM = sb("SM", [128, 1024], F32)
        self.Rsm = Res("sm")
        self.AR = Arena(sb("AR", [128, ARENA_W], F32))
        self.RR = sb("RR", [128, 16, 128], F32)
        self.RRr = [Res(f"rr{i}") for i in range(16)]
        self.PSB = [st.enter_context(nc.psum_tensor(f"ps{b}", [128, 512], F32)) for b in range(8)]
        self.RPS = [Res(f"ps{b}") for b in range(8)]
        self.pools = {}
        self.identf = self.CF[:, 0:128]
        self.Uf = self.CF[:, 128:256]
        self.Usf = self.CF[:, 256:384]
        self.onesf = self.CF[:, 384:512]
        self.invf = self.CF[:, 640:641]
        self.identb = self.CB[:, 0, :]
        self.Ub = self.CB[:, 1, :]
        self.onesb = self.CB[:, 2, :]
        self.Rmb = self.CB[:, 3, :]

    def bank(self, pool, banks=None):
        if pool not in self.pools:
            self.pools[pool] = [banks, 0]
        lst, c = self.pools[pool]
        self.pools[pool][1] += 1
        b = lst[c % len(lst)]
        return b

    def psf(self, b):
        return self.PSB[b][:]

    def psb16(self, b):
        return self.PSB[b][:].bitcast(BF16)

    def mm(self, out, lhsT, rhs, start, stop, reads, writes):
        self.P.op("pe", lambda e: e.matmul(out, lhsT=lhsT, rhs=rhs, start=start, stop=stop), reads, writes)

    def mmr(self, out, lhsT, rhs, start, stop, reads, writes):
        if not USE_F32R:
            return self.mm(out, lhsT, rhs, start, stop, reads, writes)
        a, b = lhsT.bitcast(F32R), rhs.bitcast(F32R)
        self.P.op("pe", lambda e: e.matmul(out, lhsT=a, rhs=b, start=start, stop=stop), reads, writes)

    def tr(self, out, in_, ident, reads, writes):
        self.P.op("pe", lambda e: e.transpose(out, in_, ident), reads, writes)

    def act(self, out, in_, func, reads, writes, **kw):
        self.P.op("act", lambda e: e.activation(out=out, in_=in_, func=func, **kw), reads, writes)

    def tt(self, eng, out, in0, in1, op, reads, writes):
        self.P.op(eng, lambda e: e.tensor_tensor(out=out, in0=in0, in1=in1, op=op), reads, writes)

    def ts(self, eng, out, in0, s1, s2, op0, op1, reads, writes, **kw):
        if op1 is None:
            self.P.op(eng, lambda e: e.tensor_scalar(out=out, in0=in0, scalar1=s1, scalar2=None, op0=op0, **kw), reads, writes)
        else:
            self.P.op(eng, lambda e: e.tensor_scalar(out=out, in0=in0, scalar1=s1, scalar2=s2, op0=op0, op1=op1, **kw), reads, writes)

    def stt(self, eng, out, in0, scalar, in1, op0, op1, reads, writes, **kw):
        self.P.op(eng, lambda e: e.scalar_tensor_tensor(out=out, in0=in0, scalar=scalar, in1=in1, op0=op0, op1=op1, **kw), reads, writes)

    def mmr2(self, out, lhsT, rhs, start, stop, reads, writes):
        return self.mmr(out, lhsT, rhs, start, stop, reads, writes)

    def cp(self, eng, out, in_, reads, writes):
        if eng == "act":
            self.P.op("act", lambda e: e.activation(out=out, in_=in_, func=AF.Copy), reads, writes)
        else:
            self.P.op(eng, lambda e: e.tensor_copy(out=out, in_=in_), reads, writes)

    def memset(self, eng, ap, val, writes):
        self.P.op(eng, lambda e: e.memset(ap, val), (), writes)

    def prologue(self):
        P, d = self.P, self.d
        P.dma(self.CF[:], d["cf"], writes=[self.RCF])
        for i, lo in enumerate([0, 128, 384, 512]):
            P.dma(self.CB[:, i, :], d["cf"][:, lo:lo + 128], writes=[self.RCB], eng="pool")
        xr = d["x"].rearrange("(t p) d -> p t d", p=128)
        for t in range(NT):
            P.dma(self.X[:, t, :], xr[:, t, :], writes=[self.RX[t]])
        import os
        skip = os.environ.get("PRO_SKIP", "")
        if "rope" in skip:
            if "xt" not in skip:
                self.make_xt(range(NT))
            return
        A = self.AR
        base = 32768
        posi, (Rp,) = A.carve(base, [SEQ], I32)
        r, (Rr,) = A.carve(base + 8192, [SEQ], F32)
        r2, (Rr2,) = A.carve(base + 16384, [SEQ], F32)
        ki, (Rki,) = A.carve(base + 24576, [SEQ], I32)
        kf, (Rkf,) = A.carve(base + 32768, [SEQ], F32)
        mk, (Rmk,) = A.carve(base + 40960, [SEQ], F32)
        P.dma(posi, d["pos"].broadcast_to([128, SEQ]), writes=[Rp])
        if "s1" in skip:
            self.make_xt(range(NT)); return
        self.cp("dve", r, posi, [Rp], [Rr])
        self.ts("dve", r, r, self.invf, None, ALU.mult, None, [Rr, self.RCF], [Rr])
        if "s2" in skip:
            self.make_xt(range(NT)); return
        for tab, shift in ((self.sinT, 0.0), (self.cosT, 0.25)):
            self.ts("dve", r2, r, shift, None, ALU.add, None, [Rr], [Rr2])
            self.cp("dve", ki, r2, [Rr2], [Rki])
            self.cp("dve", kf, ki, [Rki], [Rkf])
            self.tt("dve", r2, r2, kf, ALU.subtract, [Rr2, Rkf], [Rr2])
            self.ts("dve", mk, r2, 0.5, None, ALU.is_gt, None, [Rr2], [Rmk])
            self.tt("dve", r2, r2, mk, ALU.subtract, [Rr2, Rmk], [Rr2])
            self.ts("dve", mk, r2, -0.5, None, ALU.is_lt, None, [Rr2], [Rmk])
            self.tt("dve", r2, r2, mk, ALU.add, [Rr2, Rmk], [Rr2])
            if "s3" in skip:
                if "dumpr2" in skip:
                    k0 = 0 if shift == 0.0 else 2
                    self.cp("dve", self.X[:, k0, :], r2[:, 0:1024], [Rr2], [self.RX[k0]])
                    self.cp("dve", self.X[:, k0 + 1, :], r2[:, 1024:2048], [Rr2], [self.RX[k0 + 1]])
                continue
            self.act(tab[:], r2, AF.Sin, [Rr2], [self.RTAB], scale=TWO_PI * (1.0 - 1e-6))
        self.make_xt(range(NT))

    def make_xt(self, tiles):
        A = self.AR
        xb2, Rxb = A.carve(32768 + 49152, [2, DM], BF16, nres=2)
        for t in tiles:
            s = t % 2
            self.cp("act", xb2[:, s, :], self.X[:, t, :], [self.RX[t]], [Rxb[s]])
            b = self.bank("tp", [6, 7])
            pv = self.psb16(b)
            for kc in range(NKC):
                self.tr(pv[:, kc * 128:(kc + 1) * 128], xb2[:, s, kc * 128:(kc + 1) * 128], self.identb,
                        [Rxb[s], self.RCB], [self.RPS[b]])
            self.cp("dve", self.XT[:, :, t * 128:(t + 1) * 128], pv.rearrange("p (c n) -> p c n", c=NKC),
                    [], [self.RPS[b], self.RXT[t]])

    def dump_featmajor_bf16(self, ap3, res_list):
        C = ap3.shape[1]
        dst = self.d["dbg"].rearrange("(a b) d -> a (b d)", b=2)
        dst = dst.rearrange("(c p) t -> p c t", p=128)
        self.P.dma(dst[:, 0:C, :], ap3, reads=res_list, eng="pool", is_output=True)

    def dump_X(self, name="dbg"):
        yr = self.d[name].rearrange("(t p) d -> p t d", p=128)
        for t in range(NT):
            self.P.dma(yr[:, t, :], self.X[:, t, :], reads=[self.RX[t]], is_output=True)

    def layer_params(self, l):
        P, d, SM = self.P, self.d, self.SM
        Rsm = self.Rsm
        lam_init = 0.8 - 0.6 * float(np.exp(-0.3 * l))
        self.lam_init = lam_init
        for i, k in enumerate(("lam_q1", "lam_k1", "lam_q2", "lam_k2")):
            P.dma(SM[:, i * 64:(i + 1) * 64], d[k][l:l + 1, :].broadcast_to([128, 64]), writes=[Rsm])
        P.dma(SM[:, 256:384], d["subln_w"][l:l + 1, :].broadcast_to([128, 128]), writes=[Rsm])
        P.dma(SM[:, 384:512], d["gdn_norm_w"][l:l + 1, :].broadcast_to([128, 128]), writes=[Rsm])
        P.dma(SM[:, 512:516], d["dt_bias"][l:l + 1, :].broadcast_to([128, 4]), writes=[Rsm])
        P.dma(SM[:, 516:520], d["a_log"][l:l + 1, :].broadcast_to([128, 4]), writes=[Rsm])
        self.stt("dve", SM[:, 960:1024], SM[:, 0:64], 1.0, SM[:, 64:128], ALU.mult, ALU.mult, [Rsm], [Rsm], accum_out=SM[:, 520:521])
        self.stt("dve", SM[:, 960:1024], SM[:, 128:192], 1.0, SM[:, 192:256], ALU.mult, ALU.mult, [Rsm], [Rsm], accum_out=SM[:, 521:522])
        self.act(SM[:, 522:524], SM[:, 520:522], AF.Exp, [Rsm], [Rsm])
        self.tt("dve", SM[:, 520:521], SM[:, 522:523], SM[:, 523:524], ALU.subtract, [Rsm], [Rsm])
        self.ts("dve", SM[:, 524:525], SM[:, 520:521], -1.0, -lam_init, ALU.mult, ALU.add, [Rsm], [Rsm])
        self.neglam = SM[:, 524:525]
        self.ts("dve", SM[:, 256:384], SM[:, 256:384], 1.0 - lam_init, None, ALU.mult, None, [Rsm], [Rsm])
        self.WSUB = SM[:, 256:384]
        self.GNW = SM[:, 384:512]
        self.act(SM[:, 516:520], SM[:, 516:520], AF.Exp, [Rsm], [Rsm])
        self.ts("dve", SM[:, 516:520], SM[:, 516:520], -1.0, None, ALU.mult, None, [Rsm], [Rsm])

    def layer_norm_all(self, gname, bname, l, lnoff, need_xt=True):
        P, d, A = self.P, self.d, self.AR
        LNG, (Rg,) = A.carve(lnoff, [DM], F32)
        LNB, (Rb,) = A.carve(lnoff + 4096, [DM], F32)
        junk, (Rj,) = A.carve(lnoff + 8192, [DM], F32)
        ST, (Rs,) = A.carve(lnoff + 12288, [128], F32)
        junk2, (Rj2,) = A.carve(lnoff + 12800, [DM], F32)
        P.dma(LNG, d[gname][l:l + 1, :].broadcast_to([128, DM]), writes=[Rg])
        P.dma(LNB, d[bname][l:l + 1, :].broadcast_to([128, DM]), writes=[Rb])
        X, RX = self.X, self.RX
        for t in range(NT):
            self.act(junk, X[:, t, :], AF.Identity, [RX[t]], [Rj, Rs], accum_out=ST[:, t:t + 1])
            self.stt("dve", junk2, X[:, t, :], 1.0, X[:, t, :], ALU.mult, ALU.mult, [RX[t]], [Rj2, Rs], accum_out=ST[:, 16 + t:17 + t])
        self.ts("dve", ST[:, 0:32], ST[:, 0:32], 1.0 / DM, None, ALU.mult, None, [Rs], [Rs])
        self.tt("dve", ST[:, 32:48], ST[:, 0:16], ST[:, 0:16], ALU.mult, [Rs], [Rs])
        self.tt("dve", ST[:, 48:64], ST[:, 16:32], ST[:, 32:48], ALU.subtract, [Rs], [Rs])
        self.act(ST[:, 64:80], ST[:, 48:64], AF.Ln, [Rs], [Rs], bias=LN_EPS)
        self.act(ST[:, 80:96], ST[:, 64:80], AF.Exp, [Rs], [Rs], scale=-0.5)
        self.stt("dve", ST[:, 96:112], ST[:, 0:16], -1.0, ST[:, 80:96], ALU.mult, ALU.mult, [Rs], [Rs])
        for t in range(NT):
            self.act(X[:, t, :], X[:, t, :], AF.Identity, [RX[t], Rs], [RX[t]], scale=ST[:, 80 + t:81 + t], bias=ST[:, 96 + t:97 + t])
            self.tt("dve", X[:, t, :], X[:, t, :], LNG, ALU.mult, [RX[t], Rg], [RX[t]])
            self.tt("pool", X[:, t, :], X[:, t, :], LNB, ALU.add, [RX[t], Rb], [RX[t]])
        if need_xt:
            self.make_xt(range(NT))

    def da_head(self, l, h):
        P, d, A = self.P, self.d, self.AR
        XT, RXT = self.XT, self.RXT
        base = 32768
        W2, RW2 = A.carve(base, [2, NKC, 384], BF16, nres=2) if h == 0 else (self._daW, self._daRW)
        self._daW, self._daRW = W2, RW2
        W, RW = W2[:, h % 2], RW2[h % 2]
        o = base + 12288
        qk, Rqk = A.carve(o, [2, SEQ], BF16, nres=2); o += 8192
        V, (RV,) = A.carve(o, [NT, 132], BF16); o += 4224
        rawb, Rraw = A.carve(o, [8, 512], BF16, nres=8); o += 8192
        t1, Rt1 = A.carve(o, [2, 512], F32, nres=2); o += 4096
        t2, Rt2 = A.carve(o, [2, 512], F32, nres=2); o += 4096
        sq, Rsq = A.carve(o, [2, 512], BF16, nres=2); o += 2048
        ET, RET = A.carve(o, [4, 256], BF16, nres=4); o += 2048
        ep_t, Rept = A.carve(o, [2, 128], F32, nres=2); o += 1024
        ep_o, Repo = A.carve(o, [2, 128], F32, nres=2); o += 1024
        ep_j, (Repj,) = A.carve(o, [128], F32); o += 512
        ep_n, Repn = A.carve(o, [2, 128], BF16, nres=2); o += 512
        st, (Rst, RnegM, Rst_a, Rst_b) = A.carve(o, [64], F32, nres=4); o += 256
        Rst2 = [Rst_a, Rst_b]
        U2, (RU2,) = A.carve(o, [2, 128], BF16); o += 512
        kz, (Rkz,) = A.carve(o, [NT, 2, 128], BF16); o += 8192
        if h == 0:
            self.memset("pool", kz[64:128, :, 0, :], 0.0, [Rkz])
            self.memset("pool", kz[0:64, :, 1, :], 0.0, [Rkz])
        win = d["w_in"][l].rearrange("(c p) n -> p c n", p=128)

        def load_w(hh):
            Wd_, RWd_ = W2[:, hh % 2], RW2[hh % 2]
            for i, c0 in enumerate((hh * 128, 512 + hh * 128, 1024 + hh * 128)):
                P.dma(Wd_[:, :, i * 128:(i + 1) * 128], win[:, :, c0:c0 + 128], writes=[RWd_], eng="pool")
        if h == 0:
            load_w(0)
        self.cp("pool", U2[:, 0, :], self.Ub, [self.RCB], [RU2])
        self.cp("pool", U2[:, 1, :], self.Ub, [self.RCB], [RU2])
        its = [(which, tg) for which in range(2) for tg in range(4)]
        for it, (which, tg) in enumerate(its):
            cols = slice(tg * 512, (tg + 1) * 512)
            b = self.bank("proj", [0, 1])
            ps = self.psf(b)
            for kc in range(NKC):
                self.mm(ps, W[:, kc, which * 128:(which + 1) * 128], XT[:, kc, cols], kc == 0, kc == NKC - 1,
                        [RW] + RXT[4 * tg:4 * tg + 4], [self.RPS[b]])
            self.cp("act", rawb[:, it, :], ps, [], [self.RPS[b], Rraw[it]])
        for it, (which, tg) in enumerate(its):
            s = it % 2
            cols = slice(tg * 512, (tg + 1) * 512)
            b2 = self.bank("rot", [2, 3])
            ps2 = self.psf(b2)
            self.mm(ps2, self.Rmb, rawb[:, it, :], True, True, [self.RCB, Rraw[it]], [self.RPS[b2]])
            self.tt("dve", t1[:, s, :], rawb[:, it, :], self.cosT[:, cols], ALU.mult, [self.RTAB, Rraw[it]], [Rt1[s]])
            self.tt("dve", t2[:, s, :], ps2, self.sinT[:, cols], ALU.mult, [self.RTAB], [self.RPS[b2], Rt2[s]])
            self.tt("dve", qk[:, which, cols], t1[:, s, :], t2[:, s, :], ALU.add, [Rt1[s], Rt2[s]], [Rqk[which]])
            self.act(sq[:, s, :], qk[:, which, cols], AF.Square, [Rqk[which]], [Rsq[s]])
            if which == 0:
                self.cp("act", kz[0:64, 4 * tg:4 * tg + 4, 0, :], qk[0:64, 0, cols].rearrange("p (a n) -> p a n", a=4), [Rqk[0]], [Rkz])
                self.cp("pool", kz[64:128, 4 * tg:4 * tg + 4, 1, :], qk[64:128, 0, cols].rearrange("p (a n) -> p a n", a=4), [Rqk[0]], [Rkz])
            b3 = self.bank("nrm", [4, 5])
            ps3 = self.psf(b3)
            self.mm(ps3, self.onesb, sq[:, s, :], True, True, [self.RCB, Rsq[s]], [self.RPS[b3]])
            self.P.op("dve", lambda e, o_=st[:, which * 4 + tg:which * 4 + tg + 1], i_=ps3: e.reduce_max(out=o_, in_=i_, axis=AX.X),
                      [], [self.RPS[b3], Rst])
        self.P.op("dve", lambda e: e.reduce_max(out=st[:, 8:9], in_=st[:, 0:4], axis=AX.X), [Rst], [Rst])
        self.P.op("dve", lambda e: e.reduce_max(out=st[:, 9:10], in_=st[:, 4:8], axis=AX.X), [Rst], [Rst])
        self.tt("dve", st[:, 10:11], st[:, 8:9], st[:, 9:10], ALU.mult, [Rst], [Rst])
        self.act(st[:, 11:12], st[:, 10:11], AF.Ln, [Rst], [Rst], bias=1e-30)
        self.act(st[:, 12:13], st[:, 11:12], AF.Exp, [Rst], [Rst], scale=0.5)
        self.ts("dve", st[:, 13:14], st[:, 12:13], -1.05 / 8.0, None, ALU.mult, None, [Rst], [RnegM])
        negM = st[:, 13:14]
        self.memset("pool", V[:, :, 128:129], 1.0, [RV])
        VT, RVT = rawb, Rraw
        for tg in range(4):
            b = self.bank("proj", [0, 1])
            ps = self.psf(b)
            for kc in range(NKC):
                self.mm(ps, W[:, kc, 256:384], XT[:, kc, tg * 512:(tg + 1) * 512], kc == 0, kc == NKC - 1,
                        [RW] + RXT[4 * tg:4 * tg + 4], [self.RPS[b]])
            self.cp("act", VT[:, tg, :], ps, [], [self.RPS[b], RVT[tg]])
            b2 = self.bank("rot", [2, 3])
            pv = self.psb16(b2)
            for ti in range(4):
                self.tr(pv[:, ti * 128:(ti + 1) * 128], VT[:, tg, ti * 128:(ti + 1) * 128], self.identb, [RVT[tg], self.RCB], [self.RPS[b2]])
            self.cp("dve", V[:, 4 * tg:4 * tg + 4, 0:128], pv[:, 0:512].rearrange("p (a n) -> p a n", a=4), [], [self.RPS[b2], RV])
        if h + 1 < 4:
            load_w(h + 1)
        qT, kT = qk[:, 0, :], qk[:, 1, :]
        pairs = [(i, j) for i in range(NT) for j in range(i + 1)]
        info = {}

        def emit_st(n):
            i, j = pairs[n]
            b = self.bank("st", [0, 1, 6])
            ps = self.psf(b)
            self.mm(ps[:, 0:256], kT[:, j * 128:(j + 1) * 128], kz[:, i, :, :].rearrange("p c n -> p (c n)"), True, True,
                    [Rqk[1], Rkz], [self.RPS[b]])
            e = n % 4
            self.act(ET[:, e, :], ps[:, 0:256], AF.Exp, [RnegM], [self.RPS[b], RET[e]], scale=0.125, bias=negM)
            if i == j:
                self.tt("pool", ET[:, e, :], ET[:, e, :], U2.rearrange("p a n -> p (a n)"), ALU.mult, [RET[e], RU2], [RET[e]])

        def emit_pv(n):
            i, j = pairs[n]
            e = n % 4
            for c in range(2):
                b = [2, 3, 4, 5][2 * c + (i % 2)]
                self.mm(self.psf(b)[:, 0:129], ET[:, e, c * 128:(c + 1) * 128], V[:, j, 0:129], j == 0, j == i,
                        [RET[e], RV], [self.RPS[b]])

        def epilogue(i):
            s = i % 2
            b0, b1 = [2, 3][s], [4, 5][s]
            O0, O1 = self.psf(b0), self.psf(b1)
            Rst = Rst2[s]
            c = 16 + 4 * s
            self.P.op("dve", lambda e: e.reciprocal(out=st[:, c:c + 1], in_=O0[:, 128:129]), [], [self.RPS[b0], Rst])
            self.P.op("dve", lambda e: e.reciprocal(out=st[:, c + 1:c + 2], in_=O1[:, 128:129]), [], [self.RPS[b1], Rst])
            self.tt("dve", st[:, c + 2:c + 3], st[:, c + 1:c + 2], self.neglam, ALU.mult, [Rst, self.Rsm], [Rst])
            self.ts("dve", ep_t[:, s, :], O1[:, 0:128], st[:, c + 2:c + 3], None, ALU.mult, None, [Rst], [self.RPS[b1], Rept[s]])
            self.stt("dve", ep_o[:, s, :], O0[:, 0:128], st[:, c:c + 1], ep_t[:, s, :], ALU.mult, ALU.add, [Rst, Rept[s]], [self.RPS[b0], Repo[s]])
            self.stt("dve", ep_j, ep_o[:, s, :], 1.0, ep_o[:, s, :], ALU.mult, ALU.mult, [Repo[s]], [Repj, Rst], accum_out=st[:, c + 3:c + 4])
            self.act(st[:, 24 + s:25 + s], st[:, c + 3:c + 4], AF.Ln, [Rst], [Rst], scale=1.0 / 128.0, bias=RMS_EPS)
            self.act(st[:, 26 + s:27 + s], st[:, 24 + s:25 + s], AF.Exp, [Rst], [Rst], scale=-0.5)
            self.stt("dve", ep_n[:, s, :], ep_o[:, s, :], st[:, 26 + s:27 + s], self.WSUB, ALU.mult, ALU.mult, [Repo[s], Rst, self.Rsm], [Repn[s]])
            bt = 7
            pv = self.psb16(bt)
            self.tr(pv[:, 0:128], ep_n[:, s, :], self.identb, [Repn[s], self.RCB], [self.RPS[bt]])
            self.cp("dve", self.MT[:, h, i * 128:(i + 1) * 128], pv[:, 0:128], [], [self.RPS[bt], self.RMT[h][i]])

        emit_st(0)
        emit_st(1)
        pending = []
        for n in range(len(pairs)):
            if n + 2 < len(pairs):
                emit_st(n + 2)
            i, j = pairs[n]
            while pending and pending[0][1] <= i - 2:
                epilogue(pending.pop(0)[1])
            emit_pv(n)
            if i == j:
                pending.append((n + 3, i))
            while pending and pending[0][0] <= n:
                epilogue(pending.pop(0)[1])
        while pending:
            epilogue(pending.pop(0)[1])

    def gdn_prep(self, l):
        P, d, A, SM = self.P, self.d, self.AR, self.SM
        XT, RXT = self.XT, self.RXT
        Rsm = self.Rsm
        base = 32768
        WAB, (Rwab,) = A.carve(base, [NKC, 8], BF16)
        GS, (Rgs,) = A.carve(base + 1024, [6, 64], F32)
        CWR, (Rcwr,) = A.carve(base + 4096, [1536], F32)
        CW, (Rcw,) = A.carve(base + 4096 + 6144, [12, 4], F32)
        self.GS, self.Rgs, self.CW, self.Rcw = GS, Rgs, CW, Rcw
        win = d["w_in"][l].rearrange("(c p) n -> p c n", p=128)
        P.dma(WAB, win[:, :, 3584:3592], writes=[Rwab], eng="pool")
        P.dma(CWR[0:4, :], d["conv_w"][l], writes=[Rcwr])
        b = self.bank("misc", [6, 7])
        ps = self.psf(b)
        for t in range(NT):
            for kc in range(NKC):
                self.mm(ps[:, t * 8:(t + 1) * 8], XT[:, kc, t * 128:(t + 1) * 128], WAB[:, kc, :], kc == 0, kc == NKC - 1,
                        [Rwab, RXT[t]], [self.RPS[b]])
        AB = SM[:, 640:768]
        self.cp("dve", AB, ps[:, 0:128], [], [self.RPS[b], Rsm])
        AB3 = AB.rearrange("p (t e) -> p t e", e=8)
        g3 = SM[:, 768:832].rearrange("p (t h) -> p t h", h=4)
        be3 = SM[:, 832:896].rearrange("p (t h) -> p t h", h=4)
        nb3 = SM[:, 896:960].rearrange("p (t h) -> p t h", h=4)
        tmp = SM[:, 960:976]
        for h in range(4):
            self.ts("dve", tmp, AB3[:, :, h], SM[:, 512 + h:513 + h], None, ALU.add, None, [Rsm], [Rsm])
            self.act(tmp, tmp, AF.Exp, [Rsm], [Rsm])
            self.act(tmp, tmp, AF.Ln, [Rsm], [Rsm], bias=1.0)
            self.ts("dve", g3[:, :, h], tmp, SM[:, 516 + h:517 + h], None, ALU.mult, None, [Rsm], [Rsm])
        self.act(SM[:, 832:896].rearrange("p (t h) -> p t h", h=4), AB3[:, :, 4:8], AF.Sigmoid, [Rsm], [Rsm])
        self.ts("dve", SM[:, 896:960], SM[:, 832:896], -1.0, None, ALU.mult, None, [Rsm], [Rsm])
        self.g3, self.be3, self.nb3 = g3, be3, nb3
        b = self.bank("misc", [6, 7])
        ps = self.psf(b)
        self.mm(ps[:, 0:64], self.Uf, SM[:, 768:832], True, True, [self.RCF, Rsm], [self.RPS[b]])
        self.mm(ps[:, 64:128], self.onesf, SM[:, 768:832], True, True, [self.RCF, Rsm], [self.RPS[b]])
        self.cp("dve", GS[:, 0, :], ps[:, 0:64], [], [self.RPS[b], Rgs])
        self.act(GS[:, 1, :], ps[:, 0:64], AF.Exp, [], [self.RPS[b], Rgs])
        self.act(GS[:, 3, :], ps[:, 64:128], AF.Exp, [], [self.RPS[b], Rgs])
        self.tt("dve", GS[:, 4, :], ps[:, 64:128], GS[:, 0, :], ALU.subtract, [Rgs], [self.RPS[b], Rgs])
        self.act(GS[:, 2, :], GS[:, 4, :], AF.Exp, [Rgs], [Rgs])
        b = self.bank("misc", [6, 7])
        ps = self.psf(b)
        for c in range(12):
            self.mm(ps[:, c * 4:(c + 1) * 4], CWR[0:4, c * 128:(c + 1) * 128], self.identf[0:4, 0:4], True, True,
                    [Rcwr, self.RCF], [self.RPS[b]])
        self.cp("dve", CW, ps[:, 0:48].rearrange("p (c j) -> p c j", j=4), [], [self.RPS[b], Rcw])

    def gdn_head(self, l, h):
        P, d, A, SM = self.P, self.d, self.AR, self.SM
        XT, RXT = self.XT, self.RXT
        Rsm, GS, Rgs = self.Rsm, self.GS, self.Rgs
        o = 43264
        WG, (RWG,) = A.carve(o, [NKC, 512], BF16); o += 8192
        qkv, Rqkv = A.carve(o, [3, SEQ], BF16, nres=3); o += 12288
        SZ, (RSZ,) = A.carve(o, [NT, 128], BF16); o += 4096
        area = o
        Raw, (RRaw,) = A.carve(area, [2052], F32)
        acc, (Racc,) = A.carve(area + 8208, [SEQ], F32)
        sqb, Rsqb = A.carve(area + 16400, [2, 512], BF16, nres=2)
        rn, (Rrn,) = A.carve(area + 18448, [512], F32)
        win = d["w_in"][l].rearrange("(c p) n -> p c n", p=128)
        for i, c0 in enumerate((1536, 2048, 2560, 3072)):
            P.dma(WG[:, :, i * 128:(i + 1) * 128], win[:, :, c0 + h * 128:c0 + (h + 1) * 128], writes=[RWG], eng="pool")
        self.memset("pool", Raw[:, 0:3], 0.0, [RRaw])
        cnt = 0
        for which in range(3):
            for tg in range(4):
                b = self.bank("g", list(range(8)))
                ps = self.psf(b)
                for kc in range(NKC):
                    self.mm(ps, WG[:, kc, which * 128:(which + 1) * 128], XT[:, kc, tg * 512:(tg + 1) * 512], kc == 0, kc == NKC - 1,
                            [RWG] + RXT[4 * tg:4 * tg + 4], [self.RPS[b]])
                self.cp("act", Raw[:, 3 + tg * 512:3 + (tg + 1) * 512], ps, [], [self.RPS[b], RRaw])
            cw = self.CW[:, which * 4 + h, :]
            self.ts("dve", acc, Raw[:, 0:SEQ], cw[:, 0:1], None, ALU.mult, None, [RRaw, self.Rcw], [Racc])
            for j in range(1, 4):
                self.stt("dve", acc, Raw[:, j:j + SEQ], cw[:, j:j + 1], acc, ALU.mult, ALU.add, [RRaw, self.Rcw, Racc], [Racc])
            if which == 2:
                self.act(qkv[:, 2, :], acc, AF.Silu, [Racc], [Rqkv[2]])
            else:
                self.act(acc, acc, AF.Silu, [Racc], [Racc])
                for tg in range(4):
                    s = cnt % 2
                    cnt += 1
                    cols = slice(tg * 512, (tg + 1) * 512)
                    self.tt("pool", sqb[:, s, :], acc[:, cols], acc[:, cols], ALU.mult, [Racc], [Rsqb[s]])
                    b = self.bank("g", list(range(8)))
                    ps = self.psf(b)
                    self.mm(ps, self.onesb, sqb[:, s, :], True, True, [self.RCB, Rsqb[s]], [self.RPS[b]])
                    self.act(rn, ps, AF.Ln, [], [self.RPS[b], Rrn], bias=RMS_EPS)
                    self.act(rn, rn, AF.Exp, [Rrn], [Rrn], scale=-0.5, bias=(-0.5 * float(np.log(128.0)) if which == 0 else 0.0))
                    self.tt("dve", qkv[:, which, cols], acc[:, cols], rn, ALU.mult, [Racc, Rrn], [Rqkv[which]])
        SZT = SZ.rearrange("p a n -> p (a n)")
        for tg in range(4):
            b = self.bank("g", list(range(8)))
            ps = self.psf(b)
            for kc in range(NKC):
                self.mm(ps, WG[:, kc, 384:512], XT[:, kc, tg * 512:(tg + 1) * 512], kc == 0, kc == NKC - 1,
                        [RWG] + RXT[4 * tg:4 * tg + 4], [self.RPS[b]])
            self.act(SZT[:, tg * 512:(tg + 1) * 512], ps, AF.Silu, [], [self.RPS[b], RSZ])
        mats, Rm0 = A.carve(area, [16, 128], F32, nres=16)
        bm, Rb = A.carve(area + 16 * 512, [20, 128], BF16, nres=20)
        def _loc(i):
            if i < 26:
                bp, k = divmod(i, 13)
                if k < 5:
                    return ("a", bp * 5 + k)
                return ("r", bp * 8 + (k - 5))
            return ("a", 10 + (i - 26))

        class _RmProxy:
            def __getitem__(_s, i):
                kind, j = _loc(i)
                return Rm0[j] if kind == "a" else self.RRr[j]
        Rm = _RmProxy()

        def M(i):
            kind, j = _loc(i)
            return mats[:, j, :] if kind == "a" else self.RR[:, j, :]

        def MR(i):
            kind, j = _loc(i)
            assert kind == "r"
            return self.RR[:, j, :].bitcast(F32R) if USE_F32R else self.RR[:, j, :]
        B = lambda i: bm[:, i, :]
        qT, kT, vT = qkv[:, 0, :], qkv[:, 1, :], qkv[:, 2, :]
        S_i = [17, 18]
        VN, OG = 16, 19
        JK, ON = 30, 31
        og = B(OG)
        self.memset("pool", B(S_i[0]), 0.0, [Rb[S_i[0]]])
        Uf, Usf, identf, onesf = self.Uf, self.Usf, self.identf, self.onesf
        RCF = self.RCF
        g3, be3, nb3 = self.g3, self.be3, self.nb3
        gs = lambda k, n: GS[:, k, n * 4 + h:n * 4 + h + 1]
        busy = set()
        rr = [0]

        def newbank():
            for _ in range(8):
                b = rr[0] % 8
                rr[0] += 1
                if b not in busy:
                    busy.add(b)
                    return b
            raise RuntimeError("no free PSUM bank")

        def rel(b):
            busy.discard(b)

        def pre_steps(n, bpos, par):
            T = lambda k: bpos * 13 + k
            Pb = lambda k: par * 8 + bpos * 4 + k
            Pu = 26 + par * 2 + bpos
            cs = slice(n * 128, (n + 1) * 128)
            beta = be3[:, n, h:h + 1]
            st = {}
            steps = []

            def s0():
                self.act(M(T(0)), onesf, AF.Copy, [RCF, Rsm], [Rm[T(0)]], scale=g3[:, n, h:h + 1])
                b = newbank(); st["G"] = b
                self.mm(self.psf(b)[:, 0:128], M(T(0)), Uf, True, True, [Rm[T(0)], RCF], [self.RPS[b]])
                b = newbank(); st["A"] = b
                self.mm(self.psf(b)[:, 0:128], kT[:, cs], kT[:, cs], True, True, [Rqkv[1]], [self.RPS[b]])
                self.mm(self.psf(b)[:, 128:256], kT[:, cs], qT[:, cs], True, True, [Rqkv[0], Rqkv[1]], [self.RPS[b]])
                b = newbank(); st["KV"] = b
                pv = self.psb16(b)
                self.tr(pv[:, 0:128], kT[:, cs], self.identb, [Rqkv[1], self.RCB], [self.RPS[b]])
                self.tr(pv[:, 128:256], vT[:, cs], self.identb, [Rqkv[2], self.RCB], [self.RPS[b]])
            steps.append(s0)

            def s1():
                b = st["G"]
                self.ts("dve", M(T(1)), self.psf(b)[:, 0:128], gs(0, n), 0.0, ALU.subtract, ALU.min, [Rgs], [self.RPS[b], Rm[T(1)]])
                self.act(M(T(4)), self.psf(b)[:, 0:128], AF.Exp, [], [self.RPS[b], Rm[T(4)]])
                self.act(M(T(1)), M(T(1)), AF.Exp, [Rm[T(1)]], [Rm[T(1)]])
                rel(b)
                b = st["KV"]
                pv = self.psb16(b)
                self.act(B(Pb(2)), pv[:, 0:128], AF.Copy, [Rgs], [self.RPS[b], Rb[Pb(2)]], scale=gs(2, n))
            steps.append(s1)

            def s2():
                self.tt("pool", M(T(2)), M(T(1)), Uf, ALU.mult, [Rm[T(1)], RCF], [Rm[T(2)]])
                self.tt("pool", M(T(3)), M(T(1)), Usf, ALU.mult, [Rm[T(1)], RCF], [Rm[T(3)]])
                self.tt("pool", B(Pb(0)), qT[:, cs], M(T(4)), ALU.mult, [Rqkv[0], Rm[T(4)]], [Rb[Pb(0)]])
            steps.append(s2)

            def s3():
                b = st["A"]
                self.stt("dve", MR(T(5)), self.psf(b)[:, 0:128], beta, M(T(3)), ALU.mult, ALU.mult, [Rsm, Rm[T(3)]], [self.RPS[b], Rm[T(5)]])
                self.tt("dve", B(Pb(1)), self.psf(b)[:, 128:256], M(T(2)), ALU.mult, [Rm[T(2)]], [self.RPS[b], Rb[Pb(1)]])
                rel(b)
            steps.append(s3)

            def s4():
                b = newbank(); st["X"] = b
                self.tr(self.psf(b)[:, 0:128], M(T(5)), identf, [Rm[T(5)], RCF], [self.RPS[b]])
                self.tt("pool", MR(T(9)), identf, M(T(5)), ALU.subtract, [RCF, Rm[T(5)]], [Rm[T(9)]])
            steps.append(s4)

            def s5():
                b = st["X"]
                self.cp("act", MR(T(6)), self.psf(b)[:, 0:128], [], [self.RPS[b], Rm[T(6)]])
                rel(b)
                b = st["KV"]
                pv = self.psb16(b)
                self.ts("dve", MR(T(11)), pv[:, 0:128], gs(1, n), None, ALU.mult, None, [Rgs], [self.RPS[b], Rm[T(11)]])
                self.cp("act", MR(T(12)), pv[:, 128:256], [], [self.RPS[b], Rm[T(12)]])
                rel(b)
            steps.append(s5)
            zs = {0: T(5)}
            zts = {0: T(6)}
            ns = {0: T(9)}
            for j in range(1, 7):
                zs[j] = T(7) if j % 2 == 1 else T(5)
                zts[j] = T(8) if j % 2 == 1 else T(6)
                ns[j] = T(10) if j % 2 == 1 else T(9)
            KE, VC = T(11), T(12)

            def lvl_a(j):
                def f():
                    b = newbank(); st["Z%d" % j] = b
                    self.mm(self.psf(b)[:, 0:128], MR(zs[j - 1]), MR(zts[j - 1]), True, True, [Rm[zs[j - 1]], Rm[zts[j - 1]]], [self.RPS[b]])
                    if j < 6:
                        self.mm(self.psf(b)[:, 128:256], MR(zts[j - 1]), MR(zs[j - 1]), True, True, [Rm[zs[j - 1]], Rm[zts[j - 1]]], [self.RPS[b]])
                return f

            def lvl_b(j):
                def f():
                    b = st["Z%d" % j]
                    self.cp("act", MR(zts[j]), self.psf(b)[:, 0:128], [], [self.RPS[b], Rm[zts[j]]])
                    if j < 6:
                        self.cp("dve", MR(zs[j]), self.psf(b)[:, 128:256], [], [self.RPS[b], Rm[zs[j]]])
                    rel(b)
                return f

            def lvl_c(j):
                def f():
                    b = newbank(); st["N%d" % j] = b
                    self.mm(self.psf(b)[:, 0:128], MR(zts[j]), MR(ns[j - 1]), True, True, [Rm[zts[j]], Rm[ns[j - 1]]], [self.RPS[b]])
                return f

            def lvl_d(j):
                def f():
                    b = st["N%d" % j]
                    self.tt("dve", MR(ns[j]), self.psf(b)[:, 0:128], M(ns[j - 1]), ALU.add, [Rm[ns[j - 1]]], [self.RPS[b], Rm[ns[j]]])
                    rel(b)
                return f

            def both(f1, f2):
                def f():
                    f1()
                    if f2 is not None:
                        f2()
                return f
            steps += [lvl_a(1), lvl_b(1)]
            for j in range(1, 7):
                steps += [both(lvl_c(j), lvl_a(j + 1) if j < 6 else None), both(lvl_d(j), lvl_b(j + 1) if j < 6 else None)]
            NTi = ns[6]

            def s8():
                b = newbank(); st["WU"] = b
                self.mm(self.psf(b)[:, 0:128], MR(KE), MR(NTi), True, True, [Rm[KE], Rm[NTi]], [self.RPS[b]])
                self.mm(self.psf(b)[:, 128:256], MR(NTi), MR(VC), True, True, [Rm[VC], Rm[NTi]], [self.RPS[b]])
            steps.append(s8)

            def s9():
                b = st["WU"]
                self.cp("act", B(Pb(3)), self.psf(b)[:, 0:128], [], [self.RPS[b], Rb[Pb(3)]])
                self.ts("dve", M(Pu), self.psf(b)[:, 128:256], beta, None, ALU.mult, None, [Rsm], [self.RPS[b], Rm[Pu]])
                rel(b)
            steps.append(s9)
            return steps

        def scan_steps(n, bpos, par):
            Pb = lambda k: par * 8 + bpos * 4 + k
            Pu = 26 + par * 2 + bpos
            cs = slice(n * 128, (n + 1) * 128)
            Sc, Sn = S_i[n % 2], S_i[(n + 1) % 2]
            st = {}
            steps = []

            def a0():
                b = newbank(); st["1"] = b
                self.mm(self.psf(b)[:, 0:128], B(Pb(3)), B(Sc), True, True, [Rb[Pb(3)], Rb[Sc]], [self.RPS[b]])
            steps.append(a0)

            def a1():
                b = st["1"]
                self.stt("dve", B(VN), self.psf(b)[:, 0:128], nb3[:, n, h:h + 1], M(Pu), ALU.mult, ALU.add, [Rsm, Rm[Pu]], [self.RPS[b], Rb[VN]])
                rel(b)
            steps.append(a1)

            def a2():
                b = newbank(); st["O"] = b
                self.mm(self.psf(b)[:, 0:128], B(Pb(0)), B(Sc), True, False, [Rb[Pb(0)], Rb[Sc]], [self.RPS[b]])
                self.mm(self.psf(b)[:, 0:128], B(Pb(1)), B(VN), False, True, [Rb[Pb(1)], Rb[VN]], [self.RPS[b]])
                b = newbank(); st["S"] = b
                self.mm(self.psf(b)[:, 0:128], B(Pb(2)), B(VN), True, True, [Rb[Pb(2)], Rb[VN]], [self.RPS[b]])
            steps.append(a2)

            def a3():
                b = st["S"]
                self.stt("dve", B(Sn), B(Sc), gs(3, n), self.psf(b)[:, 0:128], ALU.mult, ALU.add, [Rb[Sc], Rgs], [self.RPS[b], Rb[Sn]])
                rel(b)
                b = st["O"]
                self.act(M(JK), self.psf(b)[:, 0:128], AF.Square, [], [self.RPS[b], Rm[JK], Rm[ON]], accum_out=M(ON)[:, 0:1])
            steps.append(a3)

            def a4():
                self.act(M(ON)[:, 1:2], M(ON)[:, 0:1], AF.Ln, [Rm[ON]], [Rm[ON]], scale=1.0 / 128.0, bias=RMS_EPS)
                self.act(M(ON)[:, 2:3], M(ON)[:, 1:2], AF.Exp, [Rm[ON]], [Rm[ON]], scale=-0.5)
            steps.append(a4)

            def a5():
                b = st["O"]
                self.stt("dve", og, self.psf(b)[:, 0:128], M(ON)[:, 2:3], self.GNW, ALU.mult, ALU.mult, [Rm[ON], Rsm], [self.RPS[b], Rb[OG]])
                rel(b)
                b = newbank(); st["T"] = b
                self.tr(self.psb16(b)[:, 0:128], og, self.identb, [Rb[OG], self.RCB], [self.RPS[b]])
            steps.append(a5)

            def a6():
                b = st["T"]
                self.tt("dve", self.MT[:, 4 + h, cs], self.psb16(b)[:, 0:128], SZT[:, cs], ALU.mult, [RSZ], [self.RPS[b], self.RMT[4 + h][n]])
                rel(b)
            steps.append(a6)
            return steps

        def merged(step_lists):
            idx = [0] * len(step_lists)
            alive = True
            while alive:
                alive = False
                for k, sl in enumerate(step_lists):
                    if idx[k] < len(sl):
                        sl[idx[k]]()
                        idx[k] += 1
                        alive = True

        nb = NT // 2
        pre = lambda k: [pre_steps(2 * k, 0, k % 2), pre_steps(2 * k + 1, 1, k % 2)]
        merged(pre(0))
        for k in range(nb):
            lists = []
            if k + 1 < nb:
                lists += pre(k + 1)
            sc = scan_steps(2 * k, 0, k % 2) + scan_steps(2 * k + 1, 1, k % 2)
            lists.append(sc)
            merged(lists)

    def mixer(self, l):
        A = self.AR
        self.MT, R = A.carve(0, [8, SEQ], BF16, nres=8 * NT)
        self.RMT = [[R[k * NT + t] for t in range(NT)] for k in range(8)]
        self.layer_params(l)
        for h in range(4):
            self.da_head(l, h)
        if self.upto == "da":
            return
        self.gdn_prep(l)
        for h in range(4):
            self.gdn_head(l, h)

    def out_proj_ln1(self, l):
        P, d, A = self.P, self.d, self.AR
        Wo, (RWo,) = A.carve(32768, [NKC, DM], BF16)
        P.dma(Wo, d["w_out"][l].rearrange("(c p) n -> p c n", p=128), writes=[RWo], eng="pool")
        for t in range(NT):
            for hf in range(2):
                b = self.bank("o", [0, 1, 2, 3])
                ps = self.psf(b)
                for kc in range(NKC):
                    self.mm(ps, self.MT[:, kc, t * 128:(t + 1) * 128], Wo[:, kc, hf * 512:(hf + 1) * 512], kc == 0, kc == NKC - 1,
                            [self.RMT[kc][t], RWo], [self.RPS[b]])
                xs = self.X[:, t, hf * 512:(hf + 1) * 512]
                self.stt("dve", xs, xs, ALPHA, ps, ALU.mult, ALU.add, [self.RX[t]], [self.RPS[b], self.RX[t]])
        self.layer_norm_all("ln1_g", "ln1_b", l, 49152)

    def ffn_ln2(self, l, last):
        P, d, A = self.P, self.d, self.AR
        X, RX, XT, RXT = self.X, self.RX, self.XT, self.RXT
        moe = (l % 2 == 1)
        li = l // 2
        o = 0
        WS = []
        for s in range(2):
            wg, (Rwg,) = A.carve(o, [NKC, 512], BF16); o += 8192
            wu, (Rwu,) = A.carve(o, [NKC, 512], BF16); o += 8192
            wd, (Rwd,) = A.carve(o, [4, DM], BF16); o += 8192
            WS.append((wg, Rwg, wu, Rwu, wd, Rwd))
        hT, RhT = A.carve(o, [2, 4, 512], BF16, nres=2); o += 8192
        sg, Rsg = A.carve(o, [2, 512], BF16, nres=2); o += 2048
        lnoff = o; o += 16896
        GT, (RGT,) = A.carve(o, [NT, 8], F32); o += 512
        if moe:
            RWt, (RRW,) = A.carve(o, [NKC, 8], F32); o += 256
            lg, (Rlg,) = A.carve(o, [64], F32); o += 256
            assert o <= 81920
            XTf, RXTf = A.carve(lnoff, [2, NKC, 128], F32, nres=2)
            P.dma(RWt, d["router_w"][li].rearrange("(c p) e -> p c e", p=128), writes=[RRW])
            for t in range(NT):
                s = t % 2
                for half in range(2):
                    b = self.bank("rt", [4, 5, 6, 7])
                    ps = self.psf(b)
                    for q in range(4):
                        kc = half * 4 + q
                        self.tr(ps[:, q * 128:(q + 1) * 128], X[:, t, kc * 128:(kc + 1) * 128], self.identf, [RX[t], self.RCF], [self.RPS[b]])
                    self.cp("act" if half else "dve", XTf[:, s, half * 4:half * 4 + 4, :], ps.rearrange("p (a n) -> p a n", a=4),
                            [], [self.RPS[b], RXTf[s]])
                b = self.bank("rt", [4, 5, 6, 7])
                ps = self.psf(b)
                for kc in range(NKC):
                    self.mm(ps[:, 0:8], XTf[:, s, kc, :], RWt[:, kc, :], kc == 0, kc == NKC - 1, [RXTf[s], RRW], [self.RPS[b]])
                self.cp("dve", lg[:, 0:8], ps[:, 0:8], [], [self.RPS[b], Rlg])
                self.P.op("dve", lambda e: e.max(out=lg[:, 8:16], in_=lg[:, 0:8]), [Rlg], [Rlg])
                self.tt("dve", lg[:, 16:17], lg[:, 9:10], lg[:, 8:9], ALU.subtract, [Rlg], [Rlg])
                self.act(lg[:, 17:18], lg[:, 16:17], AF.Exp, [Rlg], [Rlg])
                self.ts("dve", lg[:, 18:19], lg[:, 17:18], 1.0, None, ALU.add, None, [Rlg], [Rlg])
                self.P.op("dve", lambda e: e.reciprocal(out=lg[:, 19:20], in_=lg[:, 18:19]), [Rlg], [Rlg])
                self.tt("dve", lg[:, 20:21], lg[:, 17:18], lg[:, 19:20], ALU.mult, [Rlg], [Rlg])
                self.ts("dve", lg[:, 24:32], lg[:, 0:8], lg[:, 8:9], lg[:, 19:20], ALU.is_equal, ALU.mult, [Rlg], [Rlg])
                self.ts("dve", lg[:, 32:40], lg[:, 0:8], lg[:, 9:10], lg[:, 20:21], ALU.is_equal, ALU.mult, [Rlg], [Rlg])
                self.tt("dve", GT[:, t, :], lg[:, 24:32], lg[:, 32:40], ALU.add, [Rlg], [RGT])
        for t in range(NT):
            self.P.op("act", lambda e, t=t: e.mul(out=X[:, t, :], in_=X[:, t, :], mul=ALPHA), [RX[t]], [RX[t]])
        if moe:
            groups = [(e, c0, 4) for e in range(NEXP) for c0 in range(0, FF_MOE, 512)]
        else:
            groups = [(None, c0, min(4, (FF_DENSE - c0) // 128)) for c0 in range(0, FF_DENSE, 512)]

        def load(gi):
            e, c0, nch = groups[gi]
            wg, Rwg, wu, Rwu, wd, Rwd = WS[gi % 2]
            if moe:
                srcg, srcu, srcd = d["moe_w_gate"][li, e], d["moe_w_up"][li, e], d["moe_w_down"][li, e]
            else:
                srcg, srcu, srcd = d["ffn_w_gate"][li], d["ffn_w_up"][li], d["ffn_w_down"][li]
            w = nch * 128
            P.dma(wg[:, :, 0:w], srcg.rearrange("(c p) n -> p c n", p=128)[:, :, c0:c0 + w], writes=[Rwg], eng="pool")
            P.dma(wu[:, :, 0:w], srcu.rearrange("(c p) n -> p c n", p=128)[:, :, c0:c0 + w], writes=[Rwu], eng="pool")
            P.dma(wd[:, 0:nch, :], srcd[c0:c0 + w, :].rearrange("(c p) n -> p c n", p=128), writes=[Rwd], eng="pool")

        items = [(gi, tg) for gi in range(len(groups)) for tg in range(4)]

        def up(k):
            gi, tg = items[k]
            e, c0, nch = groups[gi]
            wg, Rwg, wu, Rwu, wd, Rwd = WS[gi % 2]
            hs = k % 2
            cols = slice(tg * 512, (tg + 1) * 512)
            for c in range(nch):
                bg = self.bank("fg", [0, 1])
                bu = self.bank("fu", [2, 3])
                for kc in range(NKC):
                    self.mm(self.psf(bg), wg[:, kc, c * 128:(c + 1) * 128], XT[:, kc, cols], kc == 0, kc == NKC - 1,
                            [Rwg] + RXT[4 * tg:4 * tg + 4], [self.RPS[bg]])
                for kc in range(NKC):
                    self.mm(self.psf(bu), wu[:, kc, c * 128:(c + 1) * 128], XT[:, kc, cols], kc == 0, kc == NKC - 1,
                            [Rwu] + RXT[4 * tg:4 * tg + 4], [self.RPS[bu]])
                s = self.bank("sg", [0, 1])
                self.act(sg[:, s, :], self.psf(bg), AF.Silu, [], [self.RPS[bg], Rsg[s]])
                self.tt("dve", hT[:, hs, c, :], self.psf(bu), sg[:, s, :], ALU.mult, [Rsg[s]], [self.RPS[bu], RhT[hs]])

        def down(k):
            gi, tg = items[k]
            e, c0, nch = groups[gi]
            wg, Rwg, wu, Rwu, wd, Rwd = WS[gi % 2]
            hs = k % 2
            for ti in range(4):
                t = 4 * tg + ti
                for hf in range(2):
                    b = self.bank("fd", [4, 5, 6, 7])
                    ps = self.psf(b)
                    for c in range(nch):
                        self.mm(ps, hT[:, hs, c, ti * 128:(ti + 1) * 128], wd[:, c, hf * 512:(hf + 1) * 512], c == 0, c == nch - 1,
                                [RhT[hs], Rwd], [self.RPS[b]])
                    xs = X[:, t, hf * 512:(hf + 1) * 512]
                    if moe:
                        self.stt("dve", xs, ps, GT[:, t, e:e + 1], xs, ALU.mult, ALU.add, [RGT, RX[t]], [self.RPS[b], RX[t]])
                    else:
                        self.tt("dve", xs, ps, xs, ALU.add, [RX[t]], [self.RPS[b], RX[t]])

        load(0)
        for k in range(len(items)):
            gi, tg = items[k]
            up(k)
            if k > 0:
                down(k - 1)
            if tg == 0 and gi + 1 < len(groups):
                load(gi + 1)
        down(len(items) - 1)
        self.layer_norm_all("ln2_g", "ln2_b", l, lnoff, need_xt=not last)

    def layer(self, l):
        if self.upto == "pro":
            return
        self.mixer(l)
        if self.upto in ("da", "mix"):
            return
        self.out_proj_ln1(l)
        if self.upto == "ln1":
            return
        self.ffn_ln2(l, last=(l == self.n_layers - 1))

    def output(self):
        self.dump_X("y")

    def debug_dump(self):
        if self.upto == "pro":
            import os
            if "xt" in os.environ.get("PRO_SKIP", ""):
                self.dump_X("dbg")
            else:
                self.dump_featmajor_bf16(self.XT[:], self.RXT)
        elif self.upto == "da":
            self.dump_featmajor_bf16(self.MT[:, 0:4, :], [r for rl in self.RMT[0:4] for r in rl])
        elif self.upto == "mix":
            self.dump_featmajor_bf16(self.MT, [r for rl in self.RMT for r in rl])
        else:
            self.dump_X("dbg")


_CONST_CACHE = {}


def _consts():
    if "cf" in _CONST_CACHE:
        return _CONST_CACHE["cf"]
    cf = np.zeros((128, 648), np.float32)
    idx = np.arange(128)
    cf[:, 0:128] = np.eye(128, dtype=np.float32)
    cf[:, 128:256] = (idx[:, None] <= idx[None, :]).astype(np.float32)
    cf[:, 256:384] = (idx[:, None] < idx[None, :]).astype(np.float32)
    cf[:, 384:512] = 1.0
    rm = np.zeros((128, 128), np.float32)
    for blk in (0, 64):
        for dd in range(32):
            rm[blk + dd + 32, blk + dd] = -1.0
            rm[blk + dd, blk + dd + 32] = 1.0
    cf[:, 512:640] = rm
    inv_freq = 10000.0 ** (-np.arange(0, 64, 2, dtype=np.float32) / 64.0)
    cf[:, 640] = (inv_freq[idx % 32].astype(np.float64) / TWO_PI).astype(np.float32)
    _CONST_CACHE["cf"] = cf
    return cf


def build_program(n_layers=DEPTH, upto=None):
    nc = bass.Bass("TRN2", target_bir_lowering=False)
    with ExitStack() as st:
        P = Prog(nc, st)
        K = Kern(nc, P, st, n_layers, upto)
        K.prologue()
        for l in range(n_layers):
            K.layer(l)
        if upto is None:
            K.output()
        else:
            K.debug_dump()
        P.finish()
    return nc


WEIGHT_KEYS = ("w_in", "conv_w", "a_log", "dt_bias", "gdn_norm_w", "lam_q1", "lam_k1", "lam_q2", "lam_k2",
               "subln_w", "w_out", "ln1_g", "ln1_b", "ln2_g", "ln2_b", "ffn_w_gate", "ffn_w_up", "ffn_w_down",
               "router_w", "moe_w_gate", "moe_w_up", "moe_w_down")


def make_in_maps(inputs, cores):
    cf = _consts()
    shared = {k: np.ascontiguousarray(np.asarray(inputs[k], dtype=np.float32)) for k in WEIGHT_KEYS}
    x = np.asarray(inputs["x"], dtype=np.float32)
    pos = np.asarray(inputs["positions"]).astype(np.int32)
    maps = []
    for b in cores:
        m = dict(shared)
        m["x"] = np.ascontiguousarray(x[b])
        m["pos"] = np.ascontiguousarray(pos[b:b + 1])
        m["cf"] = cf
        maps.append(m)
    return maps


def kernel(**inputs):
    nc = build_program()
    in_maps = make_in_maps(inputs, range(8))
    res = run_bass_kernel_spmd(nc, in_maps, core_ids=list(range(8)))
    out = np.stack([np.asarray(r["y"], dtype=np.float32) for r in res.results], axis=0)
    return out
```

```python
import numpy as np
from contextlib import ExitStack
import concourse.bass as bass
import concourse.mybir as mybir
from concourse.bass_utils import run_bass_kernel_spmd

F32 = mybir.dt.float32
F32R = mybir.dt.float32r
BF16 = mybir.dt.bfloat16
I32 = mybir.dt.int32
AF = mybir.ActivationFunctionType
ALU = mybir.AluOpType
AX = mybir.AxisListType

EPOCH = 16384
USE_F32R = True


class Res:
    __slots__ = ("w", "rs", "name")

    def __init__(self, name=""):
        self.w = None
        self.rs = []
        self.name = name


class Prog:
    ENGS = ("pe", "act", "dve", "pool", "sp")

    def __init__(self, nc, stack, n_dma_sems=32):
        self.nc = nc
        self.stack = stack
        self.streams = {e: [] for e in self.ENGS}
        self.cnt = {e: 0 for e in self.ENGS}
        self.esems = {e: [] for e in self.ENGS}
        self.seen = {e: {} for e in self.ENGS}
        self.dsems = [stack.enter_context(nc.semaphore(f"dq{i}")) for i in range(n_dma_sems)]
        self.duse = [0] * n_dma_sems
        half = n_dma_sems // 2
        self.dpool = {"sp": list(range(0, half)), "pool": list(range(half, n_dma_sems))}
        self.dnext = {"sp": 0, "pool": 0}
        self.out_events = []

    def _esem(self, eng, epoch):
        lst = self.esems[eng]
        while len(lst) <= epoch:
            lst.append(self.stack.enter_context(self.nc.semaphore(f"c_{eng}_{len(lst)}")))
        return lst[epoch]

    def _collect(self, eng, reads, writes, is_dma):
        need = {}

        def add(ev, hazard):
            if ev is None:
                return
            if ev[0] == "c":
                if ev[1] == eng and not is_dma and hazard != "raw" and eng == "pe":
                    return
                key = ("c", ev[1])
            else:
                key = ("d", ev[1])
            if need.get(key, 0) < ev[2]:
                need[key] = ev[2]

        for r in reads:
            add(r.w, "raw")
        for w in writes:
            add(w.w, "waw")
            for ev in w.rs:
                add(ev, "war")
        waits = []
        seen = self.seen[eng]
        for key, val in need.items():
            if seen.get(key, 0) >= val:
                continue
            seen[key] = val
            if key[0] == "c":
                ep, v = divmod(val - 1, EPOCH)
                waits.append((self._esem(key[1], ep), v + 1))
            else:
                waits.append((self.dsems[key[1]], val))
        return waits

    def _commit(self, ev, reads, writes):
        for r in reads:
            r.rs.append(ev)
        for w in writes:
            w.w = ev
            w.rs = []

    def op(self, eng, fn, reads=(), writes=()):
        waits = self._collect(eng, reads, writes, False)
        self.cnt[eng] += 1
        g = self.cnt[eng]
        ep, v = divmod(g - 1, EPOCH)
        self.streams[eng].append((fn, waits, (self._esem(eng, ep), 1)))
        ev = ("c", eng, g)
        self._commit(ev, reads, writes)
        return ev

    def dma(self, out, in_, reads=(), writes=(), eng="sp", is_output=False, **kw):
        lst = self.dpool[eng]
        j = lst[self.dnext[eng] % len(lst)]
        self.dnext[eng] += 1
        waits = self._collect(eng, reads, writes, True)
        prev = self.duse[j] * 16
        seen = self.seen[eng]
        if prev and seen.get(("d", j), 0) < prev:
            seen[("d", j)] = prev
            waits.append((self.dsems[j], prev))
        self.duse[j] += 1
        val = self.duse[j] * 16
        fn = lambda e, out=out, in_=in_, kw=kw: e.dma_start(out=out, in_=in_, **kw)
        self.streams[eng].append((fn, waits, (self.dsems[j], 16)))
        ev = ("d", j, val)
        self._commit(ev, reads, writes)
        if is_output:
            self.out_events.append(ev)
        return ev

    def finish(self):
        need = {}
        for ev in self.out_events:
            need[ev[1]] = max(need.get(ev[1], 0), ev[2])
        fin = [(self.dsems[j], v) for j, v in need.items()]
        nc = self.nc
        streams = self.streams
        with nc.Block() as block:
            def emit(e, lst, final=()):
                for fn, waits, inc in lst:
                    for s, v in waits:
                        e.wait_ge(s, v)
                    fn(e).then_inc(inc[0], inc[1])
                for s, v in final:
                    e.wait_ge(s, v)

            @block.tensor
            def _(e):
                emit(e, streams["pe"])

            @block.scalar
            def _(e):
                emit(e, streams["act"])

            @block.vector
            def _(e):
                emit(e, streams["dve"])

            @block.gpsimd
            def _(e):
                emit(e, streams["pool"])

            @block.sync
            def _(e):
                emit(e, streams["sp"], fin)


SEQ = 2048
DM = 1024
NT = 16
NKC = 8
DEPTH = 4
D_IN = 3592
FF_DENSE = 2816
FF_MOE = 3584
NEXP = 8
ALPHA = (2.0 * DEPTH) ** 0.25
LN_EPS = 1e-5
RMS_EPS = 1e-6
TWO_PI = 6.283185307179586
ARENA_W = 22528
MT_OFF = 0


def _dsize(dt):
    return 4 if dt in (F32, I32) else 2


class Arena:
    def __init__(self, t):
        self.t = t
        self.live = []
        self.frozen = []

    @staticmethod
    def _compress(res_list):
        d = {}
        for r in res_list:
            for ev in ([r.w] if r.w else []) + list(r.rs):
                key = (ev[0], ev[1])
                if key not in d or d[key][2] < ev[2]:
                    d[key] = ev
        return d

    def carve(self, lo, shape, dt, nres=1):
        n = 1
        for s in shape:
            n *= s
        nbytes = n * _dsize(dt)
        hi = lo + nbytes
        assert lo % 4 == 0 and hi <= ARENA_W * 4, (lo, hi)
        evs = {}
        keep = []
        for (l2, h2, rl) in self.live:
            if l2 < hi and lo < h2:
                d = self._compress(rl)
                self.frozen.append((l2, h2, d))
            else:
                keep.append((l2, h2, rl))
        newf = []
        for (l2, h2, d) in self.frozen:
            if l2 < hi and lo < h2:
                for key, ev in d.items():
                    if key not in evs or evs[key][2] < ev[2]:
                        evs[key] = ev
            if not (lo <= l2 and h2 <= hi):
                newf.append((l2, h2, d))
        self.frozen = newf
        res = [Res() for _ in range(nres)]
        for r in res:
            r.rs = list(evs.values())
        self.live = keep + [(lo, hi, res)]
        ap = self.t[:, lo // 4:(hi + 3) // 4]
        if dt != F32:
            ap = ap.bitcast(dt)
        if len(shape) == 2:
            ap = ap.rearrange("p (a b) -> p a b", a=shape[0])
        elif len(shape) == 3:
            ap = ap.rearrange("p (a b c) -> p a b c", a=shape[0], b=shape[1])
        return ap, res


class Kern:
    def __init__(self, nc, P, st, n_layers=DEPTH, upto=None):
        self.nc, self.P, self.st = nc, P, st
        self.n_layers = n_layers
        self.upto = upto
        dr = lambda name, shape, dt=F32, kind="ExternalInput": nc.dram_tensor(name, shape, dt, kind=kind).ap()
        self.d = {}
        self.d["x"] = dr("x", [SEQ, DM])
        self.d["pos"] = dr("pos", [1, SEQ], I32)
        self.d["cf"] = dr("cf", [128, 648])
        self.d["w_in"] = dr("w_in", [DEPTH, DM, D_IN])
        self.d["conv_w"] = dr("conv_w", [DEPTH, 4, 1536])
        self.d["a_log"] = dr("a_log", [DEPTH, 4])
        self.d["dt_bias"] = dr("dt_bias", [DEPTH, 4])
        self.d["gdn_norm_w"] = dr("gdn_norm_w", [DEPTH, 128])
        for k in ("lam_q1", "lam_k1", "lam_q2", "lam_k2"):
            self.d[k] = dr(k, [DEPTH, 64])
        self.d["subln_w"] = dr("subln_w", [DEPTH, 128])
        self.d["w_out"] = dr("w_out", [DEPTH, DM, DM])
        for k in ("ln1_g", "ln1_b", "ln2_g", "ln2_b"):
            self.d[k] = dr(k, [DEPTH, DM])
        self.d["ffn_w_gate"] = dr("ffn_w_gate", [2, DM, FF_DENSE])
        self.d["ffn_w_up"] = dr("ffn_w_up", [2, DM, FF_DENSE])
        self.d["ffn_w_down"] = dr("ffn_w_down", [2, FF_DENSE, DM])
        self.d["router_w"] = dr("router_w", [2, DM, NEXP])
        self.d["moe_w_gate"] = dr("moe_w_gate", [2, NEXP, DM, FF_MOE])
        self.d["moe_w_up"] = dr("moe_w_up", [2, NEXP, DM, FF_MOE])
        self.d["moe_w_down"] = dr("moe_w_down", [2, NEXP, FF_MOE, DM])
        self.d["y"] = dr("y", [SEQ, DM], F32, "ExternalOutput")
        if upto is not None:
            self.d["dbg"] = dr("dbg", [SEQ, DM], F32, "ExternalOutput")

        sb = lambda name, shape, dt: st.enter_context(nc.sbuf_tensor(name, shape, dt))
        self.X = sb("X", [128, NT, DM], F32)
        self.RX = [Res(f"X{t}") for t in range(NT)]
        self.XT = sb("XT", [128, NKC, SEQ], BF16)
        self.RXT = [Res(f"XT{t}") for t in range(NT)]
        self.CF = sb("CF", [128, 648], F32)
        self.RCF = Res("CF")
        self.CB = sb("CB", [128, 5, 128], BF16)
        self.RCB = Res("CB")
        self.cosT = sb("cosT", [128, SEQ], BF16)
        self.sinT = sb("sinT", [128, SEQ], BF16)
        self.RTAB = Res("tab")
        self.SM = sb("SM", [128, 1024], F32)
        self.Rsm = Res("sm")
        self.AR = Arena(sb("AR", [128, ARENA_W], F32))
        self.RR = sb("RR", [128, 16, 128], F32)
        self.RRr = [Res(f"rr{i}") for i in range(16)]
        self.PSB = [st.enter_context(nc.psum_tensor(f"ps{b}", [128, 512], F32)) for b in range(8)]
        self.RPS = [Res(f"ps{b}") for b in range(8)]
        self.pools = {}
        self.identf = self.CF[:, 0:128]
        self.Uf = self.CF[:, 128:256]
        self.Usf = self.CF[:, 256:384]
        self.onesf = self.CF[:, 384:512]
        self.invf = self.CF[:, 640:641]
        self.identb = self.CB[:, 0, :]
        self.Ub = self.CB[:, 1, :]
        self.onesb = self.CB[:, 2, :]
        self.Rmb = self.CB[:, 3, :]

    def bank(self, pool, banks=None):
        if pool not in self.pools:
            self.pools[pool] = [banks, 0]
        lst, c = self.pools[pool]
        self.pools[pool][1] += 1
        b = lst[c % len(lst)]
        return b

    def psf(self, b):
        return self.PSB[b][:]

    def psb16(self, b):
        return self.PSB[b][:].bitcast(BF16)

    def mm(self, out, lhsT, rhs, start, stop, reads, writes):
        self.P.op("pe", lambda e: e.matmul(out, lhsT=lhsT, rhs=rhs, start=start, stop=stop), reads, writes)

    def mmr(self, out, lhsT, rhs, start, stop, reads, writes):
        if not USE_F32R:
            return self.mm(out, lhsT, rhs, start, stop, reads, writes)
        a, b = lhsT.bitcast(F32R), rhs.bitcast(F32R)
        self.P.op("pe", lambda e: e.matmul(out, lhsT=a, rhs=b, start=start, stop=stop), reads, writes)

    def tr(self, out, in_, ident, reads, writes):
        self.P.op("pe", lambda e: e.transpose(out, in_, ident), reads, writes)

    def act(self, out, in_, func, reads, writes, **kw):
        self.P.op("act", lambda e: e.activation(out=out, in_=in_, func=func, **kw), reads, writes)

    def tt(self, eng, out, in0, in1, op, reads, writes):
        self.P.op(eng, lambda e: e.tensor_tensor(out=out, in0=in0, in1=in1, op=op), reads, writes)

    def ts(self, eng, out, in0, s1, s2, op0, op1, reads, writes, **kw):
        if op1 is None:
            self.P.op(eng, lambda e: e.tensor_scalar(out=out, in0=in0, scalar1=s1, scalar2=None, op0=op0, **kw), reads, writes)
        else:
            self.P.op(eng, lambda e: e.tensor_scalar(out=out, in0=in0, scalar1=s1, scalar2=s2, op0=op0, op1=op1, **kw), reads, writes)

    def stt(self, eng, out, in0, scalar, in1, op0, op1, reads, writes, **kw):
        self.P.op(eng, lambda e: e.scalar_tensor_tensor(out=out, in0=in0, scalar=scalar, in1=in1, op0=op0, op1=op1, **kw), reads, writes)

    def mmr2(self, out, lhsT, rhs, start, stop, reads, writes):
        return self.mmr(out, lhsT, rhs, start, stop, reads, writes)

    def cp(self, eng, out, in_, reads, writes):
        if eng == "act":
            self.P.op("act", lambda e: e.activation(out=out, in_=in_, func=AF.Copy), reads, writes)
        else:
            self.P.op(eng, lambda e: e.tensor_copy(out=out, in_=in_), reads, writes)

    def memset(self, eng, ap, val, writes):
        self.P.op(eng, lambda e: e.memset(ap, val), (), writes)

    def prologue(self):
        P, d = self.P, self.d
        P.dma(self.CF[:], d["cf"], writes=[self.RCF])
        for i, lo in enumerate([0, 128, 384, 512]):
            P.dma(self.CB[:, i, :], d["cf"][:, lo:lo + 128], writes=[self.RCB], eng="pool")
        xr = d["x"].rearrange("(t p) d -> p t d", p=128)
        for t in range(NT):
            P.dma(self.X[:, t, :], xr[:, t, :], writes=[self.RX[t]])
        import os
        skip = os.environ.get("PRO_SKIP", "")
        if "rope" in skip:
            if "xt" not in skip:
                self.make_xt(range(NT))
            return
        A = self.AR
        base = 32768
        posi, (Rp,) = A.carve(base, [SEQ], I32)
        r, (Rr,) = A.carve(base + 8192, [SEQ], F32)
        r2, (Rr2,) = A.carve(base + 16384, [SEQ], F32)
        ki, (Rki,) = A.carve(base + 24576, [SEQ], I32)
        kf, (Rkf,) = A.carve(base + 32768, [SEQ], F32)
        mk, (Rmk,) = A.carve(base + 40960, [SEQ], F32)
        P.dma(posi, d["pos"].broadcast_to([128, SEQ]), writes=[Rp])
        if "s1" in skip:
            self.make_xt(range(NT)); return
        self.cp("dve", r, posi, [Rp], [Rr])
        self.ts("dve", r, r, self.invf, None, ALU.mult, None, [Rr, self.RCF], [Rr])
        if "s2" in skip:
            self.make_xt(range(NT)); return
        for tab, shift in ((self.sinT, 0.0), (self.cosT, 0.25)):
            self.ts("dve", r2, r, shift, None, ALU.add, None, [Rr], [Rr2])
            self.cp("dve", ki, r2, [Rr2], [Rki])
            self.cp("dve", kf, ki, [Rki], [Rkf])
            self.tt("dve", r2, r2, kf, ALU.subtract, [Rr2, Rkf], [Rr2])
            self.ts("dve", mk, r2, 0.5, None, ALU.is_gt, None, [Rr2], [Rmk])
            self.tt("dve", r2, r2, mk, ALU.subtract, [Rr2, Rmk], [Rr2])
            self.ts("dve", mk, r2, -0.5, None, ALU.is_lt, None, [Rr2], [Rmk])
            self.tt("dve", r2, r2, mk, ALU.add, [Rr2, Rmk], [Rr2])
            if "s3" in skip:
                if "dumpr2" in skip:
                    k0 = 0 if shift == 0.0 else 2
                    self.cp("dve", self.X[:, k0, :], r2[:, 0:1024], [Rr2], [self.RX[k0]])
                    self.cp("dve", self.X[:, k0 + 1, :], r2[:, 1024:2048], [Rr2], [self.RX[k0 + 1]])
                continue
            self.act(tab[:], r2, AF.Sin, [Rr2], [self.RTAB], scale=TWO_PI * (1.0 - 1e-6))
        self.make_xt(range(NT))

    def make_xt(self, tiles):
        A = self.AR
        xb2, Rxb = A.carve(32768 + 49152, [2, DM], BF16, nres=2)
        for t in tiles:
            s = t % 2
            self.cp("act", xb2[:, s, :], self.X[:, t, :], [self.RX[t]], [Rxb[s]])
            b = self.bank("tp", [6, 7])
            pv = self.psb16(b)
            for kc in range(NKC):
                self.tr(pv[:, kc * 128:(kc + 1) * 128], xb2[:, s, kc * 128:(kc + 1) * 128], self.identb,
                        [Rxb[s], self.RCB], [self.RPS[b]])
            self.cp("dve", self.XT[:, :, t * 128:(t + 1) * 128], pv.rearrange("p (c n) -> p c n", c=NKC),
                    [], [self.RPS[b], self.RXT[t]])

    def dump_featmajor_bf16(self, ap3, res_list):
        C = ap3.shape[1]
        dst = self.d["dbg"].rearrange("(a b) d -> a (b d)", b=2)
        dst = dst.rearrange("(c p) t -> p c t", p=128)
        self.P.dma(dst[:, 0:C, :], ap3, reads=res_list, eng="pool", is_output=True)

    def dump_X(self, name="dbg"):
        yr = self.d[name].rearrange("(t p) d -> p t d", p=128)
        for t in range(NT):
            self.P.dma(yr[:, t, :], self.X[:, t, :], reads=[self.RX[t]], is_output=True)

    def layer_params(self, l):
        P, d, SM = self.P, self.d, self.SM
        Rsm = self.Rsm
        lam_init = 0.8 - 0.6 * float(np.exp(-0.3 * l))
        self.lam_init = lam_init
        for i, k in enumerate(("lam_q1", "lam_k1", "lam_q2", "lam_k2")):
            P.dma(SM[:, i * 64:(i + 1) * 64], d[k][l:l + 1, :].broadcast_to([128, 64]), writes=[Rsm])
        P.dma(SM[:, 256:384], d["subln_w"][l:l + 1, :].broadcast_to([128, 128]), writes=[Rsm])
        P.dma(SM[:, 384:512], d["gdn_norm_w"][l:l + 1, :].broadcast_to([128, 128]), writes=[Rsm])
        P.dma(SM[:, 512:516], d["dt_bias"][l:l + 1, :].broadcast_to([128, 4]), writes=[Rsm])
        P.dma(SM[:, 516:520], d["a_log"][l:l + 1, :].broadcast_to([128, 4]), writes=[Rsm])
        self.stt("dve", SM[:, 960:1024], SM[:, 0:64], 1.0, SM[:, 64:128], ALU.mult, ALU.mult, [Rsm], [Rsm], accum_out=SM[:, 520:521])
        self.stt("dve", SM[:, 960:1024], SM[:, 128:192], 1.0, SM[:, 192:256], ALU.mult, ALU.mult, [Rsm], [Rsm], accum_out=SM[:, 521:522])
        self.act(SM[:, 522:524], SM[:, 520:522], AF.Exp, [Rsm], [Rsm])
        self.tt("dve", SM[:, 520:521], SM[:, 522:523], SM[:, 523:524], ALU.subtract, [Rsm], [Rsm])
        self.ts("dve", SM[:, 524:525], SM[:, 520:521], -1.0, -lam_init, ALU.mult, ALU.add, [Rsm], [Rsm])
        self.neglam = SM[:, 524:525]
        self.ts("dve", SM[:, 256:384], SM[:, 256:384], 1.0 - lam_init, None, ALU.mult, None, [Rsm], [Rsm])
        self.WSUB = SM[:, 256:384]
        self.GNW = SM[:, 384:512]
        self.act(SM[:, 516:520], SM[:, 516:520], AF.Exp, [Rsm], [Rsm])
        self.ts("dve", SM[:, 516:520], SM[:, 516:520], -1.0, None, ALU.mult, None, [Rsm], [Rsm])

    def layer_norm_all(self, gname, bname, l, lnoff, need_xt=True):
        P, d, A = self.P, self.d, self.AR
        LNG, (Rg,) = A.carve(lnoff, [DM], F32)
        LNB, (Rb,) = A.carve(lnoff + 4096, [DM], F32)
        junk, (Rj,) = A.carve(lnoff + 8192, [DM], F32)
        ST, (Rs,) = A.carve(lnoff + 12288, [128], F32)
        junk2, (Rj2,) = A.carve(lnoff + 12800, [DM], F32)
        P.dma(LNG, d[gname][l:l + 1, :].broadcast_to([128, DM]), writes=[Rg])
        P.dma(LNB, d[bname][l:l + 1, :].broadcast_to([128, DM]), writes=[Rb])
        X, RX = self.X, self.RX
        for t in range(NT):
            self.act(junk, X[:, t, :], AF.Identity, [RX[t]], [Rj, Rs], accum_out=ST[:, t:t + 1])
            self.stt("dve", junk2, X[:, t, :], 1.0, X[:, t, :], ALU.mult, ALU.mult, [RX[t]], [Rj2, Rs], accum_out=ST[:, 16 + t:17 + t])
        self.ts("dve", ST[:, 0:32], ST[:, 0:32], 1.0 / DM, None, ALU.mult, None, [Rs], [Rs])
        self.tt("dve", ST[:, 32:48], ST[:, 0:16], ST[:, 0:16], ALU.mult, [Rs], [Rs])
        self.tt("dve", ST[:, 48:64], ST[:, 16:32], ST[:, 32:48], ALU.subtract, [Rs], [Rs])
        self.act(ST[:, 64:80], ST[:, 48:64], AF.Ln, [Rs], [Rs], bias=LN_EPS)
        self.act(ST[:, 80:96], ST[:, 64:80], AF.Exp, [Rs], [Rs], scale=-0.5)
        self.stt("dve", ST[:, 96:112], ST[:, 0:16], -1.0, ST[:, 80:96], ALU.mult, ALU.mult, [Rs], [Rs])
        for t in range(NT):
            self.act(X[:, t, :], X[:, t, :], AF.Identity, [RX[t], Rs], [RX[t]], scale=ST[:, 80 + t:81 + t], bias=ST[:, 96 + t:97 + t])
            self.tt("dve", X[:, t, :], X[:, t, :], LNG, ALU.mult, [RX[t], Rg], [RX[t]])
            self.tt("pool", X[:, t, :], X[:, t, :], LNB, ALU.add, [RX[t], Rb], [RX[t]])
        if need_xt:
            self.make_xt(range(NT))

    def da_head(self, l, h):
        P, d, A = self.P, self.d, self.AR
        XT, RXT = self.XT, self.RXT
        base = 32768
        W2, RW2 = A.carve(base, [2, NKC, 384], BF16, nres=2) if h == 0 else (self._daW, self._daRW)
        self._daW, self._daRW = W2, RW2
        W, RW = W2[:, h % 2], RW2[h % 2]
        o = base + 12288
        qk, Rqk = A.carve(o, [2, SEQ], BF16, nres=2); o += 8192
        V, (RV,) = A.carve(o, [NT, 132], BF16); o += 4224
        rawb, Rraw = A.carve(o, [8, 512], BF16, nres=8); o += 8192
        t1, Rt1 = A.carve(o, [2, 512], F32, nres=2); o += 4096
        t2, Rt2 = A.carve(o, [2, 512], F32, nres=2); o += 4096
        sq, Rsq = A.carve(o, [2, 512], BF16, nres=2); o += 2048
        ET, RET = A.carve(o, [4, 256], BF16, nres=4); o += 2048
        ep_t, Rept = A.carve(o, [2, 128], F32, nres=2); o += 1024
        ep_o, Repo = A.carve(o, [2, 128], F32, nres=2); o += 1024
        ep_j, (Repj,) = A.carve(o, [128], F32); o += 512
        ep_n, Repn = A.carve(o, [2, 128], BF16, nres=2); o += 512
        st, (Rst, RnegM, Rst_a, Rst_b) = A.carve(o, [64], F32, nres=4); o += 256
        Rst2 = [Rst_a, Rst_b]
        U2, (RU2,) = A.carve(o, [2, 128], BF16); o += 512
        kz, (Rkz,) = A.carve(o, [NT, 2, 128], BF16); o += 8192
        if h == 0:
            self.memset("pool", kz[64:128, :, 0, :], 0.0, [Rkz])
            self.memset("pool", kz[0:64, :, 1, :], 0.0, [Rkz])
        win = d["w_in"][l].rearrange("(c p) n -> p c n", p=128)

        def load_w(hh):
            Wd_, RWd_ = W2[:, hh % 2], RW2[hh % 2]
            for i, c0 in enumerate((hh * 128, 512 + hh * 128, 1024 + hh * 128)):
                P.dma(Wd_[:, :, i * 128:(i + 1) * 128], win[:, :, c0:c0 + 128], writes=[RWd_], eng="pool")
        if h == 0:
            load_w(0)
        self.cp("pool", U2[:, 0, :], self.Ub, [self.RCB], [RU2])
        self.cp("pool", U2[:, 1, :], self.Ub, [self.RCB], [RU2])
        its = [(which, tg) for which in range(2) for tg in range(4)]
        for it, (which, tg) in enumerate(its):
            cols = slice(tg * 512, (tg + 1) * 512)
            b = self.bank("proj", [0, 1])
            ps = self.psf(b)
            for kc in range(NKC):
                self.mm(ps, W[:, kc, which * 128:(which + 1) * 128], XT[:, kc, cols], kc == 0, kc == NKC - 1,
                        [RW] + RXT[4 * tg:4 * tg + 4], [self.RPS[b]])
            self.cp("act", rawb[:, it, :], ps, [], [self.RPS[b], Rraw[it]])
        for it, (which, tg) in enumerate(its):
            s = it % 2
            cols = slice(tg * 512, (tg + 1) * 512)
            b2 = self.bank("rot", [2, 3])
            ps2 = self.psf(b2)
            self.mm(ps2, self.Rmb, rawb[:, it, :], True, True, [self.RCB, Rraw[it]], [self.RPS[b2]])
            self.tt("dve", t1[:, s, :], rawb[:, it, :], self.cosT[:, cols], ALU.mult, [self.RTAB, Rraw[it]], [Rt1[s]])
            self.tt("dve", t2[:, s, :], ps2, self.sinT[:, cols], ALU.mult, [self.RTAB], [self.RPS[b2], Rt2[s]])
            self.tt("dve", qk[:, which, cols], t1[:, s, :], t2[:, s, :], ALU.add, [Rt1[s], Rt2[s]], [Rqk[which]])
            self.act(sq[:, s, :], qk[:, which, cols], AF.Square, [Rqk[which]], [Rsq[s]])
            if which == 0:
                self.cp("act", kz[0:64, 4 * tg:4 * tg + 4, 0, :], qk[0:64, 0, cols].rearrange("p (a n) -> p a n", a=4), [Rqk[0]], [Rkz])
                self.cp("pool", kz[64:128, 4 * tg:4 * tg + 4, 1, :], qk[64:128, 0, cols].rearrange("p (a n) -> p a n", a=4), [Rqk[0]], [Rkz])
            b3 = self.bank("nrm", [4, 5])
            ps3 = self.psf(b3)
            self.mm(ps3, self.onesb, sq[:, s, :], True, True, [self.RCB, Rsq[s]], [self.RPS[b3]])
            self.P.op("dve", lambda e, o_=st[:, which * 4 + tg:which * 4 + tg + 1], i_=ps3: e.reduce_max(out=o_, in_=i_, axis=AX.X),
                      [], [self.RPS[b3], Rst])
        self.P.op("dve", lambda e: e.reduce_max(out=st[:, 8:9], in_=st[:, 0:4], axis=AX.X), [Rst], [Rst])
        self.P.op("dve", lambda e: e.reduce_max(out=st[:, 9:10], in_=st[:, 4:8], axis=AX.X), [Rst], [Rst])
        self.tt("dve", st[:, 10:11], st[:, 8:9], st[:, 9:10], ALU.mult, [Rst], [Rst])
        self.act(st[:, 11:12], st[:, 10:11], AF.Ln, [Rst], [Rst], bias=1e-30)
        self.act(st[:, 12:13], st[:, 11:12], AF.Exp, [Rst], [Rst], scale=0.5)
        self.ts("dve", st[:, 13:14], st[:, 12:13], -1.05 / 8.0, None, ALU.mult, None, [Rst], [RnegM])
        negM = st[:, 13:14]
        self.memset("pool", V[:, :, 128:129], 1.0, [RV])
        VT, RVT = rawb, Rraw
        for tg in range(4):
            b = self.bank("proj", [0, 1])
            ps = self.psf(b)
            for kc in range(NKC):
                self.mm(ps, W[:, kc, 256:384], XT[:, kc, tg * 512:(tg + 1) * 512], kc == 0, kc == NKC - 1,
                        [RW] + RXT[4 * tg:4 * tg + 4], [self.RPS[b]])
            self.cp("act", VT[:, tg, :], ps, [], [self.RPS[b], RVT[tg]])
            b2 = self.bank("rot", [2, 3])
            pv = self.psb16(b2)
            for ti in range(4):
                self.tr(pv[:, ti * 128:(ti + 1) * 128], VT[:, tg, ti * 128:(ti + 1) * 128], self.identb, [RVT[tg], self.RCB], [self.RPS[b2]])
            self.cp("dve", V[:, 4 * tg:4 * tg + 4, 0:128], pv[:, 0:512].rearrange("p (a n) -> p a n", a=4), [], [self.RPS[b2], RV])
        if h + 1 < 4:
            load_w(h + 1)
        qT, kT = qk[:, 0, :], qk[:, 1, :]
        pairs = [(i, j) for i in range(NT) for j in range(i + 1)]
        info = {}

        def emit_st(n):
            i, j = pairs[n]
            b = self.bank("st", [0, 1, 6])
            ps = self.psf(b)
            self.mm(ps[:, 0:256], kT[:, j * 128:(j + 1) * 128], kz[:, i, :, :].rearrange("p c n -> p (c n)"), True, True,
                    [Rqk[1], Rkz], [self.RPS[b]])
            e = n % 4
            self.act(ET[:, e, :], ps[:, 0:256], AF.Exp, [RnegM], [self.RPS[b], RET[e]], scale=0.125, bias=negM)
            if i == j:
                self.tt("pool", ET[:, e, :], ET[:, e, :], U2.rearrange("p a n -> p (a n)"), ALU.mult, [RET[e], RU2], [RET[e]])

        def emit_pv(n):
            i, j = pairs[n]
            e = n % 4
            for c in range(2):
                b = [2, 3, 4, 5][2 * c + (i % 2)]
                self.mm(self.psf(b)[:, 0:129], ET[:, e, c * 128:(c + 1) * 128], V[:, j, 0:129], j == 0, j == i,
                        [RET[e], RV], [self.RPS[b]])

        def epilogue(i):
            s = i % 2
            b0, b1 = [2, 3][s], [4, 5][s]
            O0, O1 = self.psf(b0), self.psf(b1)
            Rst = Rst2[s]
            c = 16 + 4 * s
            self.P.op("dve", lambda e: e.reciprocal(out=st[:, c:c + 1], in_=O0[:, 128:129]), [], [self.RPS[b0], Rst])
            self.P.op("dve", lambda e: e.reciprocal(out=st[:, c + 1:c + 2], in_=O1[:, 128:129]), [], [self.RPS[b1], Rst])
            self.tt("dve", st[:, c + 2:c + 3], st[:, c + 1:c + 2], self.neglam, ALU.mult, [Rst, self.Rsm], [Rst])
            self.ts("dve", ep_t[:, s, :], O1[:, 0:128], st[:, c + 2:c + 3], None, ALU.mult, None, [Rst], [self.RPS[b1], Rept[s]])
            self.stt("dve", ep_o[:, s, :], O0[:, 0:128], st[:, c:c + 1], ep_t[:, s, :], ALU.mult, ALU.add, [Rst, Rept[s]], [self.RPS[b0], Repo[s]])
            self.stt("dve", ep_j, ep_o[:, s, :], 1.0, ep_o[:, s, :], ALU.mult, ALU.mult, [Repo[s]], [Repj, Rst], accum_out=st[:, c + 3:c + 4])
            self.act(st[:, 24 + s:25 + s], st[:, c + 3:c + 4], AF.Ln, [Rst], [Rst], scale=1.0 / 128.0, bias=RMS_EPS)
            self.act(st[:, 26 + s:27 + s], st[:, 24 + s:25 + s], AF.Exp, [Rst], [Rst], scale=-0.5)
            self.stt("dve", ep_n[:, s, :], ep_o[:, s, :], st[:, 26 + s:27 + s], self.WSUB, ALU.mult, ALU.mult, [Repo[s], Rst, self.Rsm], [Repn[s]])
            bt = 7
            pv = self.psb16(bt)
            self.tr(pv[:, 0:128], ep_n[:, s, :], self.identb, [Repn[s], self.RCB], [self.RPS[bt]])
            self.cp("dve", self.MT[:, h, i * 128:(i + 1) * 128], pv[:, 0:128], [], [self.RPS[bt], self.RMT[h][i]])

        emit_st(0)
        emit_st(1)
        pending = []
        for n in range(len(pairs)):
            if n + 2 < len(pairs):
                emit_st(n + 2)
            i, j = pairs[n]
            while pending and pending[0][1] <= i - 2:
                epilogue(pending.pop(0)[1])
            emit_pv(n)
            if i == j:
                pending.append((n + 3, i))
            while pending and pending[0][0] <= n:
                epilogue(pending.pop(0)[1])
        while pending:
            epilogue(pending.pop(0)[1])

    def gdn_prep(self, l):
        P, d, A, SM = self.P, self.d, self.AR, self.SM
        XT, RXT = self.XT, self.RXT
        Rsm = self.Rsm
        base = 32768
        WAB, (Rwab,) = A.carve(base, [NKC, 8], BF16)
        GS, (Rgs,) = A.carve(base + 1024, [6, 64], F32)
        CWR, (Rcwr,) = A.carve(base + 4096, [1536], F32)
        CW, (Rcw,) = A.carve(base + 4096 + 6144, [12, 4], F32)
        self.GS, self.Rgs, self.CW, self.Rcw = GS, Rgs, CW, Rcw
        win = d["w_in"][l].rearrange("(c p) n -> p c n", p=128)
        P.dma(WAB, win[:, :, 3584:3592], writes=[Rwab], eng="pool")
        P.dma(CWR[0:4, :], d["conv_w"][l], writes=[Rcwr])
        b = self.bank("misc", [6, 7])
        ps = self.psf(b)
        for t in range(NT):
            for kc in range(NKC):
                self.mm(ps[:, t * 8:(t + 1) * 8], XT[:, kc, t * 128:(t + 1) * 128], WAB[:, kc, :], kc == 0, kc == NKC - 1,
                        [Rwab, RXT[t]], [self.RPS[b]])
        AB = SM[:, 640:768]
        self.cp("dve", AB, ps[:, 0:128], [], [self.RPS[b], Rsm])
        AB3 = AB.rearrange("p (t e) -> p t e", e=8)
        g3 = SM[:, 768:832].rearrange("p (t h) -> p t h", h=4)
        be3 = SM[:, 832:896].rearrange("p (t h) -> p t h", h=4)
        nb3 = SM[:, 896:960].rearrange("p (t h) -> p t h", h=4)
        tmp = SM[:, 960:976]
        for h in range(4):
            self.ts("dve", tmp, AB3[:, :, h], SM[:, 512 + h:513 + h], None, ALU.add, None, [Rsm], [Rsm])
            self.act(tmp, tmp, AF.Exp, [Rsm], [Rsm])
            self.act(tmp, tmp, AF.Ln, [Rsm], [Rsm], bias=1.0)
            self.ts("dve", g3[:, :, h], tmp, SM[:, 516 + h:517 + h], None, ALU.mult, None, [Rsm], [Rsm])
        self.act(SM[:, 832:896].rearrange("p (t h) -> p t h", h=4), AB3[:, :, 4:8], AF.Sigmoid, [Rsm], [Rsm])
        self.ts("dve", SM[:, 896:960], SM[:, 832:896], -1.0, None, ALU.mult, None, [Rsm], [Rsm])
        self.g3, self.be3, self.nb3 = g3, be3, nb3
        b = self.bank("misc", [6, 7])
        ps = self.psf(b)
        self.mm(ps[:, 0:64], self.Uf, SM[:, 768:832], True, True, [self.RCF, Rsm], [self.RPS[b]])
        self.mm(ps[:, 64:128], self.onesf, SM[:, 768:832], True, True, [self.RCF, Rsm], [self.RPS[b]])
        self.cp("dve", GS[:, 0, :], ps[:, 0:64], [], [self.RPS[b], Rgs])
        self.act(GS[:, 1, :], ps[:, 0:64], AF.Exp, [], [self.RPS[b], Rgs])
        self.act(GS[:, 3, :], ps[:, 64:128], AF.Exp, [], [self.RPS[b], Rgs])
        self.tt("dve", GS[:, 4, :], ps[:, 64:128], GS[:, 0, :], ALU.subtract, [Rgs], [self.RPS[b], Rgs])
        self.act(GS[:, 2, :], GS[:, 4, :], AF.Exp, [Rgs], [Rgs])
        b = self.bank("misc", [6, 7])
        ps = self.psf(b)
        for c in range(12):
            self.mm(ps[:, c * 4:(c + 1) * 4], CWR[0:4, c * 128:(c + 1) * 128], self.identf[0:4, 0:4], True, True,
                    [Rcwr, self.RCF], [self.RPS[b]])
        self.cp("dve", CW, ps[:, 0:48].rearrange("p (c j) -> p c j", j=4), [], [self.RPS[b], Rcw])

    def gdn_head(self, l, h):
        P, d, A, SM = self.P, self.d, self.AR, self.SM
        XT, RXT = self.XT, self.RXT
        Rsm, GS, Rgs = self.Rsm, self.GS, self.Rgs
        o = 43264
        WG, (RWG,) = A.carve(o, [NKC, 512], BF16); o += 8192
        qkv, Rqkv = A.carve(o, [3, SEQ], BF16, nres=3); o += 12288
        SZ, (RSZ,) = A.carve(o, [NT, 128], BF16); o += 4096
        area = o
        Raw3, RRaw3 = A.carve(area, [3, 2052], BF16, nres=3)
        accs, Raccs = A.carve(area + 12312, [4, 512], BF16, nres=4)
        DW, (RDW,) = A.carve(area + 16408, [12, 128], BF16)
        sqb, Rsqb = A.carve(36864, [2, 512], BF16, nres=2)
        rn, Rrn = A.carve(38912, [2, 512], F32, nres=2)
        win = d["w_in"][l].rearrange("(c p) n -> p c n", p=128)
        for i, c0 in enumerate((1536, 2048, 2560, 3072)):
            P.dma(WG[:, :, i * 128:(i + 1) * 128], win[:, :, c0 + h * 128:c0 + (h + 1) * 128], writes=[RWG], eng="pool")
        for which in range(3):
            self.memset("pool", Raw3[:, which, 0:3], 0.0, [RRaw3[which]])
            for j in range(4):
                self.ts("dve", DW[:, which * 4 + j, :], self.identb, self.CW[:, which * 4 + h, j:j + 1], None, ALU.mult, None,
                        [self.RCB, self.Rcw], [RDW])
        for which in range(3):
            for tg in range(4):
                b = self.bank("g", list(range(8)))
                ps = self.psf(b)
                for kc in range(NKC):
                    self.mm(ps, WG[:, kc, which * 128:(which + 1) * 128], XT[:, kc, tg * 512:(tg + 1) * 512], kc == 0, kc == NKC - 1,
                            [RWG] + RXT[4 * tg:4 * tg + 4], [self.RPS[b]])
                self.cp("act", Raw3[:, which, 3 + tg * 512:3 + (tg + 1) * 512], ps, [], [self.RPS[b], RRaw3[which]])
        cnt = 0
        for which in (2, 0, 1):
            for tg in range(4):
                cols = slice(tg * 512, (tg + 1) * 512)
                b = self.bank("g", list(range(8)))
                ps = self.psf(b)
                for j in range(4):
                    self.mm(ps, DW[:, which * 4 + j, :], Raw3[:, which, j + tg * 512:j + (tg + 1) * 512], j == 0, j == 3,
                            [RDW, RRaw3[which]], [self.RPS[b]])
                if which == 2:
                    self.act(qkv[:, 2, cols], ps, AF.Silu, [], [self.RPS[b], Rqkv[2]])
                else:
                    self.act(accs[:, tg, :], ps, AF.Silu, [], [self.RPS[b], Raccs[tg]])
            if which == 2:
                continue
            for tg in range(4):
                cols = slice(tg * 512, (tg + 1) * 512)
                s = cnt % 2
                cnt += 1
                self.tt("pool", sqb[:, s, :], accs[:, tg, :], accs[:, tg, :], ALU.mult, [Raccs[tg]], [Rsqb[s]])
                b = self.bank("g", list(range(8)))
                ps = self.psf(b)
                self.mm(ps, self.onesb, sqb[:, s, :], True, True, [self.RCB, Rsqb[s]], [self.RPS[b]])
                self.act(rn[:, s, :], ps, AF.Ln, [], [self.RPS[b], Rrn[s]], bias=RMS_EPS)
                self.act(rn[:, s, :], rn[:, s, :], AF.Exp, [Rrn[s]], [Rrn[s]], scale=-0.5, bias=(-0.5 * float(np.log(128.0)) if which == 0 else 0.0))
                self.tt("dve", qkv[:, which, cols], accs[:, tg, :], rn[:, s, :], ALU.mult, [Raccs[tg], Rrn[s]], [Rqkv[which]])
        SZT = SZ.rearrange("p a n -> p (a n)")
        for tg in range(4):
            b = self.bank("g", list(range(8)))
            ps = self.psf(b)
            for kc in range(NKC):
                self.mm(ps, WG[:, kc, 384:512], XT[:, kc, tg * 512:(tg + 1) * 512], kc == 0, kc == NKC - 1,
                        [RWG] + RXT[4 * tg:4 * tg + 4], [self.RPS[b]])
            self.act(SZT[:, tg * 512:(tg + 1) * 512], ps, AF.Silu, [], [self.RPS[b], RSZ])
        mats, Rm0 = A.carve(area, [16, 128], F32, nres=16)
        bm, Rb = A.carve(area + 16 * 512, [20, 128], BF16, nres=20)
        def _loc(i):
            if i < 26:
                bp, k = divmod(i, 13)
                if k < 5:
                    return ("a", bp * 5 + k)
                return ("r", bp * 8 + (k - 5))
            return ("a", 10 + (i - 26))

        class _RmProxy:
            def __getitem__(_s, i):
                kind, j = _loc(i)
                return Rm0[j] if kind == "a" else self.RRr[j]
        Rm = _RmProxy()

        def M(i):
            kind, j = _loc(i)
            return mats[:, j, :] if kind == "a" else self.RR[:, j, :]

        def MR(i):
            kind, j = _loc(i)
            assert kind == "r"
            return self.RR[:, j, :].bitcast(F32R) if USE_F32R else self.RR[:, j, :]
        B = lambda i: bm[:, i, :]
        qT, kT, vT = qkv[:, 0, :], qkv[:, 1, :], qkv[:, 2, :]
        S_i = [17, 18]
        VN, OG = 16, 19
        JK, ON = 30, 31
        og = B(OG)
        self.memset("pool", B(S_i[0]), 0.0, [Rb[S_i[0]]])
        Uf, Usf, identf, onesf = self.Uf, self.Usf, self.identf, self.onesf
        RCF = self.RCF
        g3, be3, nb3 = self.g3, self.be3, self.nb3
        gs = lambda k, n: GS[:, k, n * 4 + h:n * 4 + h + 1]
        busy = set()
        rr = [0]

        def newbank():
            for _ in range(8):
                b = rr[0] % 8
                rr[0] += 1
                if b not in busy:
                    busy.add(b)
                    return b
            raise RuntimeError("no free PSUM bank")

        def rel(b):
            busy.discard(b)

        def pre_steps(n, bpos, par):
            T = lambda k: bpos * 13 + k
            Pb = lambda k: par * 8 + bpos * 4 + k
            Pu = 26 + par * 2 + bpos
            cs = slice(n * 128, (n + 1) * 128)
            beta = be3[:, n, h:h + 1]
            st = {}
            steps = []

            def s0():
                self.act(M(T(0)), onesf, AF.Copy, [RCF, Rsm], [Rm[T(0)]], scale=g3[:, n, h:h + 1])
                b = newbank(); st["G"] = b
                self.mm(self.psf(b)[:, 0:128], M(T(0)), Uf, True, True, [Rm[T(0)], RCF], [self.RPS[b]])
                b = newbank(); st["A"] = b
                self.mm(self.psf(b)[:, 0:128], kT[:, cs], kT[:, cs], True, True, [Rqkv[1]], [self.RPS[b]])
                self.mm(self.psf(b)[:, 128:256], kT[:, cs], qT[:, cs], True, True, [Rqkv[0], Rqkv[1]], [self.RPS[b]])
                b = newbank(); st["KV"] = b
                pv = self.psb16(b)
                self.tr(pv[:, 0:128], kT[:, cs], self.identb, [Rqkv[1], self.RCB], [self.RPS[b]])
                self.tr(pv[:, 128:256], vT[:, cs], self.identb, [Rqkv[2], self.RCB], [self.RPS[b]])
            steps.append(s0)

            def s1():
                b = st["G"]
                self.ts("dve", M(T(1)), self.psf(b)[:, 0:128], gs(0, n), 0.0, ALU.subtract, ALU.min, [Rgs], [self.RPS[b], Rm[T(1)]])
                self.act(M(T(4)), self.psf(b)[:, 0:128], AF.Exp, [], [self.RPS[b], Rm[T(4)]])
                self.act(M(T(1)), M(T(1)), AF.Exp, [Rm[T(1)]], [Rm[T(1)]])
                rel(b)
                b = st["KV"]
                pv = self.psb16(b)
                self.act(B(Pb(2)), pv[:, 0:128], AF.Copy, [Rgs], [self.RPS[b], Rb[Pb(2)]], scale=gs(2, n))
            steps.append(s1)

            def s2():
                self.tt("pool", M(T(2)), M(T(1)), Uf, ALU.mult, [Rm[T(1)], RCF], [Rm[T(2)]])
                self.tt("pool", M(T(3)), M(T(1)), Usf, ALU.mult, [Rm[T(1)], RCF], [Rm[T(3)]])
                self.tt("pool", B(Pb(0)), qT[:, cs], M(T(4)), ALU.mult, [Rqkv[0], Rm[T(4)]], [Rb[Pb(0)]])
            steps.append(s2)

            def s3():
                b = st["A"]
                self.stt("dve", MR(T(5)), self.psf(b)[:, 0:128], beta, M(T(3)), ALU.mult, ALU.mult, [Rsm, Rm[T(3)]], [self.RPS[b], Rm[T(5)]])
                self.tt("dve", B(Pb(1)), self.psf(b)[:, 128:256], M(T(2)), ALU.mult, [Rm[T(2)]], [self.RPS[b], Rb[Pb(1)]])
                rel(b)
            steps.append(s3)

            def s4():
                b = newbank(); st["X"] = b
                self.tr(self.psf(b)[:, 0:128], M(T(5)), identf, [Rm[T(5)], RCF], [self.RPS[b]])
                self.tt("pool", MR(T(9)), identf, M(T(5)), ALU.subtract, [RCF, Rm[T(5)]], [Rm[T(9)]])
            steps.append(s4)

            def s5():
                b = st["X"]
                self.cp("act", MR(T(6)), self.psf(b)[:, 0:128], [], [self.RPS[b], Rm[T(6)]])
                rel(b)
                b = st["KV"]
                pv = self.psb16(b)
                self.ts("dve", MR(T(11)), pv[:, 0:128], gs(1, n), None, ALU.mult, None, [Rgs], [self.RPS[b], Rm[T(11)]])
                self.cp("act", MR(T(12)), pv[:, 128:256], [], [self.RPS[b], Rm[T(12)]])
                rel(b)
            steps.append(s5)
            zs = {0: T(5)}
            zts = {0: T(6)}
            ns = {0: T(9)}
            for j in range(1, 7):
                zs[j] = T(7) if j % 2 == 1 else T(5)
                zts[j] = T(8) if j % 2 == 1 else T(6)
                ns[j] = T(10) if j % 2 == 1 else T(9)
            KE, VC = T(11), T(12)

            def lvl_a(j):
                def f():
                    b = newbank(); st["Z%d" % j] = b
                    self.mm(self.psf(b)[:, 0:128], MR(zs[j - 1]), MR(zts[j - 1]), True, True, [Rm[zs[j - 1]], Rm[zts[j - 1]]], [self.RPS[b]])
                    if j < 6:
                        self.mm(self.psf(b)[:, 128:256], MR(zts[j - 1]), MR(zs[j - 1]), True, True, [Rm[zs[j - 1]], Rm[zts[j - 1]]], [self.RPS[b]])
                return f

            def lvl_b(j):
                def f():
                    b = st["Z%d" % j]
                    self.cp("act", MR(zts[j]), self.psf(b)[:, 0:128], [], [self.RPS[b], Rm[zts[j]]])
                    if j < 6:
                        self.cp("dve", MR(zs[j]), self.psf(b)[:, 128:256], [], [self.RPS[b], Rm[zs[j]]])
                    rel(b)
                return f

            def lvl_c(j):
                def f():
                    b = newbank(); st["N%d" % j] = b
                    self.mm(self.psf(b)[:, 0:128], MR(zts[j]), MR(ns[j - 1]), True, True, [Rm[zts[j]], Rm[ns[j - 1]]], [self.RPS[b]])
                return f

            def lvl_d(j):
                def f():
                    b = st["N%d" % j]
                    self.tt("dve", MR(ns[j]), self.psf(b)[:, 0:128], M(ns[j - 1]), ALU.add, [Rm[ns[j - 1]]], [self.RPS[b], Rm[ns[j]]])
                    rel(b)
                return f

            def both(f1, f2):
                def f():
                    f1()
                    if f2 is not None:
                        f2()
                return f
            steps += [lvl_a(1), lvl_b(1)]
            for j in range(1, 7):
                steps += [both(lvl_c(j), lvl_a(j + 1) if j < 6 else None), both(lvl_d(j), lvl_b(j + 1) if j < 6 else None)]
            NTi = ns[6]

            def s8():
                b = newbank(); st["WU"] = b
                self.mm(self.psf(b)[:, 0:128], MR(KE), MR(NTi), True, True, [Rm[KE], Rm[NTi]], [self.RPS[b]])
                self.mm(self.psf(b)[:, 128:256], MR(NTi), MR(VC), True, True, [Rm[VC], Rm[NTi]], [self.RPS[b]])
            steps.append(s8)

            def s9():
                b = st["WU"]
                self.cp("act", B(Pb(3)), self.psf(b)[:, 0:128], [], [self.RPS[b], Rb[Pb(3)]])
                self.ts("dve", M(Pu), self.psf(b)[:, 128:256], beta, None, ALU.mult, None, [Rsm], [self.RPS[b], Rm[Pu]])
                rel(b)
            steps.append(s9)
            return steps

        def scan_steps(n, bpos, par):
            Pb = lambda k: par * 8 + bpos * 4 + k
            Pu = 26 + par * 2 + bpos
            cs = slice(n * 128, (n + 1) * 128)
            Sc, Sn = S_i[n % 2], S_i[(n + 1) % 2]
            st = {}
            steps = []

            def a0():
                b = newbank(); st["1"] = b
                self.mm(self.psf(b)[:, 0:128], B(Pb(3)), B(Sc), True, True, [Rb[Pb(3)], Rb[Sc]], [self.RPS[b]])
            steps.append(a0)

            def a1():
                b = st["1"]
                self.stt("dve", B(VN), self.psf(b)[:, 0:128], nb3[:, n, h:h + 1], M(Pu), ALU.mult, ALU.add, [Rsm, Rm[Pu]], [self.RPS[b], Rb[VN]])
                rel(b)
            steps.append(a1)

            def a2():
                b = newbank(); st["O"] = b
                self.mm(self.psf(b)[:, 0:128], B(Pb(0)), B(Sc), True, False, [Rb[Pb(0)], Rb[Sc]], [self.RPS[b]])
                self.mm(self.psf(b)[:, 0:128], B(Pb(1)), B(VN), False, True, [Rb[Pb(1)], Rb[VN]], [self.RPS[b]])
                b = newbank(); st["S"] = b
                self.mm(self.psf(b)[:, 0:128], B(Pb(2)), B(VN), True, True, [Rb[Pb(2)], Rb[VN]], [self.RPS[b]])
            steps.append(a2)

            def a3():
                b = st["S"]
                self.stt("dve", B(Sn), B(Sc), gs(3, n), self.psf(b)[:, 0:128], ALU.mult, ALU.add, [Rb[Sc], Rgs], [self.RPS[b], Rb[Sn]])
                rel(b)
                b = st["O"]
                self.act(M(JK), self.psf(b)[:, 0:128], AF.Square, [], [self.RPS[b], Rm[JK], Rm[ON]], accum_out=M(ON)[:, 0:1])
            steps.append(a3)

            def a4():
                self.act(M(ON)[:, 1:2], M(ON)[:, 0:1], AF.Ln, [Rm[ON]], [Rm[ON]], scale=1.0 / 128.0, bias=RMS_EPS)
                self.act(M(ON)[:, 2:3], M(ON)[:, 1:2], AF.Exp, [Rm[ON]], [Rm[ON]], scale=-0.5)
            steps.append(a4)

            def a5():
                b = st["O"]
                self.stt("dve", og, self.psf(b)[:, 0:128], M(ON)[:, 2:3], self.GNW, ALU.mult, ALU.mult, [Rm[ON], Rsm], [self.RPS[b], Rb[OG]])
                rel(b)
                b = newbank(); st["T"] = b
                self.tr(self.psb16(b)[:, 0:128], og, self.identb, [Rb[OG], self.RCB], [self.RPS[b]])
            steps.append(a5)

            def a6():
                b = st["T"]
                self.tt("dve", self.MT[:, 4 + h, cs], self.psb16(b)[:, 0:128], SZT[:, cs], ALU.mult, [RSZ], [self.RPS[b], self.RMT[4 + h][n]])
                rel(b)
            steps.append(a6)
            return steps

        def merged(step_lists):
            idx = [0] * len(step_lists)
            alive = True
            while alive:
                alive = False
                for k, sl in enumerate(step_lists):
                    if idx[k] < len(sl):
                        sl[idx[k]]()
                        idx[k] += 1
                        alive = True

        nb = NT // 2
        pre = lambda k: [pre_steps(2 * k, 0, k % 2), pre_steps(2 * k + 1, 1, k % 2)]
        merged(pre(0))
        for k in range(nb):
            lists = []
            if k + 1 < nb:
                lists += pre(k + 1)
            sc = scan_steps(2 * k, 0, k % 2) + scan_steps(2 * k + 1, 1, k % 2)
            lists.append(sc)
            merged(lists)

    def mixer(self, l):
        A = self.AR
        self.MT, R = A.carve(0, [8, SEQ], BF16, nres=8 * NT)
        self.RMT = [[R[k * NT + t] for t in range(NT)] for k in range(8)]
        self.layer_params(l)
        for h in range(4):
            self.da_head(l, h)
        if self.upto == "da":
            return
        self.gdn_prep(l)
        for h in range(4):
            self.gdn_head(l, h)

    def out_proj_ln1(self, l):
        P, d, A = self.P, self.d, self.AR
        Wo, (RWo,) = A.carve(32768, [NKC, DM], BF16)
        P.dma(Wo, d["w_out"][l].rearrange("(c p) n -> p c n", p=128), writes=[RWo], eng="pool")
        for t in range(NT):
            for hf in range(2):
                b = self.bank("o", [0, 1, 2, 3])
                ps = self.psf(b)
                for kc in range(NKC):
                    self.mm(ps, self.MT[:, kc, t * 128:(t + 1) * 128], Wo[:, kc, hf * 512:(hf + 1) * 512], kc == 0, kc == NKC - 1,
                            [self.RMT[kc][t], RWo], [self.RPS[b]])
                xs = self.X[:, t, hf * 512:(hf + 1) * 512]
                self.stt("dve", xs, xs, ALPHA, ps, ALU.mult, ALU.add, [self.RX[t]], [self.RPS[b], self.RX[t]])
        self.layer_norm_all("ln1_g", "ln1_b", l, 49152)

    def ffn_ln2(self, l, last):
        P, d, A = self.P, self.d, self.AR
        X, RX, XT, RXT = self.X, self.RX, self.XT, self.RXT
        moe = (l % 2 == 1)
        li = l // 2
        o = 0
        WS = []
        for s in range(2):
            wg, (Rwg,) = A.carve(o, [NKC, 512], BF16); o += 8192
            wu, (Rwu,) = A.carve(o, [NKC, 512], BF16); o += 8192
            wd, (Rwd,) = A.carve(o, [4, DM], BF16); o += 8192
            WS.append((wg, Rwg, wu, Rwu, wd, Rwd))
        hT, RhT = A.carve(o, [2, 4, 512], BF16, nres=2); o += 8192
        sg, Rsg = A.carve(o, [2, 512], BF16, nres=2); o += 2048
        lnoff = o; o += 16896
        GT, (RGT,) = A.carve(o, [NT, 8], F32); o += 512
        if moe:
            RWt, (RRW,) = A.carve(o, [NKC, 8], F32); o += 256
            lg, (Rlg,) = A.carve(o, [64], F32); o += 256
            assert o <= 81920
            XTf, RXTf = A.carve(lnoff, [2, NKC, 128], F32, nres=2)
            P.dma(RWt, d["router_w"][li].rearrange("(c p) e -> p c e", p=128), writes=[RRW])
            for t in range(NT):
                s = t % 2
                for half in range(2):
                    b = self.bank("rt", [4, 5, 6, 7])
                    ps = self.psf(b)
                    for q in range(4):
                        kc = half * 4 + q
                        self.tr(ps[:, q * 128:(q + 1) * 128], X[:, t, kc * 128:(kc + 1) * 128], self.identf, [RX[t], self.RCF], [self.RPS[b]])
                    self.cp("act" if half else "dve", XTf[:, s, half * 4:half * 4 + 4, :], ps.rearrange("p (a n) -> p a n", a=4),
                            [], [self.RPS[b], RXTf[s]])
                b = self.bank("rt", [4, 5, 6, 7])
                ps = self.psf(b)
                for kc in range(NKC):
                    self.mm(ps[:, 0:8], XTf[:, s, kc, :], RWt[:, kc, :], kc == 0, kc == NKC - 1, [RXTf[s], RRW], [self.RPS[b]])
                self.cp("dve", lg[:, 0:8], ps[:, 0:8], [], [self.RPS[b], Rlg])
                self.P.op("dve", lambda e: e.max(out=lg[:, 8:16], in_=lg[:, 0:8]), [Rlg], [Rlg])
                self.tt("dve", lg[:, 16:17], lg[:, 9:10], lg[:, 8:9], ALU.subtract, [Rlg], [Rlg])
                self.act(lg[:, 17:18], lg[:, 16:17], AF.Exp, [Rlg], [Rlg])
                self.ts("dve", lg[:, 18:19], lg[:, 17:18], 1.0, None, ALU.add, None, [Rlg], [Rlg])
                self.P.op("dve", lambda e: e.reciprocal(out=lg[:, 19:20], in_=lg[:, 18:19]), [Rlg], [Rlg])
                self.tt("dve", lg[:, 20:21], lg[:, 17:18], lg[:, 19:20], ALU.mult, [Rlg], [Rlg])
                self.ts("dve", lg[:, 24:32], lg[:, 0:8], lg[:, 8:9], lg[:, 19:20], ALU.is_equal, ALU.mult, [Rlg], [Rlg])
                self.ts("dve", lg[:, 32:40], lg[:, 0:8], lg[:, 9:10], lg[:, 20:21], ALU.is_equal, ALU.mult, [Rlg], [Rlg])
                self.tt("dve", GT[:, t, :], lg[:, 24:32], lg[:, 32:40], ALU.add, [Rlg], [RGT])
        for t in range(NT):
            self.P.op("act", lambda e, t=t: e.mul(out=X[:, t, :], in_=X[:, t, :], mul=ALPHA), [RX[t]], [RX[t]])
        if moe:
            groups = [(e, c0, 4) for e in range(NEXP) for c0 in range(0, FF_MOE, 512)]
        else:
            groups = [(None, c0, min(4, (FF_DENSE - c0) // 128)) for c0 in range(0, FF_DENSE, 512)]

        def load(gi):
            e, c0, nch = groups[gi]
            wg, Rwg, wu, Rwu, wd, Rwd = WS[gi % 2]
            if moe:
                srcg, srcu, srcd = d["moe_w_gate"][li, e], d["moe_w_up"][li, e], d["moe_w_down"][li, e]
            else:
                srcg, srcu, srcd = d["ffn_w_gate"][li], d["ffn_w_up"][li], d["ffn_w_down"][li]
            w = nch * 128
            P.dma(wg[:, :, 0:w], srcg.rearrange("(c p) n -> p c n", p=128)[:, :, c0:c0 + w], writes=[Rwg], eng="pool")
            P.dma(wu[:, :, 0:w], srcu.rearrange("(c p) n -> p c n", p=128)[:, :, c0:c0 + w], writes=[Rwu], eng="pool")
            P.dma(wd[:, 0:nch, :], srcd[c0:c0 + w, :].rearrange("(c p) n -> p c n", p=128), writes=[Rwd], eng="pool")

        items = [(gi, tg) for gi in range(len(groups)) for tg in range(4)]

        def up(k):
            gi, tg = items[k]
            e, c0, nch = groups[gi]
            wg, Rwg, wu, Rwu, wd, Rwd = WS[gi % 2]
            hs = k % 2
            cols = slice(tg * 512, (tg + 1) * 512)
            for c in range(nch):
                bg = self.bank("fg", [0, 1])
                bu = self.bank("fu", [2, 3])
                for kc in range(NKC):
                    self.mm(self.psf(bg), wg[:, kc, c * 128:(c + 1) * 128], XT[:, kc, cols], kc == 0, kc == NKC - 1,
                            [Rwg] + RXT[4 * tg:4 * tg + 4], [self.RPS[bg]])
                for kc in range(NKC):
                    self.mm(self.psf(bu), wu[:, kc, c * 128:(c + 1) * 128], XT[:, kc, cols], kc == 0, kc == NKC - 1,
                            [Rwu] + RXT[4 * tg:4 * tg + 4], [self.RPS[bu]])
                s = self.bank("sg", [0, 1])
                self.act(sg[:, s, :], self.psf(bg), AF.Silu, [], [self.RPS[bg], Rsg[s]])
                self.tt("dve", hT[:, hs, c, :], self.psf(bu), sg[:, s, :], ALU.mult, [Rsg[s]], [self.RPS[bu], RhT[hs]])

        def down(k):
            gi, tg = items[k]
            e, c0, nch = groups[gi]
            wg, Rwg, wu, Rwu, wd, Rwd = WS[gi % 2]
            hs = k % 2
            for ti in range(4):
                t = 4 * tg + ti
                for hf in range(2):
                    b = self.bank("fd", [4, 5, 6, 7])
                    ps = self.psf(b)
                    for c in range(nch):
                        self.mm(ps, hT[:, hs, c, ti * 128:(ti + 1) * 128], wd[:, c, hf * 512:(hf + 1) * 512], c == 0, c == nch - 1,
                                [RhT[hs], Rwd], [self.RPS[b]])
                    xs = X[:, t, hf * 512:(hf + 1) * 512]
                    if moe:
                        self.stt("dve", xs, ps, GT[:, t, e:e + 1], xs, ALU.mult, ALU.add, [RGT, RX[t]], [self.RPS[b], RX[t]])
                    else:
                        self.tt("dve", xs, ps, xs, ALU.add, [RX[t]], [self.RPS[b], RX[t]])

        load(0)
        for k in range(len(items)):
            gi, tg = items[k]
            up(k)
            if k > 0:
                down(k - 1)
            if tg == 0 and gi + 1 < len(groups):
                load(gi + 1)
        down(len(items) - 1)
        self.layer_norm_all("ln2_g", "ln2_b", l, lnoff, need_xt=not last)

    def layer(self, l):
        if self.upto == "pro":
            return
        self.mixer(l)
        if self.upto in ("da", "mix"):
            return
        self.out_proj_ln1(l)
        if self.upto == "ln1":
            return
        self.ffn_ln2(l, last=(l == self.n_layers - 1))

    def output(self):
        self.dump_X("y")

    def debug_dump(self):
        if self.upto == "pro":
            import os
            if "xt" in os.environ.get("PRO_SKIP", ""):
                self.dump_X("dbg")
            else:
                self.dump_featmajor_bf16(self.XT[:], self.RXT)
        elif self.upto == "da":
            self.dump_featmajor_bf16(self.MT[:, 0:4, :], [r for rl in self.RMT[0:4] for r in rl])
        elif self.upto == "mix":
            self.dump_featmajor_bf16(self.MT, [r for rl in self.RMT for r in rl])
        else:
            self.dump_X("dbg")


_CONST_CACHE = {}


def _consts():
    if "cf" in _CONST_CACHE:
        return _CONST_CACHE["cf"]
    cf = np.zeros((128, 648), np.float32)
    idx = np.arange(128)
    cf[:, 0:128] = np.eye(128, dtype=np.float32)
    cf[:, 128:256] = (idx[:, None] <= idx[None, :]).astype(np.float32)
    cf[:, 256:384] = (idx[:, None] < idx[None, :]).astype(np.float32)
    cf[:, 384:512] = 1.0
    rm = np.zeros((128, 128), np.float32)
    for blk in (0, 64):
        for dd in range(32):
            rm[blk + dd + 32, blk + dd] = -1.0
            rm[blk + dd, blk + dd + 32] = 1.0
    cf[:, 512:640] = rm
    inv_freq = 10000.0 ** (-np.arange(0, 64, 2, dtype=np.float32) / 64.0)
    cf[:, 640] = (inv_freq[idx % 32].astype(np.float64) / TWO_PI).astype(np.float32)
    _CONST_CACHE["cf"] = cf
    return cf


def build_program(n_layers=DEPTH, upto=None):
    nc = bass.Bass("TRN2", target_bir_lowering=False)
    with ExitStack() as st:
        P = Prog(nc, st)
        K = Kern(nc, P, st, n_layers, upto)
        K.prologue()
        for l in range(n_layers):
            K.layer(l)
        if upto is None:
            K.output()
        else:
            K.debug_dump()
        P.finish()
    return nc


WEIGHT_KEYS = ("w_in", "conv_w", "a_log", "dt_bias", "gdn_norm_w", "lam_q1", "lam_k1", "lam_q2", "lam_k2",
               "subln_w", "w_out", "ln1_g", "ln1_b", "ln2_g", "ln2_b", "ffn_w_gate", "ffn_w_up", "ffn_w_down",
               "router_w", "moe_w_gate", "moe_w_up", "moe_w_down")


def make_in_maps(inputs, cores):
    cf = _consts()
    shared = {k: np.ascontiguousarray(np.asarray(inputs[k], dtype=np.float32)) for k in WEIGHT_KEYS}
    x = np.asarray(inputs["x"], dtype=np.float32)
    pos = np.asarray(inputs["positions"]).astype(np.int32)
    maps = []
    for b in cores:
        m = dict(shared)
        m["x"] = np.ascontiguousarray(x[b])
        m["pos"] = np.ascontiguousarray(pos[b:b + 1])
        m["cf"] = cf
        maps.append(m)
    return maps


def kernel(**inputs):
    nc = build_program()
    in_maps = make_in_maps(inputs, range(8))
    res = run_bass_kernel_spmd(nc, in_maps, core_ids=list(range(8)))
    out = np.stack([np.asarray(r["y"], dtype=np.float32) for r in res.results], axis=0)
    return out
```

```python
import numpy as np
from contextlib import ExitStack
import concourse.bass as bass
import concourse.mybir as mybir
from concourse.bass_utils import run_bass_kernel_spmd

F32 = mybir.dt.float32
F32R = mybir.dt.float32r
BF16 = mybir.dt.bfloat16
I32 = mybir.dt.int32
AF = mybir.ActivationFunctionType
ALU = mybir.AluOpType
AX = mybir.AxisListType

EPOCH = 16384
USE_F32R = True


class Res:
    __slots__ = ("w", "rs", "name")

    def __init__(self, name=""):
        self.w = None
        self.rs = []
        self.name = name


class Prog:
    ENGS = ("pe", "act", "dve", "pool", "sp")

    def __init__(self, nc, stack, n_dma_sems=32):
        self.nc = nc
        self.stack = stack
        self.streams = {e: [] for e in self.ENGS}
        self.cnt = {e: 0 for e in self.ENGS}
        self.esems = {e: [] for e in self.ENGS}
        self.seen = {e: {} for e in self.ENGS}
        self.dsems = [stack.enter_context(nc.semaphore(f"dq{i}")) for i in range(n_dma_sems)]
        self.duse = [0] * n_dma_sems
        half = n_dma_sems // 2
        self.dpool = {"sp": list(range(0, half)), "pool": list(range(half, n_dma_sems))}
        self.dnext = {"sp": 0, "pool": 0}
        self.out_events = []

    def _esem(self, eng, epoch):
        lst = self.esems[eng]
        while len(lst) <= epoch:
            lst.append(self.stack.enter_context(self.nc.semaphore(f"c_{eng}_{len(lst)}")))
        return lst[epoch]

    def _collect(self, eng, reads, writes, is_dma):
        need = {}

        def add(ev, hazard):
            if ev is None:
                return
            if ev[0] == "c":
                if ev[1] == eng and not is_dma and hazard != "raw" and eng == "pe":
                    return
                key = ("c", ev[1])
            else:
                key = ("d", ev[1])
            if need.get(key, 0) < ev[2]:
                need[key] = ev[2]

        for r in reads:
            add(r.w, "raw")
        for w in writes:
            add(w.w, "waw")
            for ev in w.rs:
                add(ev, "war")
        waits = []
        seen = self.seen[eng]
        for key, val in need.items():
            if seen.get(key, 0) >= val:
                continue
            seen[key] = val
            if key[0] == "c":
                ep, v = divmod(val - 1, EPOCH)
                waits.append((self._esem(key[1], ep), v + 1))
            else:
                waits.append((self.dsems[key[1]], val))
        return waits

    def _commit(self, ev, reads, writes):
        for r in reads:
            r.rs.append(ev)
        for w in writes:
            w.w = ev
            w.rs = []

    def op(self, eng, fn, reads=(), writes=()):
        waits = self._collect(eng, reads, writes, False)
        self.cnt[eng] += 1
        g = self.cnt[eng]
        ep, v = divmod(g - 1, EPOCH)
        self.streams[eng].append((fn, waits, (self._esem(eng, ep), 1)))
        ev = ("c", eng, g)
        self._commit(ev, reads, writes)
        return ev

    def dma(self, out, in_, reads=(), writes=(), eng="sp", is_output=False, **kw):
        lst = self.dpool[eng]
        j = lst[self.dnext[eng] % len(lst)]
        self.dnext[eng] += 1
        waits = self._collect(eng, reads, writes, True)
        prev = self.duse[j] * 16
        seen = self.seen[eng]
        if prev and seen.get(("d", j), 0) < prev:
            seen[("d", j)] = prev
            waits.append((self.dsems[j], prev))
        self.duse[j] += 1
        val = self.duse[j] * 16
        fn = lambda e, out=out, in_=in_, kw=kw: e.dma_start(out=out, in_=in_, **kw)
        self.streams[eng].append((fn, waits, (self.dsems[j], 16)))
        ev = ("d", j, val)
        self._commit(ev, reads, writes)
        if is_output:
            self.out_events.append(ev)
        return ev

    def finish(self):
        need = {}
        for ev in self.out_events:
            need[ev[1]] = max(need.get(ev[1], 0), ev[2])
        fin = [(self.dsems[j], v) for j, v in need.items()]
        nc = self.nc
        streams = self.streams
        with nc.Block() as block:
            def emit(e, lst, final=()):
                for fn, waits, inc in lst:
                    for s, v in waits:
                        e.wait_ge(s, v)
                    fn(e).then_inc(inc[0], inc[1])
                for s, v in final:
                    e.wait_ge(s, v)

            @block.tensor
            def _(e):
                emit(e, streams["pe"])

            @block.scalar
            def _(e):
                emit(e, streams["act"])

            @block.vector
            def _(e):
                emit(e, streams["dve"])

            @block.gpsimd
            def _(e):
                emit(e, streams["pool"])

            @block.sync
            def _(e):
                emit(e, streams["sp"], fin)


SEQ = 2048
DM = 1024
NT = 16
NKC = 8
DEPTH = 4
D_IN = 3592
FF_DENSE = 2816
FF_MOE = 3584
NEXP = 8
ALPHA = (2.0 * DEPTH) ** 0.25
LN_EPS = 1e-5
RMS_EPS = 1e-6
TWO_PI = 6.283185307179586
ARENA_W = 22528
MT_OFF = 0


def _dsize(dt):
    return 4 if dt in (F32, I32) else 2


class Arena:
    def __init__(self, t):
        self.t = t
        self.live = []
        self.frozen = []

    @staticmethod
    def _compress(res_list):
        d = {}
        for r in res_list:
            for ev in ([r.w] if r.w else []) + list(r.rs):
                key = (ev[0], ev[1])
                if key not in d or d[key][2] < ev[2]:
                    d[key] = ev
        return d

    def carve(self, lo, shape, dt, nres=1):
        n = 1
        for s in shape:
            n *= s
        nbytes = n * _dsize(dt)
        hi = lo + nbytes
        assert lo % 4 == 0 and hi <= ARENA_W * 4, (lo, hi)
        evs = {}
        keep = []
        for (l2, h2, rl) in self.live:
            if l2 < hi and lo < h2:
                d = self._compress(rl)
                self.frozen.append((l2, h2, d))
            else:
                keep.append((l2, h2, rl))
        newf = []
        for (l2, h2, d) in self.frozen:
            if l2 < hi and lo < h2:
                for key, ev in d.items():
                    if key not in evs or evs[key][2] < ev[2]:
                        evs[key] = ev
            if not (lo <= l2 and h2 <= hi):
                newf.append((l2, h2, d))
        self.frozen = newf
        res = [Res() for _ in range(nres)]
        for r in res:
            r.rs = list(evs.values())
        self.live = keep + [(lo, hi, res)]
        ap = self.t[:, lo // 4:(hi + 3) // 4]
        if dt != F32:
            ap = ap.bitcast(dt)
        if len(shape) == 2:
            ap = ap.rearrange("p (a b) -> p a b", a=shape[0])
        elif len(shape) == 3:
            ap = ap.rearrange("p (a b c) -> p a b c", a=shape[0], b=shape[1])
        return ap, res


class Kern:
    def __init__(self, nc, P, st, n_layers=DEPTH, upto=None):
        self.nc, self.P, self.st = nc, P, st
        self.n_layers = n_layers
        self.upto = upto
        dr = lambda name, shape, dt=F32, kind="ExternalInput": nc.dram_tensor(name, shape, dt, kind=kind).ap()
        self.d = {}
        self.d["x"] = dr("x", [SEQ, DM])
        self.d["pos"] = dr("pos", [1, SEQ], I32)
        self.d["cf"] = dr("cf", [128, 648])
        self.d["w_in"] = dr("w_in", [DEPTH, DM, D_IN])
        self.d["conv_w"] = dr("conv_w", [DEPTH, 4, 1536])
        self.d["a_log"] = dr("a_log", [DEPTH, 4])
        self.d["dt_bias"] = dr("dt_bias", [DEPTH, 4])
        self.d["gdn_norm_w"] = dr("gdn_norm_w", [DEPTH, 128])
        for k in ("lam_q1", "lam_k1", "lam_q2", "lam_k2"):
            self.d[k] = dr(k, [DEPTH, 64])
        self.d["subln_w"] = dr("subln_w", [DEPTH, 128])
        self.d["w_out"] = dr("w_out", [DEPTH, DM, DM])
        for k in ("ln1_g", "ln1_b", "ln2_g", "ln2_b"):
            self.d[k] = dr(k, [DEPTH, DM])
        self.d["ffn_w_gate"] = dr("ffn_w_gate", [2, DM, FF_DENSE])
        self.d["ffn_w_up"] = dr("ffn_w_up", [2, DM, FF_DENSE])
        self.d["ffn_w_down"] = dr("ffn_w_down", [2, FF_DENSE, DM])
        self.d["router_w"] = dr("router_w", [2, DM, NEXP])
        self.d["moe_w_gate"] = dr("moe_w_gate", [2, NEXP, DM, FF_MOE])
        self.d["moe_w_up"] = dr("moe_w_up", [2, NEXP, DM, FF_MOE])
        self.d["moe_w_down"] = dr("moe_w_down", [2, NEXP, FF_MOE, DM])
        self.d["y"] = dr("y", [SEQ, DM], F32, "ExternalOutput")
        if upto is not None:
            self.d["dbg"] = dr("dbg", [SEQ, DM], F32, "ExternalOutput")

        sb = lambda name, shape, dt: st.enter_context(nc.sbuf_tensor(name, shape, dt))
        self.X = sb("X", [128, NT, DM], F32)
        self.RX = [Res(f"X{t}") for t in range(NT)]
        self.XT = sb("XT", [128, NKC, SEQ], BF16)
        self.RXT = [Res(f"XT{t}") for t in range(NT)]
        self.CF = sb("CF", [128, 648], F32)
        self.RCF = Res("CF")
        self.CB = sb("CB", [128, 5, 128], BF16)
        self.RCB = Res("CB")
        self.cosT = sb("cosT", [128, SEQ], BF16)
        self.sinT = sb("sinT", [128, SEQ], BF16)
        self.RTAB = Res("tab")
        self.SM = sb("SM", [128, 1024], F32)
        self.Rsm = Res("sm")
        self.AR = Arena(sb("AR", [128, ARENA_W], F32))
        self.RR = sb("RR", [128, 16, 128], F32)
        self.RRr = [Res(f"rr{i}") for i in range(16)]
        self.PSB = [st.enter_context(nc.psum_tensor(f"ps{b}", [128, 512], F32)) for b in range(8)]
        self.RPS = [Res(f"ps{b}") for b in range(8)]
        self.pools = {}
        self.identf = self.CF[:, 0:128]
        self.Uf = self.CF[:, 128:256]
        self.Usf = self.CF[:, 256:384]
        self.onesf = self.CF[:, 384:512]
        self.invf = self.CF[:, 640:641]
        self.identb = self.CB[:, 0, :]
        self.Ub = self.CB[:, 1, :]
        self.onesb = self.CB[:, 2, :]
        self.Rmb = self.CB[:, 3, :]

    def bank(self, pool, banks=None):
        if pool not in self.pools:
            self.pools[pool] = [banks, 0]
        lst, c = self.pools[pool]
        self.pools[pool][1] += 1
        b = lst[c % len(lst)]
        return b

    def psf(self, b):
        return self.PSB[b][:]

    def psb16(self, b):
        return self.PSB[b][:].bitcast(BF16)

    def mm(self, out, lhsT, rhs, start, stop, reads, writes):
        self.P.op("pe", lambda e: e.matmul(out, lhsT=lhsT, rhs=rhs, start=start, stop=stop), reads, writes)

    def mmr(self, out, lhsT, rhs, start, stop, reads, writes):
        if not USE_F32R:
            return self.mm(out, lhsT, rhs, start, stop, reads, writes)
        a, b = lhsT.bitcast(F32R), rhs.bitcast(F32R)
        self.P.op("pe", lambda e: e.matmul(out, lhsT=a, rhs=b, start=start, stop=stop), reads, writes)

    def tr(self, out, in_, ident, reads, writes):
        self.P.op("pe", lambda e: e.transpose(out, in_, ident), reads, writes)

    def act(self, out, in_, func, reads, writes, **kw):
        self.P.op("act", lambda e: e.activation(out=out, in_=in_, func=func, **kw), reads, writes)

    def tt(self, eng, out, in0, in1, op, reads, writes):
        self.P.op(eng, lambda e: e.tensor_tensor(out=out, in0=in0, in1=in1, op=op), reads, writes)

    def ts(self, eng, out, in0, s1, s2, op0, op1, reads, writes, **kw):
        if op1 is None:
            self.P.op(eng, lambda e: e.tensor_scalar(out=out, in0=in0, scalar1=s1, scalar2=None, op0=op0, **kw), reads, writes)
        else:
            self.P.op(eng, lambda e: e.tensor_scalar(out=out, in0=in0, scalar1=s1, scalar2=s2, op0=op0, op1=op1, **kw), reads, writes)

    def stt(self, eng, out, in0, scalar, in1, op0, op1, reads, writes, **kw):
        self.P.op(eng, lambda e: e.scalar_tensor_tensor(out=out, in0=in0, scalar=scalar, in1=in1, op0=op0, op1=op1, **kw), reads, writes)

    def mmr2(self, out, lhsT, rhs, start, stop, reads, writes):
        return self.mmr(out, lhsT, rhs, start, stop, reads, writes)

    def cp(self, eng, out, in_, reads, writes):
        if eng == "act":
            self.P.op("act", lambda e: e.activation(out=out, in_=in_, func=AF.Copy), reads, writes)
        else:
            self.P.op(eng, lambda e: e.tensor_copy(out=out, in_=in_), reads, writes)

    def memset(self, eng, ap, val, writes):
        self.P.op(eng, lambda e: e.memset(ap, val), (), writes)

    def prologue(self):
        P, d = self.P, self.d
        P.dma(self.CF[:], d["cf"], writes=[self.RCF])
        for i, lo in enumerate([0, 128, 384, 512]):
            P.dma(self.CB[:, i, :], d["cf"][:, lo:lo + 128], writes=[self.RCB], eng="pool")
        xr = d["x"].rearrange("(t p) d -> p t d", p=128)
        for t in range(NT):
            P.dma(self.X[:, t, :], xr[:, t, :], writes=[self.RX[t]])
        import os
        skip = os.environ.get("PRO_SKIP", "")
        if "rope" in skip:
            if "xt" not in skip:
                self.make_xt(range(NT))
            return
        A = self.AR
        base = 32768
        posi, (Rp,) = A.carve(base, [SEQ], I32)
        r, (Rr,) = A.carve(base + 8192, [SEQ], F32)
        r2, (Rr2,) = A.carve(base + 16384, [SEQ], F32)
        ki, (Rki,) = A.carve(base + 24576, [SEQ], I32)
        kf, (Rkf,) = A.carve(base + 32768, [SEQ], F32)
        mk, (Rmk,) = A.carve(base + 40960, [SEQ], F32)
        P.dma(posi, d["pos"].broadcast_to([128, SEQ]), writes=[Rp])
        if "s1" in skip:
            self.make_xt(range(NT)); return
        self.cp("dve", r, posi, [Rp], [Rr])
        self.ts("dve", r, r, self.invf, None, ALU.mult, None, [Rr, self.RCF], [Rr])
        if "s2" in skip:
            self.make_xt(range(NT)); return
        for tab, shift in ((self.sinT, 0.0), (self.cosT, 0.25)):
            self.ts("dve", r2, r, shift, None, ALU.add, None, [Rr], [Rr2])
            self.cp("dve", ki, r2, [Rr2], [Rki])
            self.cp("dve", kf, ki, [Rki], [Rkf])
            self.tt("dve", r2, r2, kf, ALU.subtract, [Rr2, Rkf], [Rr2])
            self.ts("dve", mk, r2, 0.5, None, ALU.is_gt, None, [Rr2], [Rmk])
            self.tt("dve", r2, r2, mk, ALU.subtract, [Rr2, Rmk], [Rr2])
            self.ts("dve", mk, r2, -0.5, None, ALU.is_lt, None, [Rr2], [Rmk])
            self.tt("dve", r2, r2, mk, ALU.add, [Rr2, Rmk], [Rr2])
            if "s3" in skip:
                if "dumpr2" in skip:
                    k0 = 0 if shift == 0.0 else 2
                    self.cp("dve", self.X[:, k0, :], r2[:, 0:1024], [Rr2], [self.RX[k0]])
                    self.cp("dve", self.X[:, k0 + 1, :], r2[:, 1024:2048], [Rr2], [self.RX[k0 + 1]])
                continue
            self.act(tab[:], r2, AF.Sin, [Rr2], [self.RTAB], scale=TWO_PI * (1.0 - 1e-6))
        self.make_xt(range(NT))

    def make_xt(self, tiles):
        A = self.AR
        xb2, Rxb = A.carve(32768 + 49152, [2, DM], BF16, nres=2)
        for t in tiles:
            s = t % 2
            self.cp("act", xb2[:, s, :], self.X[:, t, :], [self.RX[t]], [Rxb[s]])
            b = self.bank("tp", [6, 7])
            pv = self.psb16(b)
            for kc in range(NKC):
                self.tr(pv[:, kc * 128:(kc + 1) * 128], xb2[:, s, kc * 128:(kc + 1) * 128], self.identb,
                        [Rxb[s], self.RCB], [self.RPS[b]])
            self.cp("dve", self.XT[:, :, t * 128:(t + 1) * 128], pv.rearrange("p (c n) -> p c n", c=NKC),
                    [], [self.RPS[b], self.RXT[t]])

    def dump_featmajor_bf16(self, ap3, res_list):
        C = ap3.shape[1]
        dst = self.d["dbg"].rearrange("(a b) d -> a (b d)", b=2)
        dst = dst.rearrange("(c p) t -> p c t", p=128)
        self.P.dma(dst[:, 0:C, :], ap3, reads=res_list, eng="pool", is_output=True)

    def dump_X(self, name="dbg"):
        yr = self.d[name].rearrange("(t p) d -> p t d", p=128)
        for t in range(NT):
            self.P.dma(yr[:, t, :], self.X[:, t, :], reads=[self.RX[t]], is_output=True)

    def layer_params(self, l):
        P, d, SM = self.P, self.d, self.SM
        Rsm = self.Rsm
        lam_init = 0.8 - 0.6 * float(np.exp(-0.3 * l))
        self.lam_init = lam_init
        for i, k in enumerate(("lam_q1", "lam_k1", "lam_q2", "lam_k2")):
            P.dma(SM[:, i * 64:(i + 1) * 64], d[k][l:l + 1, :].broadcast_to([128, 64]), writes=[Rsm])
        P.dma(SM[:, 256:384], d["subln_w"][l:l + 1, :].broadcast_to([128, 128]), writes=[Rsm])
        P.dma(SM[:, 384:512], d["gdn_norm_w"][l:l + 1, :].broadcast_to([128, 128]), writes=[Rsm])
        P.dma(SM[:, 512:516], d["dt_bias"][l:l + 1, :].broadcast_to([128, 4]), writes=[Rsm])
        P.dma(SM[:, 516:520], d["a_log"][l:l + 1, :].broadcast_to([128, 4]), writes=[Rsm])
        self.stt("dve", SM[:, 960:1024], SM[:, 0:64], 1.0, SM[:, 64:128], ALU.mult, ALU.mult, [Rsm], [Rsm], accum_out=SM[:, 520:521])
        self.stt("dve", SM[:, 960:1024], SM[:, 128:192], 1.0, SM[:, 192:256], ALU.mult, ALU.mult, [Rsm], [Rsm], accum_out=SM[:, 521:522])
        self.act(SM[:, 522:524], SM[:, 520:522], AF.Exp, [Rsm], [Rsm])
        self.tt("dve", SM[:, 520:521], SM[:, 522:523], SM[:, 523:524], ALU.subtract, [Rsm], [Rsm])
        self.ts("dve", SM[:, 524:525], SM[:, 520:521], -1.0, -lam_init, ALU.mult, ALU.add, [Rsm], [Rsm])
        self.neglam = SM[:, 524:525]
        self.ts("dve", SM[:, 256:384], SM[:, 256:384], 1.0 - lam_init, None, ALU.mult, None, [Rsm], [Rsm])
        self.WSUB = SM[:, 256:384]
        self.GNW = SM[:, 384:512]
        self.act(SM[:, 516:520], SM[:, 516:520], AF.Exp, [Rsm], [Rsm])
        self.ts("dve", SM[:, 516:520], SM[:, 516:520], -1.0, None, ALU.mult, None, [Rsm], [Rsm])

    def layer_norm_all(self, gname, bname, l, lnoff, need_xt=True):
        P, d, A = self.P, self.d, self.AR
        LNG, (Rg,) = A.carve(lnoff, [DM], F32)
        LNB, (Rb,) = A.carve(lnoff + 4096, [DM], F32)
        junk, (Rj,) = A.carve(lnoff + 8192, [DM], F32)
        ST, (Rs,) = A.carve(lnoff + 12288, [128], F32)
        junk2, (Rj2,) = A.carve(lnoff + 12800, [DM], F32)
        P.dma(LNG, d[gname][l:l + 1, :].broadcast_to([128, DM]), writes=[Rg])
        P.dma(LNB, d[bname][l:l + 1, :].broadcast_to([128, DM]), writes=[Rb])
        X, RX = self.X, self.RX
        for t in range(NT):
            self.act(junk, X[:, t, :], AF.Identity, [RX[t]], [Rj, Rs], accum_out=ST[:, t:t + 1])
            self.stt("dve", junk2, X[:, t, :], 1.0, X[:, t, :], ALU.mult, ALU.mult, [RX[t]], [Rj2, Rs], accum_out=ST[:, 16 + t:17 + t])
        self.ts("dve", ST[:, 0:32], ST[:, 0:32], 1.0 / DM, None, ALU.mult, None, [Rs], [Rs])
        self.tt("dve", ST[:, 32:48], ST[:, 0:16], ST[:, 0:16], ALU.mult, [Rs], [Rs])
        self.tt("dve", ST[:, 48:64], ST[:, 16:32], ST[:, 32:48], ALU.subtract, [Rs], [Rs])
        self.act(ST[:, 64:80], ST[:, 48:64], AF.Ln, [Rs], [Rs], bias=LN_EPS)
        self.act(ST[:, 80:96], ST[:, 64:80], AF.Exp, [Rs], [Rs], scale=-0.5)
        self.stt("dve", ST[:, 96:112], ST[:, 0:16], -1.0, ST[:, 80:96], ALU.mult, ALU.mult, [Rs], [Rs])
        for t in range(NT):
            self.act(X[:, t, :], X[:, t, :], AF.Identity, [RX[t], Rs], [RX[t]], scale=ST[:, 80 + t:81 + t], bias=ST[:, 96 + t:97 + t])
            self.tt("dve", X[:, t, :], X[:, t, :], LNG, ALU.mult, [RX[t], Rg], [RX[t]])
            self.tt("pool", X[:, t, :], X[:, t, :], LNB, ALU.add, [RX[t], Rb], [RX[t]])
        if need_xt:
            self.make_xt(range(NT))

    def da_head(self, l, h):
        P, d, A = self.P, self.d, self.AR
        XT, RXT = self.XT, self.RXT
        base = 32768
        W2, RW2 = A.carve(base, [2, NKC, 384], BF16, nres=2) if h == 0 else (self._daW, self._daRW)
        self._daW, self._daRW = W2, RW2
        W, RW = W2[:, h % 2], RW2[h % 2]
        o = base + 12288
        qk, Rqk = A.carve(o, [2, SEQ], BF16, nres=2); o += 8192
        V, (RV,) = A.carve(o, [NT, 132], BF16); o += 4224
        rawb, Rraw = A.carve(o, [8, 512], BF16, nres=8); o += 8192
        t1, Rt1 = A.carve(o, [2, 512], F32, nres=2); o += 4096
        t2, Rt2 = A.carve(o, [2, 512], F32, nres=2); o += 4096
        sq, Rsq = A.carve(o, [2, 512], BF16, nres=2); o += 2048
        ET, RET = A.carve(o, [4, 256], BF16, nres=4); o += 2048
        ep_t, Rept = A.carve(o, [2, 128], F32, nres=2); o += 1024
        ep_o, Repo = A.carve(o, [2, 128], F32, nres=2); o += 1024
        ep_j, (Repj,) = A.carve(o, [128], F32); o += 512
        ep_n, Repn = A.carve(o, [2, 128], BF16, nres=2); o += 512
        st, (Rst, RnegM, Rst_a, Rst_b) = A.carve(o, [64], F32, nres=4); o += 256
        Rst2 = [Rst_a, Rst_b]
        U2, (RU2,) = A.carve(o, [2, 128], BF16); o += 512
        kz, (Rkz,) = A.carve(o, [NT, 2, 128], BF16); o += 8192
        if h == 0:
            self.memset("pool", kz[64:128, :, 0, :], 0.0, [Rkz])
            self.memset("pool", kz[0:64, :, 1, :], 0.0, [Rkz])
        win = d["w_in"][l].rearrange("(c p) n -> p c n", p=128)

        def load_w(hh):
            Wd_, RWd_ = W2[:, hh % 2], RW2[hh % 2]
            for i, c0 in enumerate((hh * 128, 512 + hh * 128, 1024 + hh * 128)):
                P.dma(Wd_[:, :, i * 128:(i + 1) * 128], win[:, :, c0:c0 + 128], writes=[RWd_], eng="pool")
        if h == 0:
            load_w(0)
        self.ts("dve", U2[:, 0, :], self.Ub, -1.0, 30000.0, ALU.add, ALU.mult, [self.RCB], [RU2])
        self.ts("dve", U2[:, 1, :], self.Ub, -1.0, 30000.0, ALU.add, ALU.mult, [self.RCB], [RU2])
        its = [(which, tg) for which in range(2) for tg in range(4)]
        for it, (which, tg) in enumerate(its):
            cols = slice(tg * 512, (tg + 1) * 512)
            b = self.bank("proj", [0, 1])
            ps = self.psf(b)
            for kc in range(NKC):
                self.mm(ps, W[:, kc, which * 128:(which + 1) * 128], XT[:, kc, cols], kc == 0, kc == NKC - 1,
                        [RW] + RXT[4 * tg:4 * tg + 4], [self.RPS[b]])
            self.cp("act", rawb[:, it, :], ps, [], [self.RPS[b], Rraw[it]])
        for it, (which, tg) in enumerate(its):
            s = it % 2
            cols = slice(tg * 512, (tg + 1) * 512)
            b2 = self.bank("rot", [2, 3])
            ps2 = self.psf(b2)
            self.mm(ps2, self.Rmb, rawb[:, it, :], True, True, [self.RCB, Rraw[it]], [self.RPS[b2]])
            self.tt("dve", t1[:, s, :], rawb[:, it, :], self.cosT[:, cols], ALU.mult, [self.RTAB, Rraw[it]], [Rt1[s]])
            self.tt("dve", t2[:, s, :], ps2, self.sinT[:, cols], ALU.mult, [self.RTAB], [self.RPS[b2], Rt2[s]])
            self.tt("dve", qk[:, which, cols], t1[:, s, :], t2[:, s, :], ALU.add, [Rt1[s], Rt2[s]], [Rqk[which]])
            self.act(sq[:, s, :], qk[:, which, cols], AF.Square, [Rqk[which]], [Rsq[s]])
            if which == 0:
                self.cp("act", kz[0:64, 4 * tg:4 * tg + 4, 0, :], qk[0:64, 0, cols].rearrange("p (a n) -> p a n", a=4), [Rqk[0]], [Rkz])
                self.cp("pool", kz[64:128, 4 * tg:4 * tg + 4, 1, :], qk[64:128, 0, cols].rearrange("p (a n) -> p a n", a=4), [Rqk[0]], [Rkz])
            b3 = self.bank("nrm", [4, 5])
            ps3 = self.psf(b3)
            self.mm(ps3, self.onesb, sq[:, s, :], True, True, [self.RCB, Rsq[s]], [self.RPS[b3]])
            self.P.op("dve", lambda e, o_=st[:, which * 4 + tg:which * 4 + tg + 1], i_=ps3: e.reduce_max(out=o_, in_=i_, axis=AX.X),
                      [], [self.RPS[b3], Rst])
        self.P.op("dve", lambda e: e.reduce_max(out=st[:, 8:9], in_=st[:, 0:4], axis=AX.X), [Rst], [Rst])
        self.P.op("dve", lambda e: e.reduce_max(out=st[:, 9:10], in_=st[:, 4:8], axis=AX.X), [Rst], [Rst])
        self.tt("dve", st[:, 10:11], st[:, 8:9], st[:, 9:10], ALU.mult, [Rst], [Rst])
        self.act(st[:, 11:12], st[:, 10:11], AF.Ln, [Rst], [Rst], bias=1e-30)
        self.act(st[:, 12:13], st[:, 11:12], AF.Exp, [Rst], [Rst], scale=0.5)
        self.ts("dve", st[:, 13:14], st[:, 12:13], -1.05 / 8.0, None, ALU.mult, None, [Rst], [RnegM])
        negM = st[:, 13:14]
        self.memset("pool", V[:, :, 128:129], 1.0, [RV])
        VT, RVT = rawb, Rraw
        for tg in range(4):
            b = self.bank("proj", [0, 1])
            ps = self.psf(b)
            for kc in range(NKC):
                self.mm(ps, W[:, kc, 256:384], XT[:, kc, tg * 512:(tg + 1) * 512], kc == 0, kc == NKC - 1,
                        [RW] + RXT[4 * tg:4 * tg + 4], [self.RPS[b]])
            self.cp("act", VT[:, tg, :], ps, [], [self.RPS[b], RVT[tg]])
            b2 = self.bank("rot", [2, 3])
            pv = self.psb16(b2)
            for ti in range(4):
                self.tr(pv[:, ti * 128:(ti + 1) * 128], VT[:, tg, ti * 128:(ti + 1) * 128], self.identb, [RVT[tg], self.RCB], [self.RPS[b2]])
            self.cp("dve", V[:, 4 * tg:4 * tg + 4, 0:128], pv[:, 0:512].rearrange("p (a n) -> p a n", a=4), [], [self.RPS[b2], RV])
        if h + 1 < 4:
            load_w(h + 1)
        qT, kT = qk[:, 0, :], qk[:, 1, :]
        pairs = [(i, j) for i in range(NT) for j in range(i + 1)]
        info = {}

        def emit_st(n):
            i, j = pairs[n]
            b = self.bank("st", [0, 1, 6])
            ps = self.psf(b)
            self.mm(ps[:, 0:256], kT[:, j * 128:(j + 1) * 128], kz[:, i, :, :].rearrange("p c n -> p (c n)"), True, i != j,
                    [Rqk[1], Rkz], [self.RPS[b]])
            if i == j:
                self.mm(ps[:, 0:256], self.identb, U2.rearrange("p a n -> p (a n)"), False, True,
                        [self.RCB, RU2], [self.RPS[b]])
            e = n % 4
            self.act(ET[:, e, :], ps[:, 0:256], AF.Exp, [RnegM], [self.RPS[b], RET[e]], scale=0.125, bias=negM)

        def emit_pv(n):
            i, j = pairs[n]
            e = n % 4
            for c in range(2):
                b = [2, 3, 4, 5][2 * c + (i % 2)]
                self.mm(self.psf(b)[:, 0:129], ET[:, e, c * 128:(c + 1) * 128], V[:, j, 0:129], j == 0, j == i,
                        [RET[e], RV], [self.RPS[b]])

        def epilogue(i):
            s = i % 2
            b0, b1 = [2, 3][s], [4, 5][s]
            O0, O1 = self.psf(b0), self.psf(b1)
            Rst = Rst2[s]
            c = 16 + 4 * s
            self.P.op("dve", lambda e: e.reciprocal(out=st[:, c:c + 1], in_=O0[:, 128:129]), [], [self.RPS[b0], Rst])
            self.P.op("dve", lambda e: e.reciprocal(out=st[:, c + 1:c + 2], in_=O1[:, 128:129]), [], [self.RPS[b1], Rst])
            self.tt("dve", st[:, c + 2:c + 3], st[:, c + 1:c + 2], self.neglam, ALU.mult, [Rst, self.Rsm], [Rst])
            self.ts("dve", ep_t[:, s, :], O1[:, 0:128], st[:, c + 2:c + 3], None, ALU.mult, None, [Rst], [self.RPS[b1], Rept[s]])
            self.stt("dve", ep_o[:, s, :], O0[:, 0:128], st[:, c:c + 1], ep_t[:, s, :], ALU.mult, ALU.add, [Rst, Rept[s]], [self.RPS[b0], Repo[s]])
            self.stt("dve", ep_j, ep_o[:, s, :], 1.0, ep_o[:, s, :], ALU.mult, ALU.mult, [Repo[s]], [Repj, Rst], accum_out=st[:, c + 3:c + 4])
            self.act(st[:, 24 + s:25 + s], st[:, c + 3:c + 4], AF.Ln, [Rst], [Rst], scale=1.0 / 128.0, bias=RMS_EPS)
            self.act(st[:, 26 + s:27 + s], st[:, 24 + s:25 + s], AF.Exp, [Rst], [Rst], scale=-0.5)
            self.stt("dve", ep_n[:, s, :], ep_o[:, s, :], st[:, 26 + s:27 + s], self.WSUB, ALU.mult, ALU.mult, [Repo[s], Rst, self.Rsm], [Repn[s]])
            bt = 7
            pv = self.psb16(bt)
            self.tr(pv[:, 0:128], ep_n[:, s, :], self.identb, [Repn[s], self.RCB], [self.RPS[bt]])
            self.cp("dve", self.MT[:, h, i * 128:(i + 1) * 128], pv[:, 0:128], [], [self.RPS[bt], self.RMT[h][i]])

        emit_st(0)
        emit_st(1)
        pending = []
        for n in range(len(pairs)):
            if n + 2 < len(pairs):
                emit_st(n + 2)
            i, j = pairs[n]
            while pending and pending[0][1] <= i - 2:
                epilogue(pending.pop(0)[1])
            emit_pv(n)
            if i == j:
                pending.append((n + 3, i))
            while pending and pending[0][0] <= n:
                epilogue(pending.pop(0)[1])
        while pending:
            epilogue(pending.pop(0)[1])

    def gdn_prep(self, l):
        P, d, A, SM = self.P, self.d, self.AR, self.SM
        XT, RXT = self.XT, self.RXT
        Rsm = self.Rsm
        base = 32768
        WAB, (Rwab,) = A.carve(base, [NKC, 8], BF16)
        GS, (Rgs,) = A.carve(base + 1024, [6, 64], F32)
        CWR, (Rcwr,) = A.carve(base + 4096, [1536], F32)
        CW, (Rcw,) = A.carve(base + 4096 + 6144, [12, 4], F32)
        self.GS, self.Rgs, self.CW, self.Rcw = GS, Rgs, CW, Rcw
        win = d["w_in"][l].rearrange("(c p) n -> p c n", p=128)
        P.dma(WAB, win[:, :, 3584:3592], writes=[Rwab], eng="pool")
        P.dma(CWR[0:4, :], d["conv_w"][l], writes=[Rcwr])
        b = self.bank("misc", [6, 7])
        ps = self.psf(b)
        for t in range(NT):
            for kc in range(NKC):
                self.mm(ps[:, t * 8:(t + 1) * 8], XT[:, kc, t * 128:(t + 1) * 128], WAB[:, kc, :], kc == 0, kc == NKC - 1,
                        [Rwab, RXT[t]], [self.RPS[b]])
        AB = SM[:, 640:768]
        self.cp("dve", AB, ps[:, 0:128], [], [self.RPS[b], Rsm])
        AB3 = AB.rearrange("p (t e) -> p t e", e=8)
        g3 = SM[:, 768:832].rearrange("p (t h) -> p t h", h=4)
        be3 = SM[:, 832:896].rearrange("p (t h) -> p t h", h=4)
        nb3 = SM[:, 896:960].rearrange("p (t h) -> p t h", h=4)
        tmp = SM[:, 960:976]
        for h in range(4):
            self.ts("dve", tmp, AB3[:, :, h], SM[:, 512 + h:513 + h], None, ALU.add, None, [Rsm], [Rsm])
            self.act(tmp, tmp, AF.Exp, [Rsm], [Rsm])
            self.act(tmp, tmp, AF.Ln, [Rsm], [Rsm], bias=1.0)
            self.ts("dve", g3[:, :, h], tmp, SM[:, 516 + h:517 + h], None, ALU.mult, None, [Rsm], [Rsm])
        self.act(SM[:, 832:896].rearrange("p (t h) -> p t h", h=4), AB3[:, :, 4:8], AF.Sigmoid, [Rsm], [Rsm])
        self.ts("dve", SM[:, 896:960], SM[:, 832:896], -1.0, None, ALU.mult, None, [Rsm], [Rsm])
        self.g3, self.be3, self.nb3 = g3, be3, nb3
        b = self.bank("misc", [6, 7])
        ps = self.psf(b)
        self.mm(ps[:, 0:64], self.Uf, SM[:, 768:832], True, True, [self.RCF, Rsm], [self.RPS[b]])
        self.mm(ps[:, 64:128], self.onesf, SM[:, 768:832], True, True, [self.RCF, Rsm], [self.RPS[b]])
        self.cp("dve", GS[:, 0, :], ps[:, 0:64], [], [self.RPS[b], Rgs])
        self.act(GS[:, 1, :], ps[:, 0:64], AF.Exp, [], [self.RPS[b], Rgs])
        self.act(GS[:, 3, :], ps[:, 64:128], AF.Exp, [], [self.RPS[b], Rgs])
        self.tt("dve", GS[:, 4, :], ps[:, 64:128], GS[:, 0, :], ALU.subtract, [Rgs], [self.RPS[b], Rgs])
        self.act(GS[:, 2, :], GS[:, 4, :], AF.Exp, [Rgs], [Rgs])
        b = self.bank("misc", [6, 7])
        ps = self.psf(b)
        for c in range(12):
            self.mm(ps[:, c * 4:(c + 1) * 4], CWR[0:4, c * 128:(c + 1) * 128], self.identf[0:4, 0:4], True, True,
                    [Rcwr, self.RCF], [self.RPS[b]])
        self.cp("dve", CW, ps[:, 0:48].rearrange("p (c j) -> p c j", j=4), [], [self.RPS[b], Rcw])

    def gdn_head(self, l, h):
        P, d, A, SM = self.P, self.d, self.AR, self.SM
        XT, RXT = self.XT, self.RXT
        Rsm, GS, Rgs = self.Rsm, self.GS, self.Rgs
        o = 43264
        WG, (RWG,) = A.carve(o, [NKC, 512], BF16); o += 8192
        qkv, Rqkv = A.carve(o, [3, SEQ], BF16, nres=3); o += 12288
        SZ, (RSZ,) = A.carve(o, [NT, 128], BF16); o += 4096
        area = o
        Raw3, RRaw3 = A.carve(area, [3, 2052], BF16, nres=3)
        accs, Raccs = A.carve(area + 12312, [4, 512], BF16, nres=4)
        DW, (RDW,) = A.carve(area + 16408, [12, 128], BF16)
        sqb, Rsqb = A.carve(36864, [2, 512], BF16, nres=2)
        rn, Rrn = A.carve(38912, [2, 512], F32, nres=2)
        win = d["w_in"][l].rearrange("(c p) n -> p c n", p=128)
        for i, c0 in enumerate((1536, 2048, 2560, 3072)):
            P.dma(WG[:, :, i * 128:(i + 1) * 128], win[:, :, c0 + h * 128:c0 + (h + 1) * 128], writes=[RWG], eng="pool")
        for which in range(3):
            self.memset("pool", Raw3[:, which, 0:3], 0.0, [RRaw3[which]])
            for j in range(4):
                self.ts("dve", DW[:, which * 4 + j, :], self.identb, self.CW[:, which * 4 + h, j:j + 1], None, ALU.mult, None,
                        [self.RCB, self.Rcw], [RDW])
        for which in range(3):
            for tg in range(4):
                b = self.bank("g", list(range(8)))
                ps = self.psf(b)
                for kc in range(NKC):
                    self.mm(ps, WG[:, kc, which * 128:(which + 1) * 128], XT[:, kc, tg * 512:(tg + 1) * 512], kc == 0, kc == NKC - 1,
                            [RWG] + RXT[4 * tg:4 * tg + 4], [self.RPS[b]])
                self.cp("act", Raw3[:, which, 3 + tg * 512:3 + (tg + 1) * 512], ps, [], [self.RPS[b], RRaw3[which]])
        cnt = 0
        for which in (2, 0, 1):
            for tg in range(4):
                cols = slice(tg * 512, (tg + 1) * 512)
                b = self.bank("g", list(range(8)))
                ps = self.psf(b)
                for j in range(4):
                    self.mm(ps, DW[:, which * 4 + j, :], Raw3[:, which, j + tg * 512:j + (tg + 1) * 512], j == 0, j == 3,
                            [RDW, RRaw3[which]], [self.RPS[b]])
                if which == 2:
                    self.act(qkv[:, 2, cols], ps, AF.Silu, [], [self.RPS[b], Rqkv[2]])
                else:
                    self.act(accs[:, tg, :], ps, AF.Silu, [], [self.RPS[b], Raccs[tg]])
            if which == 2:
                continue
            for tg in range(4):
                cols = slice(tg * 512, (tg + 1) * 512)
                s = cnt % 2
                cnt += 1
                self.tt("pool", sqb[:, s, :], accs[:, tg, :], accs[:, tg, :], ALU.mult, [Raccs[tg]], [Rsqb[s]])
                b = self.bank("g", list(range(8)))
                ps = self.psf(b)
                self.mm(ps, self.onesb, sqb[:, s, :], True, True, [self.RCB, Rsqb[s]], [self.RPS[b]])
                self.act(rn[:, s, :], ps, AF.Ln, [], [self.RPS[b], Rrn[s]], bias=RMS_EPS)
                self.act(rn[:, s, :], rn[:, s, :], AF.Exp, [Rrn[s]], [Rrn[s]], scale=-0.5, bias=(-0.5 * float(np.log(128.0)) if which == 0 else 0.0))
                self.tt("dve", qkv[:, which, cols], accs[:, tg, :], rn[:, s, :], ALU.mult, [Raccs[tg], Rrn[s]], [Rqkv[which]])
        SZT = SZ.rearrange("p a n -> p (a n)")
        for tg in range(4):
            b = self.bank("g", list(range(8)))
            ps = self.psf(b)
            for kc in range(NKC):
                self.mm(ps, WG[:, kc, 384:512], XT[:, kc, tg * 512:(tg + 1) * 512], kc == 0, kc == NKC - 1,
                        [RWG] + RXT[4 * tg:4 * tg + 4], [self.RPS[b]])
            self.act(SZT[:, tg * 512:(tg + 1) * 512], ps, AF.Silu, [], [self.RPS[b], RSZ])
        mats, Rm0 = A.carve(area, [16, 128], F32, nres=16)
        bm, Rb = A.carve(area + 16 * 512, [20, 128], BF16, nres=20)
        def _loc(i):
            if i < 26:
                bp, k = divmod(i, 13)
                if k < 5:
                    return ("a", bp * 5 + k)
                return ("r", bp * 8 + (k - 5))
            return ("a", 10 + (i - 26))

        class _RmProxy:
            def __getitem__(_s, i):
                kind, j = _loc(i)
                return Rm0[j] if kind == "a" else self.RRr[j]
        Rm = _RmProxy()

        def M(i):
            kind, j = _loc(i)
            return mats[:, j, :] if kind == "a" else self.RR[:, j, :]

        def MR(i):
            kind, j = _loc(i)
            assert kind == "r"
            return self.RR[:, j, :].bitcast(F32R) if USE_F32R else self.RR[:, j, :]
        B = lambda i: bm[:, i, :]
        qT, kT, vT = qkv[:, 0, :], qkv[:, 1, :], qkv[:, 2, :]
        S_i = [17, 18]
        VN, OG = 16, 19
        JK, ON = 30, 31
        og = B(OG)
        self.memset("pool", B(S_i[0]), 0.0, [Rb[S_i[0]]])
        Uf, Usf, identf, onesf = self.Uf, self.Usf, self.identf, self.onesf
        RCF = self.RCF
        g3, be3, nb3 = self.g3, self.be3, self.nb3
        gs = lambda k, n: GS[:, k, n * 4 + h:n * 4 + h + 1]
        busy = set()
        rr = [0]

        def newbank():
            for _ in range(8):
                b = rr[0] % 8
                rr[0] += 1
                if b not in busy:
                    busy.add(b)
                    return b
            raise RuntimeError("no free PSUM bank")

        def rel(b):
            busy.discard(b)

        def pre_steps(n, bpos, par):
            T = lambda k: bpos * 13 + k
            Pb = lambda k: par * 8 + bpos * 4 + k
            Pu = 26 + par * 2 + bpos
            cs = slice(n * 128, (n + 1) * 128)
            beta = be3[:, n, h:h + 1]
            st = {}
            steps = []

            def s0():
                self.act(M(T(0)), onesf, AF.Copy, [RCF, Rsm], [Rm[T(0)]], scale=g3[:, n, h:h + 1])
                b = newbank(); st["G"] = b
                self.mm(self.psf(b)[:, 0:128], M(T(0)), Uf, True, True, [Rm[T(0)], RCF], [self.RPS[b]])
                b = newbank(); st["A"] = b
                self.mm(self.psf(b)[:, 0:128], kT[:, cs], kT[:, cs], True, True, [Rqkv[1]], [self.RPS[b]])
                self.mm(self.psf(b)[:, 128:256], kT[:, cs], qT[:, cs], True, True, [Rqkv[0], Rqkv[1]], [self.RPS[b]])
                b = newbank(); st["KV"] = b
                pv = self.psb16(b)
                self.tr(pv[:, 0:128], kT[:, cs], self.identb, [Rqkv[1], self.RCB], [self.RPS[b]])
                self.tr(pv[:, 128:256], vT[:, cs], self.identb, [Rqkv[2], self.RCB], [self.RPS[b]])
            steps.append(s0)

            def s1():
                b = st["G"]
                self.ts("dve", M(T(1)), self.psf(b)[:, 0:128], gs(0, n), 0.0, ALU.subtract, ALU.min, [Rgs], [self.RPS[b], Rm[T(1)]])
                self.act(M(T(4)), self.psf(b)[:, 0:128], AF.Exp, [], [self.RPS[b], Rm[T(4)]])
                self.act(M(T(1)), M(T(1)), AF.Exp, [Rm[T(1)]], [Rm[T(1)]])
                rel(b)
                b = st["KV"]
                pv = self.psb16(b)
                self.act(B(Pb(2)), pv[:, 0:128], AF.Copy, [Rgs], [self.RPS[b], Rb[Pb(2)]], scale=gs(2, n))
            steps.append(s1)

            def s2():
                self.tt("pool", M(T(2)), M(T(1)), Uf, ALU.mult, [Rm[T(1)], RCF], [Rm[T(2)]])
                self.tt("pool", M(T(3)), M(T(1)), Usf, ALU.mult, [Rm[T(1)], RCF], [Rm[T(3)]])
                self.tt("pool", B(Pb(0)), qT[:, cs], M(T(4)), ALU.mult, [Rqkv[0], Rm[T(4)]], [Rb[Pb(0)]])
            steps.append(s2)

            def s3():
                b = st["A"]
                self.stt("dve", MR(T(5)), self.psf(b)[:, 0:128], beta, M(T(3)), ALU.mult, ALU.mult, [Rsm, Rm[T(3)]], [self.RPS[b], Rm[T(5)]])
                self.tt("dve", B(Pb(1)), self.psf(b)[:, 128:256], M(T(2)), ALU.mult, [Rm[T(2)]], [self.RPS[b], Rb[Pb(1)]])
                rel(b)
            steps.append(s3)

            def s4():
                b = newbank(); st["X"] = b
                self.tr(self.psf(b)[:, 0:128], M(T(5)), identf, [Rm[T(5)], RCF], [self.RPS[b]])
                self.tt("pool", MR(T(9)), identf, M(T(5)), ALU.subtract, [RCF, Rm[T(5)]], [Rm[T(9)]])
            steps.append(s4)

            def s5():
                b = st["X"]
                self.cp("act", MR(T(6)), self.psf(b)[:, 0:128], [], [self.RPS[b], Rm[T(6)]])
                rel(b)
                b = st["KV"]
                pv = self.psb16(b)
                self.ts("dve", MR(T(11)), pv[:, 0:128], gs(1, n), None, ALU.mult, None, [Rgs], [self.RPS[b], Rm[T(11)]])
                self.cp("act", MR(T(12)), pv[:, 128:256], [], [self.RPS[b], Rm[T(12)]])
                rel(b)
            steps.append(s5)
            zs = {0: T(5)}
            zts = {0: T(6)}
            ns = {0: T(9)}
            for j in range(1, 7):
                zs[j] = T(7) if j % 2 == 1 else T(5)
                zts[j] = T(8) if j % 2 == 1 else T(6)
                ns[j] = T(10) if j % 2 == 1 else T(9)
            KE, VC = T(11), T(12)

            def lvl_a(j):
                def f():
                    b = newbank(); st["Z%d" % j] = b
                    self.mm(self.psf(b)[:, 0:128], MR(zs[j - 1]), MR(zts[j - 1]), True, True, [Rm[zs[j - 1]], Rm[zts[j - 1]]], [self.RPS[b]])
                    if j < 6:
                        self.mm(self.psf(b)[:, 128:256], MR(zts[j - 1]), MR(zs[j - 1]), True, True, [Rm[zs[j - 1]], Rm[zts[j - 1]]], [self.RPS[b]])
                return f

            def lvl_b(j):
                def f():
                    b = st["Z%d" % j]
                    self.cp("act", MR(zts[j]), self.psf(b)[:, 0:128], [], [self.RPS[b], Rm[zts[j]]])
                    if j < 6:
                        self.cp("dve", MR(zs[j]), self.psf(b)[:, 128:256], [], [self.RPS[b], Rm[zs[j]]])
                    rel(b)
                return f

            def lvl_c(j):
                def f():
                    b = newbank(); st["N%d" % j] = b
                    self.mm(self.psf(b)[:, 0:128], MR(zts[j]), MR(ns[j - 1]), True, True, [Rm[zts[j]], Rm[ns[j - 1]]], [self.RPS[b]])
                return f

            def lvl_d(j):
                def f():
                    b = st["N%d" % j]
                    self.tt("dve", MR(ns[j]), self.psf(b)[:, 0:128], M(ns[j - 1]), ALU.add, [Rm[ns[j - 1]]], [self.RPS[b], Rm[ns[j]]])
                    rel(b)
                return f

            def both(f1, f2):
                def f():
                    f1()
                    if f2 is not None:
                        f2()
                return f
            steps += [lvl_a(1), lvl_b(1)]
            for j in range(1, 7):
                steps += [both(lvl_c(j), lvl_a(j + 1) if j < 6 else None), both(lvl_d(j), lvl_b(j + 1) if j < 6 else None)]
            NTi = ns[6]

            def s8():
                b = newbank(); st["WU"] = b
                self.mm(self.psf(b)[:, 0:128], MR(KE), MR(NTi), True, True, [Rm[KE], Rm[NTi]], [self.RPS[b]])
                self.mm(self.psf(b)[:, 128:256], MR(NTi), MR(VC), True, True, [Rm[VC], Rm[NTi]], [self.RPS[b]])
            steps.append(s8)

            def s9():
                b = st["WU"]
                self.cp("act", B(Pb(3)), self.psf(b)[:, 0:128], [], [self.RPS[b], Rb[Pb(3)]])
                self.ts("dve", M(Pu), self.psf(b)[:, 128:256], beta, None, ALU.mult, None, [Rsm], [self.RPS[b], Rm[Pu]])
                rel(b)
            steps.append(s9)
            return steps

        def scan_steps(n, bpos, par):
            Pb = lambda k: par * 8 + bpos * 4 + k
            Pu = 26 + par * 2 + bpos
            cs = slice(n * 128, (n + 1) * 128)
            Sc, Sn = S_i[n % 2], S_i[(n + 1) % 2]
            st = {}
            steps = []

            def a0():
                b = newbank(); st["1"] = b
                self.mm(self.psf(b)[:, 0:128], B(Pb(3)), B(Sc), True, True, [Rb[Pb(3)], Rb[Sc]], [self.RPS[b]])
            steps.append(a0)

            def a1():
                b = st["1"]
                self.stt("dve", B(VN), self.psf(b)[:, 0:128], nb3[:, n, h:h + 1], M(Pu), ALU.mult, ALU.add, [Rsm, Rm[Pu]], [self.RPS[b], Rb[VN]])
                rel(b)
            steps.append(a1)

            def a2():
                b = newbank(); st["O"] = b
                self.mm(self.psf(b)[:, 0:128], B(Pb(0)), B(Sc), True, False, [Rb[Pb(0)], Rb[Sc]], [self.RPS[b]])
                self.mm(self.psf(b)[:, 0:128], B(Pb(1)), B(VN), False, True, [Rb[Pb(1)], Rb[VN]], [self.RPS[b]])
                b = newbank(); st["S"] = b
                self.mm(self.psf(b)[:, 0:128], B(Pb(2)), B(VN), True, True, [Rb[Pb(2)], Rb[VN]], [self.RPS[b]])
            steps.append(a2)

            def a3():
                b = st["S"]
                self.stt("dve", B(Sn), B(Sc), gs(3, n), self.psf(b)[:, 0:128], ALU.mult, ALU.add, [Rb[Sc], Rgs], [self.RPS[b], Rb[Sn]])
                rel(b)
                b = st["O"]
                self.act(M(JK), self.psf(b)[:, 0:128], AF.Square, [], [self.RPS[b], Rm[JK], Rm[ON]], accum_out=M(ON)[:, 0:1])
            steps.append(a3)

            def a4():
                self.act(M(ON)[:, 1:2], M(ON)[:, 0:1], AF.Ln, [Rm[ON]], [Rm[ON]], scale=1.0 / 128.0, bias=RMS_EPS)
                self.act(M(ON)[:, 2:3], M(ON)[:, 1:2], AF.Exp, [Rm[ON]], [Rm[ON]], scale=-0.5)
            steps.append(a4)

            def a5():
                b = st["O"]
                self.stt("dve", og, self.psf(b)[:, 0:128], M(ON)[:, 2:3], self.GNW, ALU.mult, ALU.mult, [Rm[ON], Rsm], [self.RPS[b], Rb[OG]])
                rel(b)
                b = newbank(); st["T"] = b
                self.tr(self.psb16(b)[:, 0:128], og, self.identb, [Rb[OG], self.RCB], [self.RPS[b]])
            steps.append(a5)

            def a6():
                b = st["T"]
                self.tt("dve", self.MT[:, 4 + h, cs], self.psb16(b)[:, 0:128], SZT[:, cs], ALU.mult, [RSZ], [self.RPS[b], self.RMT[4 + h][n]])
                rel(b)
            steps.append(a6)
            return steps

        def merged(step_lists):
            idx = [0] * len(step_lists)
            alive = True
            while alive:
                alive = False
                for k, sl in enumerate(step_lists):
                    if idx[k] < len(sl):
                        sl[idx[k]]()
                        idx[k] += 1
                        alive = True

        nb = NT // 2
        pre = lambda k: [pre_steps(2 * k, 0, k % 2), pre_steps(2 * k + 1, 1, k % 2)]
        merged(pre(0))
        for k in range(nb):
            lists = []
            if k + 1 < nb:
                lists += pre(k + 1)
            sc = scan_steps(2 * k, 0, k % 2) + scan_steps(2 * k + 1, 1, k % 2)
            lists.append(sc)
            merged(lists)

    def mixer(self, l):
        A = self.AR
        self.MT, R = A.carve(0, [8, SEQ], BF16, nres=8 * NT)
        self.RMT = [[R[k * NT + t] for t in range(NT)] for k in range(8)]
        self.layer_params(l)
        for h in range(4):
            self.da_head(l, h)
        if self.upto == "da":
            return
        self.gdn_prep(l)
        for h in range(4):
            self.gdn_head(l, h)

    def out_proj_ln1(self, l):
        P, d, A = self.P, self.d, self.AR
        Wo, (RWo,) = A.carve(32768, [NKC, DM], BF16)
        P.dma(Wo, d["w_out"][l].rearrange("(c p) n -> p c n", p=128), writes=[RWo], eng="pool")
        for t in range(NT):
            for hf in range(2):
                b = self.bank("o", [0, 1, 2, 3])
                ps = self.psf(b)
                for kc in range(NKC):
                    self.mm(ps, self.MT[:, kc, t * 128:(t + 1) * 128], Wo[:, kc, hf * 512:(hf + 1) * 512], kc == 0, kc == NKC - 1,
                            [self.RMT[kc][t], RWo], [self.RPS[b]])
                xs = self.X[:, t, hf * 512:(hf + 1) * 512]
                self.stt("dve", xs, xs, ALPHA, ps, ALU.mult, ALU.add, [self.RX[t]], [self.RPS[b], self.RX[t]])
        self.layer_norm_all("ln1_g", "ln1_b", l, 49152)

    def ffn_ln2(self, l, last):
        P, d, A = self.P, self.d, self.AR
        X, RX, XT, RXT = self.X, self.RX, self.XT, self.RXT
        moe = (l % 2 == 1)
        li = l // 2
        o = 0
        WS = []
        for s in range(2):
            wg, (Rwg,) = A.carve(o, [NKC, 512], BF16); o += 8192
            wu, (Rwu,) = A.carve(o, [NKC, 512], BF16); o += 8192
            wd, (Rwd,) = A.carve(o, [4, DM], BF16); o += 8192
            WS.append((wg, Rwg, wu, Rwu, wd, Rwd))
        hT, RhT = A.carve(o, [2, 4, 512], BF16, nres=2); o += 8192
        sg, Rsg = A.carve(o, [2, 512], BF16, nres=2); o += 2048
        lnoff = o; o += 16896
        GT, (RGT,) = A.carve(o, [NT, 8], F32); o += 512
        if moe:
            RWt, (RRW,) = A.carve(o, [NKC, 8], F32); o += 256
            lg, (Rlg,) = A.carve(o, [64], F32); o += 256
            assert o <= 81920
            XTf, RXTf = A.carve(lnoff, [2, NKC, 128], F32, nres=2)
            P.dma(RWt, d["router_w"][li].rearrange("(c p) e -> p c e", p=128), writes=[RRW])
            for t in range(NT):
                s = t % 2
                for half in range(2):
                    b = self.bank("rt", [4, 5, 6, 7])
                    ps = self.psf(b)
                    for q in range(4):
                        kc = half * 4 + q
                        self.tr(ps[:, q * 128:(q + 1) * 128], X[:, t, kc * 128:(kc + 1) * 128], self.identf, [RX[t], self.RCF], [self.RPS[b]])
                    self.cp("act" if half else "dve", XTf[:, s, half * 4:half * 4 + 4, :], ps.rearrange("p (a n) -> p a n", a=4),
                            [], [self.RPS[b], RXTf[s]])
                b = self.bank("rt", [4, 5, 6, 7])
                ps = self.psf(b)
                for kc in range(NKC):
                    self.mm(ps[:, 0:8], XTf[:, s, kc, :], RWt[:, kc, :], kc == 0, kc == NKC - 1, [RXTf[s], RRW], [self.RPS[b]])
                self.cp("dve", lg[:, 0:8], ps[:, 0:8], [], [self.RPS[b], Rlg])
                self.P.op("dve", lambda e: e.max(out=lg[:, 8:16], in_=lg[:, 0:8]), [Rlg], [Rlg])
                self.tt("dve", lg[:, 16:17], lg[:, 9:10], lg[:, 8:9], ALU.subtract, [Rlg], [Rlg])
                self.act(lg[:, 17:18], lg[:, 16:17], AF.Exp, [Rlg], [Rlg])
                self.ts("dve", lg[:, 18:19], lg[:, 17:18], 1.0, None, ALU.add, None, [Rlg], [Rlg])
                self.P.op("dve", lambda e: e.reciprocal(out=lg[:, 19:20], in_=lg[:, 18:19]), [Rlg], [Rlg])
                self.tt("dve", lg[:, 20:21], lg[:, 17:18], lg[:, 19:20], ALU.mult, [Rlg], [Rlg])
                self.ts("dve", lg[:, 24:32], lg[:, 0:8], lg[:, 8:9], lg[:, 19:20], ALU.is_equal, ALU.mult, [Rlg], [Rlg])
                self.ts("dve", lg[:, 32:40], lg[:, 0:8], lg[:, 9:10], lg[:, 20:21], ALU.is_equal, ALU.mult, [Rlg], [Rlg])
                self.tt("dve", GT[:, t, :], lg[:, 24:32], lg[:, 32:40], ALU.add, [Rlg], [RGT])
        for t in range(NT):
            self.P.op("act", lambda e, t=t: e.mul(out=X[:, t, :], in_=X[:, t, :], mul=ALPHA), [RX[t]], [RX[t]])
        if moe:
            groups = [(e, c0, 4) for e in range(NEXP) for c0 in range(0, FF_MOE, 512)]
        else:
            groups = [(None, c0, min(4, (FF_DENSE - c0) // 128)) for c0 in range(0, FF_DENSE, 512)]

        def load(gi):
            e, c0, nch = groups[gi]
            wg, Rwg, wu, Rwu, wd, Rwd = WS[gi % 2]
            if moe:
                srcg, srcu, srcd = d["moe_w_gate"][li, e], d["moe_w_up"][li, e], d["moe_w_down"][li, e]
            else:
                srcg, srcu, srcd = d["ffn_w_gate"][li], d["ffn_w_up"][li], d["ffn_w_down"][li]
            w = nch * 128
            P.dma(wg[:, :, 0:w], srcg.rearrange("(c p) n -> p c n", p=128)[:, :, c0:c0 + w], writes=[Rwg], eng="pool")
            P.dma(wu[:, :, 0:w], srcu.rearrange("(c p) n -> p c n", p=128)[:, :, c0:c0 + w], writes=[Rwu], eng="pool")
            P.dma(wd[:, 0:nch, :], srcd[c0:c0 + w, :].rearrange("(c p) n -> p c n", p=128), writes=[Rwd], eng="pool")

        items = [(gi, tg) for gi in range(len(groups)) for tg in range(4)]

        def up(k):
            gi, tg = items[k]
            e, c0, nch = groups[gi]
            wg, Rwg, wu, Rwu, wd, Rwd = WS[gi % 2]
            hs = k % 2
            cols = slice(tg * 512, (tg + 1) * 512)
            for c in range(nch):
                bg = self.bank("fg", [0, 1])
                bu = self.bank("fu", [2, 3])
                for kc in range(NKC):
                    self.mm(self.psf(bg), wg[:, kc, c * 128:(c + 1) * 128], XT[:, kc, cols], kc == 0, kc == NKC - 1,
                            [Rwg] + RXT[4 * tg:4 * tg + 4], [self.RPS[bg]])
                for kc in range(NKC):
                    self.mm(self.psf(bu), wu[:, kc, c * 128:(c + 1) * 128], XT[:, kc, cols], kc == 0, kc == NKC - 1,
                            [Rwu] + RXT[4 * tg:4 * tg + 4], [self.RPS[bu]])
                s = self.bank("sg", [0, 1])
                self.act(sg[:, s, :], self.psf(bg), AF.Silu, [], [self.RPS[bg], Rsg[s]])
                self.tt("dve", hT[:, hs, c, :], self.psf(bu), sg[:, s, :], ALU.mult, [Rsg[s]], [self.RPS[bu], RhT[hs]])

        def down(k):
            gi, tg = items[k]
            e, c0, nch = groups[gi]
            wg, Rwg, wu, Rwu, wd, Rwd = WS[gi % 2]
            hs = k % 2
            for ti in range(4):
                t = 4 * tg + ti
                for hf in range(2):
                    b = self.bank("fd", [4, 5, 6, 7])
                    ps = self.psf(b)
                    for c in range(nch):
                        self.mm(ps, hT[:, hs, c, ti * 128:(ti + 1) * 128], wd[:, c, hf * 512:(hf + 1) * 512], c == 0, c == nch - 1,
                                [RhT[hs], Rwd], [self.RPS[b]])
                    xs = X[:, t, hf * 512:(hf + 1) * 512]
                    if moe:
                        self.stt("dve", xs, ps, GT[:, t, e:e + 1], xs, ALU.mult, ALU.add, [RGT, RX[t]], [self.RPS[b], RX[t]])
                    else:
                        self.tt("dve", xs, ps, xs, ALU.add, [RX[t]], [self.RPS[b], RX[t]])

        load(0)
        for k in range(len(items)):
            gi, tg = items[k]
            up(k)
            if k > 0:
                down(k - 1)
            if tg == 0 and gi + 1 < len(groups):
                load(gi + 1)
        down(len(items) - 1)
        self.layer_norm_all("ln2_g", "ln2_b", l, lnoff, need_xt=not last)

    def layer(self, l):
        if self.upto == "pro":
            return
        self.mixer(l)
        if self.upto in ("da", "mix"):
            return
        self.out_proj_ln1(l)
        if self.upto == "ln1":
            return
        self.ffn_ln2(l, last=(l == self.n_layers - 1))

    def output(self):
        self.dump_X("y")

    def debug_dump(self):
        if self.upto == "pro":
            import os
            if "xt" in os.environ.get("PRO_SKIP", ""):
                self.dump_X("dbg")
            else:
                self.dump_featmajor_bf16(self.XT[:], self.RXT)
        elif self.upto == "da":
            self.dump_featmajor_bf16(self.MT[:, 0:4, :], [r for rl in self.RMT[0:4] for r in rl])
        elif self.upto == "mix":
            self.dump_featmajor_bf16(self.MT, [r for rl in self.RMT for r in rl])
        else:
            self.dump_X("dbg")


_CONST_CACHE = {}


def _consts():
    if "cf" in _CONST_CACHE:
        return _CONST_CACHE["cf"]
    cf = np.zeros((128, 648), np.float32)
    idx = np.arange(128)
    cf[:, 0:128] = np.eye(128, dtype=np.float32)
    cf[:, 128:256] = (idx[:, None] <= idx[None, :]).astype(np.float32)
    cf[:, 256:384] = (idx[:, None] < idx[None, :]).astype(np.float32)
    cf[:, 384:512] = 1.0
    rm = np.zeros((128, 128), np.float32)
    for blk in (0, 64):
        for dd in range(32):
            rm[blk + dd + 32, blk + dd] = -1.0
            rm[blk + dd, blk + dd + 32] = 1.0
    cf[:, 512:640] = rm
    inv_freq = 10000.0 ** (-np.arange(0, 64, 2, dtype=np.float32) / 64.0)
    cf[:, 640] = (inv_freq[idx % 32].astype(np.float64) / TWO_PI).astype(np.float32)
    _CONST_CACHE["cf"] = cf
    return cf


def build_program(n_layers=DEPTH, upto=None):
    nc = bass.Bass("TRN2", target_bir_lowering=False)
    with ExitStack() as st:
        P = Prog(nc, st)
        K = Kern(nc, P, st, n_layers, upto)
        K.prologue()
        for l in range(n_layers):
            K.layer(l)
        if upto is None:
            K.output()
        else:
            K.debug_dump()
        P.finish()
    return nc


WEIGHT_KEYS = ("w_in", "conv_w", "a_log", "dt_bias", "gdn_norm_w", "lam_q1", "lam_k1", "lam_q2", "lam_k2",
               "subln_w", "w_out", "ln1_g", "ln1_b", "ln2_g", "ln2_b", "ffn_w_gate", "ffn_w_up", "ffn_w_down",
               "router_w", "moe_w_gate", "moe_w_up", "moe_w_down")


def make_in_maps(inputs, cores):
    cf = _consts()
    shared = {k: np.ascontiguousarray(np.asarray(inputs[k], dtype=np.float32)) for k in WEIGHT_KEYS}
    x = np.asarray(inputs["x"], dtype=np.float32)
    pos = np.asarray(inputs["positions"]).astype(np.int32)
    maps = []
    for b in cores:
        m = dict(shared)
        m["x"] = np.ascontiguousarray(x[b])
        m["pos"] = np.ascontiguousarray(pos[b:b + 1])
        m["cf"] = cf
        maps.append(m)
    return maps


def kernel(**inputs):
    nc = build_program()
    in_maps = make_in_maps(inputs, range(8))
    res = run_bass_kernel_spmd(nc, in_maps, core_ids=list(range(8)))
    out = np.stack([np.asarray(r["y"], dtype=np.float32) for r in res.results], axis=0)
    return out
```
